# Optimizing a Trainium2 kernel written in Bass

```python
import jax, jax.numpy as jnp
from jax import lax
import numpy as np

D_MODEL = 1024
BATCH = 32
SEQ = 2048
DEPTH = 1

D_MIX = D_MODEL
HEAD_DIM = 64
RWKV_WIDTH = D_MIX // 2
RWKV_HEADS = RWKV_WIDTH // HEAD_DIM
ATTN_WIDTH = D_MIX - RWKV_WIDTH
ATTN_Q_HEADS = ATTN_WIDTH // HEAD_DIM
ATTN_KV_HEADS = 2
ATTN_GROUP = ATTN_Q_HEADS // ATTN_KV_HEADS
ATTN_KV_WIDTH = ATTN_KV_HEADS * HEAD_DIM
WINDOW = 128
D_DECAY_LORA = 32
D_AAA_LORA = 32
D_GATE_LORA = 96
RWKV_COLS = 3 * RWKV_WIDTH + D_DECAY_LORA + D_AAA_LORA + D_GATE_LORA
ATTN_COLS = ATTN_WIDTH + 2 * ATTN_KV_WIDTH
IN_COLS = RWKV_COLS + ATTN_COLS
PEER_HEADS = 8
PEER_N_KEYS = 128
PEER_N_EXPERTS = PEER_N_KEYS * PEER_N_KEYS
PEER_D_KEY = 256
PEER_HALF = PEER_D_KEY // 2
PEER_TOPK = 16
PEER_TOKEN_BLOCK = 128
RMS_EPS = 1e-6
LNX_EPS = 64e-5
NEG_INF = -1e30

kernel_name = "hymba_rwkv7_swa_sink_peer_block"


def rms_norm(x, g):
    xf = x.astype(jnp.float32)
    y = xf * lax.rsqrt(jnp.mean(xf * xf, axis=-1, keepdims=True) + RMS_EPS)
    return (y * g.astype(jnp.float32)).astype(x.dtype)


def token_shift(p, mu):
    prev = jnp.pad(p[:, :-1], ((0, 0), (1, 0), (0, 0)))
    return p + mu * (prev - p)


def rwkv7_mixer(p, mu, w0, w_up, a0, a_up, g_up, k_k, k_a, r_k, lnx_w, lnx_b):
    B, S, _ = p.shape
    H, Dh, W = RWKV_HEADS, HEAD_DIM, RWKV_WIDTH
    f32 = jnp.float32
    p = token_shift(p.astype(f32), mu.astype(f32))
    r = p[..., :W]
    k = p[..., W:2 * W]
    v = p[..., 2 * W:3 * W]
    xw = p[..., 3 * W:3 * W + D_DECAY_LORA]
    xa = p[..., 3 * W + D_DECAY_LORA:3 * W + D_DECAY_LORA + D_AAA_LORA]
    xg = p[..., 3 * W + D_DECAY_LORA + D_AAA_LORA:]
    w = -jax.nn.softplus(-(w0 + jnp.tanh(xw) @ w_up)) - 0.5
    decay = jnp.exp(-jnp.exp(w))
    a = jax.nn.sigmoid(a0 + xa @ a_up)
    g = jax.nn.sigmoid(xg) @ g_up
    heads = lambda t: t.reshape(B, S, H, Dh)
    kk = heads(k * k_k)
    kk = kk / jnp.maximum(jnp.sqrt(jnp.sum(kk * kk, axis=-1, keepdims=True)), 1e-12)
    k = k * (1.0 + (a - 1.0) * k_a)
    r_h, k_h, v_h, a_h, w_h = heads(r), heads(k), heads(v), heads(a), heads(decay)
    tm = lambda t: jnp.moveaxis(t, 1, 0)
    seqs = (tm(r_h), tm(w_h), tm(k_h), tm(v_h), tm(-kk), tm(kk * a_h))

    def step(state, inp):
        r_t, w_t, k_t, v_t, a_t, b_t = inp
        sa = jnp.einsum('bhvk,bhk->bhv', state, a_t)
        state = (state * w_t[:, :, None, :] + sa[..., None] * b_t[:, :, None, :]
                 + v_t[..., None] * k_t[:, :, None, :])
        return state, jnp.einsum('bhvk,bhk->bhv', state, r_t)

    s0 = jnp.zeros((B, H, Dh, Dh), f32)
    _, ys = lax.scan(step, s0, seqs)
    y = jnp.moveaxis(ys, 0, 1)
    mean = jnp.mean(y, axis=-1, keepdims=True)
    var = jnp.mean(jnp.square(y - mean), axis=-1, keepdims=True)
    y = ((y - mean) * lax.rsqrt(var + LNX_EPS)).reshape(B, S, W) * lnx_w + lnx_b
    bonus = jnp.sum(r_h * k_h * r_k, axis=-1, keepdims=True) * v_h
    y = (y + bonus.reshape(B, S, W)) * g
    return y


def swa_sink_attention(q, k, v, sinks):
    B, S, _ = q.shape
    nb = S // WINDOW
    KV, G, Dh = ATTN_KV_HEADS, ATTN_GROUP, HEAD_DIM
    f32 = jnp.float32
    scale = 1.0 / np.sqrt(Dh)
    q = q.reshape(B, nb, WINDOW, KV, G, Dh)
    k = k.reshape(B, nb, WINDOW, KV, Dh)
    v = v.reshape(B, nb, WINDOW, KV, Dh)
    prev = lambda t: jnp.concatenate([jnp.zeros_like(t[:, :1]), t[:, :-1]], axis=1)
    k_ext = jnp.concatenate([prev(k), k], axis=2)
    v_ext = jnp.concatenate([prev(v), v], axis=2)
    qi = jnp.arange(WINDOW)[:, None]
    kj = jnp.arange(2 * WINDOW)[None, :]
    diff = qi + WINDOW - kj
    band = (diff >= 0) & (diff < WINDOW)
    in_current = kj >= WINDOW
    sink_logits = sinks.astype(f32).reshape(KV, G, 1, 1)

    def block(args):
        q_b, k_b, v_b, n = args
        allowed = band & ((n > 0) | in_current)
        s = jnp.einsum('bqkgd,bskd->bkgqs', q_b.astype(f32), k_b.astype(f32)) * scale
        s = jnp.where(allowed, s, NEG_INF)
        sink = jnp.broadcast_to(sink_logits, s.shape[:-1] + (1,))
        pr = jax.nn.softmax(jnp.concatenate([s, sink], axis=-1), axis=-1)[..., :-1]
        o = jnp.einsum('bkgqs,bskd->bqkgd', pr, v_b.astype(f32))
        return o.astype(q_b.dtype)

    mv = lambda t: jnp.moveaxis(t, 1, 0)
    o = lax.map(block, (mv(q), mv(k_ext), mv(v_ext), jnp.arange(nb)))
    return jnp.moveaxis(o, 0, 1).reshape(B, S, ATTN_WIDTH)


def peer_ffn(h, wq, subkeys, u_tab, v_tab):
    B, S, D = h.shape
    T = B * S
    nblk = T // PEER_TOKEN_BLOCK
    K, H = PEER_TOPK, PEER_HEADS
    f32 = jnp.float32
    xb = h.reshape(nblk, PEER_TOKEN_BLOCK, D)

    def block(x_b):
        q = (x_b @ wq).reshape(-1, H, 2, PEER_HALF).astype(f32)
        s = jnp.einsum('thcd,hcnd->thcn', q, subkeys.astype(f32))
        s_top, i_top = lax.top_k(s, K)
        cand_s = (s_top[:, :, 0, :, None] + s_top[:, :, 1, None, :]).reshape(-1, H, K * K)
        cand_i = (i_top[:, :, 0, :, None] * PEER_N_KEYS + i_top[:, :, 1, None, :]).reshape(-1, H, K * K)
        best_s, best_pos = lax.top_k(cand_s, K)
        idx = jnp.take_along_axis(cand_i, best_pos, axis=-1).reshape(-1, H * K)
        gate = jax.nn.softmax(best_s, axis=-1).reshape(-1, H * K).astype(x_b.dtype)
        u = u_tab[idx]
        act = jax.nn.gelu(jnp.einsum('td,ted->te', x_b, u), approximate=False)
        return jnp.einsum('te,ted->td', gate * act, v_tab[idx])

    y = lax.map(block, xb)
    return y.reshape(B, S, D)


def setup_inputs(seed: int = 0) -> dict:
    key = jax.random.key(seed)
    ks = jax.random.split(key, 26)
    L, D, W = DEPTH, D_MODEL, RWKV_WIDTH
    nrm = lambda k, shape, s: jax.random.normal(k, shape, jnp.float32) * s
    return {
        "x": nrm(ks[0], (BATCH, SEQ, D), 1.0),
        "ln1_g": 1.0 + nrm(ks[1], (L, D), 0.02),
        "w_in": nrm(ks[2], (L, D, IN_COLS), D ** -0.5),
        "b_attn": nrm(ks[3], (L, ATTN_COLS), 0.02),
        "mu_shift": jax.random.uniform(ks[4], (L, RWKV_COLS), jnp.float32),
        "w0": jax.random.uniform(ks[5], (L, W), jnp.float32, -4.0, 1.0),
        "w_up": nrm(ks[6], (L, D_DECAY_LORA, W), 0.1),
        "a0": nrm(ks[7], (L, W), 0.1),
        "a_up": nrm(ks[8], (L, D_AAA_LORA, W), 0.1),
        "g_up": nrm(ks[9], (L, D_GATE_LORA, W), D_GATE_LORA ** -0.5),
        "k_k": 0.85 + nrm(ks[10], (L, W), 0.02),
        "k_a": 1.0 + nrm(ks[11], (L, W), 0.02),
        "r_k": nrm(ks[12], (L, RWKV_HEADS, HEAD_DIM), 0.1),
        "lnx_w": 1.0 + nrm(ks[13], (L, W), 0.02),
        "lnx_b": nrm(ks[14], (L, W), 0.02),
        "attn_sinks": nrm(ks[15], (L, ATTN_Q_HEADS), 1.0),
        "attn_norm_g": 1.0 + nrm(ks[16], (L, ATTN_WIDTH), 0.02),
        "w_out": nrm(ks[17], (L, D_MIX, D), D_MIX ** -0.5),
        "ln2_g": 1.0 + nrm(ks[18], (L, D), 0.02),
        "peer_wq": nrm(ks[19], (L, D, PEER_HEADS * PEER_D_KEY), D ** -0.5),
        "peer_subkeys": nrm(ks[20], (L, PEER_HEADS, 2, PEER_N_KEYS, PEER_HALF), PEER_HALF ** -0.5),
        "peer_u": nrm(ks[21], (L, PEER_N_EXPERTS, D), D ** -0.5),
        "peer_v": nrm(ks[22], (L, PEER_N_EXPERTS, D), PEER_HEADS ** -0.5),
        "lnf_g": 1.0 + nrm(ks[23], (D,), 0.02),
    }


def reference(x, ln1_g, w_in, b_attn, mu_shift, w0, w_up, a0, a_up, g_up, k_k, k_a, r_k,
              lnx_w, lnx_b, attn_sinks, attn_norm_g, w_out, ln2_g, peer_wq, peer_subkeys,
              peer_u, peer_v, lnf_g):
    for l in range(DEPTH):
        h = rms_norm(x, ln1_g[l])
        p = h @ w_in[l]
        p_rwkv = p[..., :RWKV_COLS]
        p_attn = p[..., RWKV_COLS:] + b_attn[l]
        y_rwkv = rwkv7_mixer(p_rwkv, mu_shift[l], w0[l], w_up[l], a0[l], a_up[l], g_up[l],
                             k_k[l], k_a[l], r_k[l], lnx_w[l], lnx_b[l]).astype(x.dtype)
        q = p_attn[..., :ATTN_WIDTH]
        k = p_attn[..., ATTN_WIDTH:ATTN_WIDTH + ATTN_KV_WIDTH]
        v = p_attn[..., ATTN_WIDTH + ATTN_KV_WIDTH:]
        y_attn = rms_norm(swa_sink_attention(q, k, v, attn_sinks[l]), attn_norm_g[l])
        x = x + jnp.concatenate([y_rwkv, y_attn], axis=-1) @ w_out[l]
        x = x + peer_ffn(rms_norm(x, ln2_g[l]), peer_wq[l], peer_subkeys[l], peer_u[l], peer_v[l])
    return rms_norm(x, lnf_g)
```

```python
from contextlib import ExitStack
import numpy as np
import concourse.bass as bass
import concourse.mybir as mybir

F32 = mybir.dt.float32
BF16 = mybir.dt.bfloat16
F32R = mybir.dt.float32r
U32 = mybir.dt.uint32
I32 = mybir.dt.int32
AF = mybir.ActivationFunctionType
ALU = mybir.AluOpType
AX = mybir.AxisListType

NSLOT = 8
PEER_A_STEPS = 80
PEER_B_START = 100
import os as _osx
SAME_ENGINE_INORDER = ('pe',)


class Sched:
    def __init__(self, nc, es):
        self.nc = nc
        self.engs = ['pe', 'act', 'dve', 'pool', 'sp']
        self.dq = ('sp', 'pool', 'act')
        self.esem = {e: es.enter_context(nc.semaphore('s_' + e)) for e in self.engs}
        self.dsem = {e: [es.enter_context(nc.semaphore('d_%s%d' % (e, s))) for s in range(NSLOT)]
                     for e in self.dq}
        self.cnt = {e: 0 for e in self.engs}
        self.dcnt = {e: 0 for e in self.dq}
        self.slot_uses = {e: [0] * NSLOT for e in self.dq}
        self.waited = {e: {} for e in self.engs}
        self.pending = None
        self.ops = []
        self.dummy = es.enter_context(nc.sbuf_tensor('sched_dummy', [128, 8], F32))
        self.n_emitted = 0

    def add(self, eng, fn, r=(), w=(), dma=False):
        self.ops.append(dict(eng=eng, fn=fn, r=tuple(r), w=tuple(w), dma=dma))

    def pe(self, fn, r=(), w=()):
        self.add('pe', fn, r, w)

    def act(self, fn, r=(), w=()):
        self.add('act', fn, r, w)

    def dve(self, fn, r=(), w=()):
        self.add('dve', fn, r, w)

    def pool(self, fn, r=(), w=()):
        self.add('pool', fn, r, w)

    def dma(self, fn, r=(), w=(), q='sp'):
        self.add(q, fn, r, w, dma=True)

    def flush(self):
        nc = self.nc
        dummy = self.dummy
        self.dve(lambda e: e.memset(dummy[:], 0.0), w=['__barrier__'])
        ops = self.ops
        self.ops = []
        self.n_emitted += len(ops)
        last_w = {}
        readers = {}
        for i, o in enumerate(ops):
            deps = set()
            for k in o['r']:
                if k in last_w:
                    deps.add(last_w[k])
            for k in o['w']:
                if k in last_w:
                    deps.add(last_w[k])
                deps.update(readers.get(k, ()))
            deps.discard(i)
            o['deps'] = deps
            for k in o['r']:
                readers.setdefault(k, []).append(i)
            for k in o['w']:
                last_w[k] = i
                readers[k] = []
        J = len(ops) - 1
        last_eng = {}
        for i, o in enumerate(ops[:-1]):
            if o['dma']:
                ops[J]['deps'].add(i)
            else:
                last_eng[o['eng']] = i
        ops[J]['deps'].update(last_eng.values())
        needed = {J}
        for o in ops:
            needed.update(o['deps'])
        slot_last = {e: [None] * NSLOT for e in self.dq}
        for i, o in enumerate(ops):
            e = o['eng']
            if o['dma']:
                s = self.dcnt[e] % NSLOT
                self.dcnt[e] += 1
                self.slot_uses[e][s] += 1
                o['sem'] = self.dsem[e][s]
                o['val'] = 16 * self.slot_uses[e][s]
                o['inc'] = 16
                if slot_last[e][s] is not None:
                    o['deps'].add(slot_last[e][s])
                slot_last[e][s] = i
            elif i in needed:
                self.cnt[e] += 1
                o['sem'] = self.esem[e]
                o['val'] = self.cnt[e]
                o['inc'] = 1
            else:
                o['sem'] = None
        per = {e: [] for e in self.engs}
        for i, o in enumerate(ops):
            per[o['eng']].append(i)
        pending = self.pending

        def run(ename, eng):
            waited = self.waited[ename]
            if pending is not None and ename != 'dve':
                if waited.get(id(pending[0]), 0) < pending[1]:
                    eng.wait_ge(pending[0], pending[1])
                    waited[id(pending[0])] = pending[1]
            for i in per[ename]:
                o = ops[i]
                need = {}
                for j in o['deps']:
                    p = ops[j]
                    if p['sem'] is None:
                        continue
                    if (not p['dma']) and p['eng'] == ename and ename in SAME_ENGINE_INORDER:
                        continue
                    key = id(p['sem'])
                    if key not in need or need[key][1] < p['val']:
                        need[key] = (p['sem'], p['val'])
                for key, (sem, val) in need.items():
                    if waited.get(key, 0) >= val:
                        continue
                    eng.wait_ge(sem, val)
                    waited[key] = val
                ins = o['fn'](eng)
                if o['sem'] is not None:
                    ins.then_inc(o['sem'], o['inc'])
            if ename in self.dsem:
                for s in range(NSLOT):
                    v = 16 * self.slot_uses[ename][s]
                    if v > 0 and waited.get(id(self.dsem[ename][s]), 0) < v:
                        eng.wait_ge(self.dsem[ename][s], v)
                        waited[id(self.dsem[ename][s])] = v

        with nc.Block() as block:
            @block.tensor
            def _(e):
                run('pe', e)

            @block.scalar
            def _(e):
                run('act', e)

            @block.vector
            def _(e):
                run('dve', e)

            @block.gpsimd
            def _(e):
                run('pool', e)

            @block.sync
            def _(e):
                run('sp', e)
        self.pending = (ops[J]['sem'], ops[J]['val'])

from concourse.bass_utils import run_bass_kernel_spmd

D = 1024
HD = 64
NEG = -1e30
C0 = -0.6065306597126334
RMS_EPS = 1e-6
LNX_EPS = 64e-5


class KB:
    def __init__(self, nc, S):
        self.nc = nc
        self.S = S

    def mm(self, out, lhsT, rhs, start=True, stop=True, r=(), w=()):
        self.S.pe(lambda e: e.matmul(out, lhsT=lhsT, rhs=rhs, start=start, stop=stop), r, w)

    def mmr(self, out, lhsT, rhs, start=True, stop=True, r=(), w=()):
        lt = lhsT if lhsT.dtype == F32R else lhsT.bitcast(F32R)
        rh = rhs if rhs.dtype == F32R else rhs.bitcast(F32R)
        self.S.pe(lambda e: e.matmul(out, lhsT=lt, rhs=rh, start=start, stop=stop), r, w)

    def tr(self, out, in_, ident, r=(), w=()):
        self.S.pe(lambda e: e.transpose(out, in_, ident), r, w)

    def tt(self, out, in0, in1, op, r=(), w=(), eng='dve'):
        self.S.add(eng, lambda e: e.tensor_tensor(out=out, in0=in0, in1=in1, op=op), r, w)

    def ts(self, out, in0, s1, op0, s2=None, op1=None, r=(), w=(), eng='dve'):
        if op1 is None:
            self.S.add(eng, lambda e: e.tensor_scalar(out=out, in0=in0, scalar1=s1, scalar2=None, op0=op0), r, w)
        else:
            self.S.add(eng, lambda e: e.tensor_scalar(out=out, in0=in0, scalar1=s1, scalar2=s2, op0=op0, op1=op1), r, w)

    def stt(self, out, in0, scalar, in1, op0, op1, r=(), w=(), eng='dve'):
        self.S.add(eng, lambda e: e.scalar_tensor_tensor(out=out, in0=in0, scalar=scalar, in1=in1, op0=op0, op1=op1), r, w)

    def af(self, out, in_, func, bias=None, scale=None, accum=None, r=(), w=()):
        kw = {}
        if bias is not None:
            kw['bias'] = bias
        if scale is not None:
            kw['scale'] = scale
        if accum is not None:
            kw['accum_out'] = accum
        self.S.act(lambda e: e.activation(out=out, in_=in_, func=func, **kw), r, w)

    def cp(self, out, in_, r=(), w=(), eng='dve'):
        if eng == 'act':
            self.S.act(lambda e: e.copy(out=out, in_=in_), r, w)
        else:
            self.S.add(eng, lambda e: e.tensor_copy(out=out, in_=in_), r, w)

    def red(self, out, in_, op, r=(), w=(), axis=AX.X):
        self.S.dve(lambda e: e.tensor_reduce(out=out, in_=in_, axis=axis, op=op), r, w)

    def recip(self, out, in_, r=(), w=()):
        self.S.dve(lambda e: e.reciprocal(out=out, in_=in_), r, w)

    def memset(self, ap, val, r=(), w=(), eng='dve'):
        self.S.add(eng, lambda e: e.memset(ap, val), r, w)

    def dma(self, out, in_, r=(), w=(), q='sp', slow=False):
        if slow:
            self.S.dma(lambda e: e.dma_start(out=out, in_=in_, allow_slow_non_contiguous=True), r, w, q=q)
        else:
            self.S.dma(lambda e: e.dma_start(out=out, in_=in_), r, w, q=q)


def bc(ap, axis, shape):
    return ap.unsqueeze(axis).broadcast_to(shape)


def setup_consts(nc, kb, es):
    sb = lambda name, shape, dt: es.enter_context(nc.sbuf_tensor(name, shape, dt))
    c = {}
    c['iot'] = sb('iot', [128, 128], F32)
    c['pidx'] = sb('pidx', [128, 1], F32)
    c['idf'] = sb('idf', [128, 128], F32)
    c['idb'] = sb('idb', [128, 128], BF16)
    iot, pidx, idf, idb = c['iot'], c['pidx'], c['idf'], c['idb']
    kb.S.pool(lambda e: e.iota(iot[:], pattern=[[1, 128]], base=0, channel_multiplier=0,
                               allow_small_or_imprecise_dtypes=True), w=['iot'])
    kb.S.pool(lambda e: e.iota(pidx[:], pattern=[[0, 1]], base=0, channel_multiplier=1,
                               allow_small_or_imprecise_dtypes=True), w=['pidx'])
    kb.ts(idf[:], iot[:], pidx[:, 0:1], ALU.is_equal, r=['iot', 'pidx'], w=['idf'])
    kb.cp(idb[:], idf[:], r=['idf'], w=['idb'])
    return c


def phase_rwkv(nc, kb, c, NSEQ, SEQ, x, A, yr):
    RDT = F32R
    S = kb.S
    NT = SEQ // 128
    idf, idb, iot, pidx = c['idf'], c['idb'], c['iot'], c['pidx']
    with ExitStack() as es:
        sb = lambda name, shape, dt: es.enter_context(nc.sbuf_tensor(name, shape, dt))
        ps = es.enter_context(nc.psum_tensor('ps_a', [128, 8, 512], F32))
        W1 = sb('W1', [128, 8, 1696], BF16)
        stage = [sb('stage0', [128, 1696], F32)] * 2
        g1c = sb('g1c', [128, 8], F32)
        w0_b = sb('w0_b', [128, 512], F32)
        a0_b = sb('a0_b', [128, 512], F32)
        kk_b = sb('kk_b', [128, 512], F32)
        ka_b = sb('ka_b', [128, 512], F32)
        lw_b = sb('lw_b', [128, 512], F32)
        lb_b = sb('lb_b', [128, 512], F32)
        rk_b = sb('rk_b', [128, 512], F32)
        mu_b = sb('mu_b', [128, 1536], F32)
        mucol = sb('mucol', [96, 3], F32)
        wup = sb('wup', [32, 512], F32)
        aup = sb('aup', [32, 512], F32)
        gup = sb('gup', [96, 512], F32)
        m_ab = sb('m_ab', [128, 256], F32)
        m_ls = sb('m_ls', [128, 256], F32)
        triI = sb('triI', [128, 128], F32)
        triE = sb('triE', [128, 128], F32)
        triT = sb('triT', [128, 128], F32)
        kb.dma(g1c[:, :], A['ln1_g'][0].rearrange("(c p) -> p c", p=128), w=['g1c'], slow=True)
        for cc in range(8):
            st = stage[cc % 2]
            kb.dma(st[:], A['w_in'][0, cc * 128:(cc + 1) * 128, 0:1696], w=['stage0'])
            (lambda st_, cc_: S.act(lambda e: e.mul(out=W1[:, cc_, :], in_=st_[:], mul=g1c[:, cc_:cc_ + 1]),
                                    r=['stage0', 'g1c'], w=['W1']))(st, cc)
        for t, name in ((w0_b, 'w0'), (a0_b, 'a0'), (kk_b, 'k_k'), (ka_b, 'k_a'), (lw_b, 'lnx_w'), (lb_b, 'lnx_b')):
            kb.dma(t[:], A[name][0:1, :].partition_broadcast(128), w=['cst'])
        kb.dma(rk_b[:], A['r_k'].rearrange("o h d -> o (h d)").partition_broadcast(128), w=['cst'])
        kb.dma(mu_b[:], A['mu_shift'][0:1, 0:1536].partition_broadcast(128), w=['cst'])
        kb.dma(mucol[0:32, 0:1], A['mu_shift'][0, 1536:1568].rearrange("(p o) -> p o", o=1), w=['cst'])
        kb.dma(mucol[0:32, 1:2], A['mu_shift'][0, 1568:1600].rearrange("(p o) -> p o", o=1), w=['cst'])
        kb.dma(mucol[0:96, 2:3], A['mu_shift'][0, 1600:1696].rearrange("(p o) -> p o", o=1), w=['cst'])
        kb.dma(wup[:], A['w_up'][0], w=['cst'])
        kb.dma(aup[:], A['a_up'][0], w=['cst'])
        kb.dma(gup[:], A['g_up'][0], w=['cst'])
        kb.ts(m_ab[:, 0:128], iot[:], pidx[:, 0:1], ALU.is_gt, r=['iot', 'pidx'], w=['cst'])
        kb.ts(m_ab[:, 128:256], iot[:], pidx[:, 0:1], ALU.is_ge, r=['iot', 'pidx'], w=['cst'])
        kb.ts(m_ls[:, 0:128], iot[:], pidx[:, 0:1], ALU.is_lt, r=['iot', 'pidx'], w=['cst'])
        kb.ts(m_ls[:, 128:256], iot[:], pidx[:, 0:1], ALU.is_lt, r=['iot', 'pidx'], w=['cst'])
        kb.ts(triE[:], m_ab[:, 0:128], C0, ALU.mult, r=['cst'], w=['cst2'])
        kb.ts(triI[:], m_ab[:, 128:256], C0, ALU.mult, r=['cst'], w=['cst2'])
        kb.memset(triT[:], C0, w=['cst2'])
        CST = ['cst', 'cst2']
        xt = sb('xt', [128, 1024], F32)
        xn = sb('xn', [128, 1024], BF16)
        hT = sb('hT', [128, 1024], BF16)
        ss = sb('ss', [128, 4], F32)
        rkv2 = [sb('rkv%d' % i, [128, 1536], RDT) for i in range(2)]
        pprev = sb('pprev', [128, 1536], F32)
        lastrow = sb('lastrow', [1, 1536], F32)
        lin = sb('lin', [96, 3, 129], F32)
        ldf = sb('ldf', [96, 3, 128], F32)
        lsh = sb('lsh', [96, 3, 128], F32)
        lact = sb('lact', [96, 2, 128], F32)
        tmpA = sb('tmpA', [128, 512], F32)
        Etot = tmpA
        tmpB = sb('tmpB', [128, 512], F32)
        sgw = sb('sgw', [128, 512], F32)
        a_t = sb('a_t', [128, 512], F32)
        g_t2 = [sb('g_t%d' % i, [128, 512], F32) for i in range(2)]
        kk = sb('kk', [128, 512], F32)
        b_t = sb('b_t', [128, 512], F32)
        TM2 = [sb('TM%d' % i, [128, 4, 512], RDT) for i in range(2)]
        Kh2 = [sb('Kh%d' % i, [128, 512], F32) for i in range(2)]
        Bh2 = [sb('Bh%d' % i, [128, 512], F32) for i in range(2)]
        ex0 = sb('ex0', [128, 512], F32)
        ex1 = sb('ex1', [128, 512], F32)
        n2 = sb('n2', [128, 8], F32)
        FT = sb('FT', [128, 4, 512], RDT)
        gC2 = [sb('gC%d' % i, [64, 8], F32) for i in range(2)]
        tmpP = sb('tmpP', [128, 512], F32)
        Gb2 = [sb('Gb%d' % i, [128, 4, 320], RDT) for i in range(2)]
        Grk2 = [sb('Grk%d' % i, [128, 4, 128], F32) for i in range(2)]
        RTs = sb('RTs', [64, 4, 128], F32)
        GT2 = [sb('GT%d' % i, [128, 4, 256], RDT) for i in range(2)]
        Nn = [sb('Nn%d' % i, [128, 4, 128], BF16) for i in range(2)]
        NTb = [sb('NTb%d' % i, [128, 4, 128], BF16) for i in range(2)]
        NTn = [sb('NTn%d' % i, [128, 4, 128], RDT) for i in range(2)]
        P2 = [sb('P%d' % i, [128, 4, 128], RDT) for i in range(2)]
        ZT = sb('ZT', [128, 4, 192], RDT)
        MN1 = sb('MN1', [128, 4, 192], RDT)
        MN2 = sb('MN2', [128, 4, 192], RDT)
        S0T = [sb('S0T%d' % i, [128, 512], RDT) for i in range(2)]
        y = sb('y', [128, 512], F32)
        yo = sb('yo', [128, 512], BF16)
        st1 = sb('st1', [128, 8], F32)
        st2 = sb('st2', [128, 8], F32)
        st3 = sb('st3', [128, 8], F32)
        def zero_r(ap2d, n, key, rows=128):
            kb.ts(ap2d, idf[0:rows, 0:1].broadcast_to([rows, n]), 0.0, ALU.mult, r=['idf'], w=[key])
        zero_r(FT[:].rearrange("p a b -> p (a b)"), 2048, 'FT')
        kb.memset(lin[:], 0.0, w=['lin', 'lin0'])
        zero_r(MN1[:].rearrange("p a b -> p (a b)"), 768, 'MN1')
        zero_r(S0T[0][:], 512, 'S0T0')
        zero_r(S0T[1][:], 512, 'S0T1')
        fv = lambda ap: (ap.bitcast(F32) if ap.dtype == F32R else ap)
        v3h = lambda ap: ap.rearrange("p (h d) -> p h d", h=8)
        FTf = fv(FT[:])
        psT = ps[:, 6, :].bitcast(BF16)
        tiles = [(s_, i_) for s_ in range(NSEQ) for i_ in range(NT)]

        def prep(n):
            s_, i = tiles[n]
            pb = n % 2
            t0 = n * 128
            first = (i == 0)
            rkv, g_t, TM, Kh, Bh, gC = rkv2[pb], g_t2[pb], TM2[pb], Kh2[pb], Bh2[pb], gC2[pb]
            RK, GK, TK, KK, BK, CK = 'rkv%d' % pb, 'g_t%d' % pb, 'TM%d' % pb, 'Kh%d' % pb, 'Bh%d' % pb, 'gC%d' % pb
            rkvf = fv(rkv[:])
            r_ = rkvf[:, 0:512]
            k_ = rkvf[:, 512:1024]
            k_w = rkv[:, 512:1024]
            kb.dma(xt[:], x[t0:t0 + 128, :], w=['xt'])
            kb.af(xn[:], xt[:], AF.Square, accum=ss[:, 0:1], r=['xt'], w=['xn', 'ss'])
            kb.af(ss[:, 1:2], ss[:, 0:1], AF.Ln, scale=1.0 / D, bias=RMS_EPS, r=['ss'], w=['ss1'])
            kb.af(ss[:, 2:3], ss[:, 1:2], AF.Exp, scale=-0.5, r=['ss1'], w=['ss2'])
            S.act(lambda e: e.mul(out=xn[:], in_=xt[:], mul=ss[:, 2:3]), r=['xt', 'ss2'], w=['xn'])
            for cc in range(8):
                kb.tr(psT[:, cc * 128:(cc + 1) * 128], xn[:, cc * 128:(cc + 1) * 128], idb[:], r=['xn', 'idb'], w=['ps6'])
            kb.cp(hT[:], psT[:, :], r=['ps6'], w=['hT'], eng='act')
            for j in range(3):
                bk = 7 if j % 2 == 0 else 6
                for cc in range(8):
                    kb.mm(ps[:, bk, :], hT[:, cc * 128:(cc + 1) * 128], W1[:, cc, j * 512:(j + 1) * 512],
                          start=(cc == 0), stop=(cc == 7), r=['hT', 'W1'], w=['ps%d' % bk])
                kb.cp(rkv[:, j * 512:(j + 1) * 512], ps[:, bk, :], r=['ps%d' % bk], w=[RK], eng=('act' if j != 1 else 'dve'))
            for (lo, nn, col) in ((1536, 32, 0), (1568, 32, 128), (1600, 96, 256)):
                for cc in range(8):
                    kb.mm(ps[0:nn, 6, col:col + 128], W1[:, cc, lo:lo + nn], hT[:, cc * 128:(cc + 1) * 128],
                          start=(cc == 0), stop=(cc == 7), r=['hT', 'W1'], w=['ps6'])
            if first:
                kb.memset(pprev[0:1, :], 0.0, w=['pprev0'])
            else:
                kb.dma(pprev[0:1, :], lastrow[0:1, :], r=['lastrow'], w=['pprev0'])
            kb.dma(pprev[1:128, :], rkvf[0:127, :], r=[RK], w=['pprevR'])
            kb.dma(lastrow[0:1, :], rkvf[127:128, :], r=[RK], w=['lastrow'])
            kb.tt(pprev[:], pprev[:], rkvf, ALU.subtract, r=['pprev0', 'pprevR', RK], w=['pprev0', 'pprevR'])
            kb.tt(pprev[:], pprev[:], mu_b[:], ALU.mult, r=['pprev0', 'pprevR'] + CST, w=['pprev0', 'pprevR'], eng='pool')
            kb.tt(rkv[:], rkvf, pprev[:], ALU.add, r=['pprev0', 'pprevR', RK], w=[RK])
            if first:
                kb.memset(lin[:, :, 0:1], 0.0, w=['lin0'])
            else:
                kb.cp(lin[:, :, 0:1], lin[:, :, 128:129], r=['lin'], w=['lin0'])
            kb.cp(lin[0:32, 0, 1:129], ps[0:32, 6, 0:128], r=['ps6', 'lin0'], w=['lin'], eng='act')
            kb.cp(lin[0:32, 1, 1:129], ps[0:32, 6, 128:256], r=['ps6', 'lin0'], w=['lin'], eng='act')
            kb.cp(lin[0:96, 2, 1:129], ps[0:96, 6, 256:384], r=['ps6', 'lin0'], w=['lin'], eng='act')
            for l, nn in ((0, 32), (1, 32), (2, 96)):
                kb.tt(ldf[0:nn, l, :], lin[0:nn, l, 0:128], lin[0:nn, l, 1:129], ALU.subtract, r=['lin', 'lin0'], w=['ldf'])
                kb.stt(lsh[0:nn, l, :], ldf[0:nn, l, :], mucol[0:nn, l:l + 1], lin[0:nn, l, 1:129], ALU.mult, ALU.add,
                       r=['ldf', 'lin'] + CST, w=['lsh'])
            kb.af(lact[0:32, 0, :], lsh[0:32, 0, :], AF.Tanh, r=['lsh'], w=['lact'])
            kb.af(lact[0:96, 1, :], lsh[0:96, 2, :], AF.Sigmoid, r=['lsh'], w=['lact'])
            kb.mm(ps[:, 7, :], lact[0:32, 0, :], wup[0:32, :], r=['lact'] + CST, w=['ps7'])
            kb.tt(tmpA[:], ps[:, 7, :], w0_b[:], ALU.add, r=['ps7'] + CST, w=['tmpA'])
            kb.mm(ps[:, 6, :], lsh[0:32, 1, :], aup[0:32, :], r=['lsh'] + CST, w=['ps6'])
            kb.tt(tmpB[:], ps[:, 6, :], a0_b[:], ALU.add, r=['ps6'] + CST, w=['tmpB'])
            kb.mm(ps[:, 7, :], lact[0:96, 1, :], gup[0:96, :], r=['lact'] + CST, w=['ps7'])
            kb.cp(g_t[:], ps[:, 7, :], r=['ps7'], w=[GK], eng='act')
            kb.af(sgw[:], tmpA[:], AF.Sigmoid, r=['tmpA'], w=['sgw'])
            kb.af(a_t[:], tmpB[:], AF.Sigmoid, r=['tmpB'], w=['a_t'])
            kb.tt(kk[:], k_, kk_b[:], ALU.mult, r=[RK] + CST, w=['kk'])
            kb.tt(tmpA[:], kk[:], kk[:], ALU.mult, r=['kk'], w=['tmpA'], eng='pool')
            kb.red(n2[:, 0:8], v3h(tmpA[:]), ALU.add, r=['tmpA'], w=['n2'])
            kb.af(n2[:, 0:8], n2[:, 0:8], AF.Sqrt, r=['n2'], w=['n2'])
            kb.ts(n2[:, 0:8], n2[:, 0:8], 1e-12, ALU.max, r=['n2'], w=['n2'])
            kb.recip(n2[:, 0:8], n2[:, 0:8], r=['n2'], w=['n2'])
            kb.tt(v3h(kk[:]), v3h(kk[:]), bc(n2[:, 0:8], 2, [128, 8, 64]), ALU.mult, r=['kk', 'n2'], w=['kk'])
            kb.stt(tmpB[:], a_t[:], -1.0, ka_b[:], ALU.add, ALU.mult, r=['a_t'] + CST, w=['tmpB'])
            kb.stt(k_w, tmpB[:], 1.0, k_, ALU.add, ALU.mult, r=['tmpB', RK], w=[RK])
            kb.tt(b_t[:], kk[:], a_t[:], ALU.mult, r=['kk', 'a_t'], w=['b_t'], eng='pool')
            kb.mm(ps[:, 6, :], triI[:], sgw[:], r=['sgw'] + CST, w=['ps6'])
            kb.mm(ps[:, 7, :], triE[:], sgw[:], r=['sgw'] + CST, w=['ps7'])
            kb.af(ex0[:], ps[:, 6, :], AF.Exp, r=['ps6'], w=['ex0'])
            kb.tt(TM[:, 1, :], r_, ex0[:], ALU.mult, r=[RK, 'ex0'], w=[TK])
            kb.af(ex1[:], ps[:, 7, :], AF.Exp, r=['ps7'], w=['ex1'])
            kb.stt(TM[:, 0, :], kk[:], -1.0, ex1[:], ALU.mult, ALU.mult, r=['kk', 'ex1'], w=[TK])
            kb.af(ex0[:], ps[:, 6, :], AF.Exp, scale=-1.0, r=['ps6'], w=['ex0'])
            kb.mm(ps[:, 7, :], triT[:], sgw[:], r=['sgw'] + CST, w=['ps7'])
            kb.tt(TM[:, 2, :], b_t[:], ex0[:], ALU.mult, r=['b_t', 'ex0'], w=[TK])
            kb.tt(TM[:, 3, :], k_, ex0[:], ALU.mult, r=[RK, 'ex0'], w=[TK])
            kb.af(Etot[:], ps[:, 7, :], AF.Exp, r=['ps7'], w=['tmpA'])
            kb.tt(ex1[:], Etot[:], ex0[:], ALU.mult, r=['tmpA', 'ex0'], w=['ex1'], eng='pool')
            kb.tt(Kh[:], k_, ex1[:], ALU.mult, r=[RK, 'ex1'], w=[KK], eng='pool')
            kb.tt(Bh[:], b_t[:], ex1[:], ALU.mult, r=['b_t', 'ex1'], w=[BK])
            for h in range(8):
                kb.tr(ps[0:64, 6, h * 32:(h + 1) * 32], Etot[0:32, h * 64:(h + 1) * 64], idf[0:32, 0:32],
                      r=['tmpA', 'idf'], w=['ps6'])
            kb.cp(gC[0:64, 0:8], ps[0:64, 6, 0:256].rearrange("p (h c) -> p h c", c=32)[:, :, 0], r=['ps6'], w=[CK], eng='act')

        state = {'cur': 0}

        def heavy(n):
            s_, i = tiles[n]
            pb = n % 2
            t0 = n * 128
            cur = state['cur']
            rkv, g_t, TM, Kh, Bh, gC = rkv2[pb], g_t2[pb], TM2[pb], Kh2[pb], Bh2[pb], gC2[pb]
            RK, GK, TK, KK, BK, CK = 'rkv%d' % pb, 'g_t%d' % pb, 'TM%d' % pb, 'Kh%d' % pb, 'Bh%d' % pb, 'gC%d' % pb
            rkvf = fv(rkv[:])
            r_ = rkvf[:, 0:512]
            k_ = rkvf[:, 512:1024]
            v_ = rkvf[:, 1024:1536]
            TMf = fv(TM[:])
            nxt = 1 - cur
            for g in range(2):
                Gb, GT, Grk, P = Gb2[g], GT2[g], Grk2[g], P2[g]
                GBK, GTK, GRK, PK = 'Gb%d' % g, 'GT%d' % g, 'Grk%d' % g, 'P%d' % g
                for hh in range(4):
                    h = 4 * g + hh
                    for q in range(4):
                        kb.tr(ps[0:64, 5, q * 128:(q + 1) * 128], TMf[:, q, h * 64:(h + 1) * 64], idf[:],
                              r=[TK, 'idf'], w=['ps5'])
                    kb.cp(FT[0:64, hh, :], ps[0:64, 5, :], r=['ps5'], w=['FT'], eng=('act' if hh % 2 == 0 else 'dve'))
                for hh in range(4):
                    kb.mmr(ps[:, hh // 2, (hh % 2) * 256:(hh % 2) * 256 + 256], FT[:, hh, 256:384], FT[:, hh, 0:256],
                           r=['FT'], w=['ps%d' % (hh // 2)])
                    kb.mmr(ps[:, 2, hh * 128:(hh + 1) * 128], FT[:, hh, 384:512], FT[:, hh, 128:256], r=['FT'], w=['ps2'])
                    kb.mmr(ps[:, 3 + hh // 2, (hh % 2) * 256:(hh % 2) * 256 + 256], FT[:, hh, 0:128], FT[:, hh, 256:512],
                           r=['FT'], w=['ps%d' % (3 + hh // 2)])
                if g == 0:
                    kb.cp(RTs[0:64, :, :], FTf[0:64, :, 128:256], r=['FT'], w=['RTs'])
                for b2 in range(2):
                    kb.tt(Gb[:, 2 * b2:2 * b2 + 2, 0:256], ps[:, b2, :].rearrange("p (a c) -> p a c", a=2),
                          bc(m_ab[:], 1, [128, 2, 256]), ALU.mult, r=['ps%d' % b2] + CST, w=[GBK])
                    kb.tt(GT[:, 2 * b2:2 * b2 + 2, :], ps[:, 3 + b2, :].rearrange("p (a c) -> p a c", a=2),
                          bc(m_ls[:], 1, [128, 2, 256]), ALU.mult, r=['ps%d' % (3 + b2)] + CST, w=[GTK])
                kb.tt(Grk[:], ps[:, 2, :].rearrange("p (a c) -> p a c", a=4), bc(m_ab[:, 128:256], 1, [128, 4, 128]),
                      ALU.mult, r=['ps2'] + CST, w=[GRK])
                kb.cp(Gb[:, :, 256:320], Bh[:, g * 256:(g + 1) * 256].rearrange("p (a c) -> p a c", a=4), r=[BK], w=[GBK])
                kb.tt(P[:], fv(Gb[:])[:, :, 0:128], bc(idf[:], 1, [128, 4, 128]), ALU.add, r=[GBK, 'idf'], w=[PK])
            for it in range(6):
                for g in range(2):
                    Gb, GT, P = Gb2[g], GT2[g], P2[g]
                    PK = 'P%d' % g
                    bN, bT, bP = 3 * g, 3 * g + 1, 3 * g + 2
                    if it == 0:
                        Ncur = (lambda Gb_: (lambda hh: Gb_[:, hh, 0:128]))(Gb)
                        NTcur = (lambda GT_: (lambda hh: GT_[:, hh, 0:128]))(GT)
                        nk, ntk = 'Gb%d' % g, 'GT%d' % g
                    else:
                        Ncur = (lambda g_: (lambda hh: Nn[g_][:, hh, :]))(g)
                        NTcur = (lambda g_: (lambda hh: NTb[g_][:, hh, :]))(g)
                        nk, ntk = 'Nn%d' % g, 'NTb%d' % g
                    mmf = kb.mmr if it == 0 else kb.mm
                    if it < 5:
                        for hh in range(4):
                            mmf(ps[:, bN, hh * 128:(hh + 1) * 128], NTcur(hh), Ncur(hh), r=[nk, ntk], w=['ps%d' % bN])
                    for hh in range(4):
                        mmf(ps[:, bT, hh * 128:(hh + 1) * 128], Ncur(hh), NTcur(hh), r=[nk, ntk], w=['ps%d' % bT])
                    if it < 5:
                        kb.cp(Nn[g][:], ps[:, bN, :].rearrange("p (a c) -> p a c", a=4), r=['ps%d' % bN], w=['Nn%d' % g], eng='act')
                        kb.cp(NTb[g][:], ps[:, bT, :].rearrange("p (a c) -> p a c", a=4), r=['ps%d' % bT], w=['NTb%d' % g], eng='act')
                    kb.cp(NTn[g][:], ps[:, bT, :].rearrange("p (a c) -> p a c", a=4), r=['ps%d' % bT, 'NTb%d' % g], w=['NTn%d' % g], eng='act')
                    for hh in range(4):
                        kb.mmr(ps[:, bP, hh * 128:(hh + 1) * 128], NTn[g][:, hh, :], P[:, hh, :], r=['NTn%d' % g, PK], w=['ps%d' % bP])
                    kb.tt(P[:], fv(P[:]), ps[:, bP, :].rearrange("p (a c) -> p a c", a=4), ALU.add, r=[PK, 'ps%d' % bP], w=[PK])
            for g in range(2):
                Gb, GT, Grk, P = Gb2[g], GT2[g], Grk2[g], P2[g]
                GBK, GTK, GRK, PK = 'Gb%d' % g, 'GT%d' % g, 'Grk%d' % g, 'P%d' % g
                for hh in range(4):
                    h = 4 * g + hh
                    o = (hh % 2) * 192
                    kb.mmr(ps[:, hh // 2, o:o + 64], P[:, hh, :], TM[:, 0, h * 64:(h + 1) * 64], r=[PK, TK], w=['ps%d' % (hh // 2)])
                    kb.mmr(ps[:, hh // 2, o + 64:o + 192], P[:, hh, :], GT[:, hh, 128:256], r=[PK, GTK], w=['ps%d' % (hh // 2)])
                for b2 in range(2):
                    kb.cp(ZT[:, 2 * b2:2 * b2 + 2, :], ps[:, b2, 0:384].rearrange("p (a c) -> p a c", a=2),
                          r=['ps%d' % b2], w=['ZT'], eng=('act' if b2 == 0 else 'dve'))
                for hh in range(4):
                    o = (hh % 2) * 192
                    kb.mmr(ps[0:64, 3 + hh // 2, o:o + 192], ZT[:, hh, 0:64], Gb[:, hh, 128:320], r=['ZT', GBK], w=['ps%d' % (3 + hh // 2)])
                for hh in range(4):
                    o = (hh % 2) * 192
                    kb.mmr(ps[:, hh // 2, o:o + 192], ZT[:, hh, 64:192], Gb[:, hh, 128:320], r=['ZT', GBK], w=['ps%d' % (hh // 2)])
                for b2 in range(2):
                    pv1 = ps[0:64, 3 + b2, 0:384].rearrange("p (a c) -> p a c", a=2)
                    rsrc = RTs[0:64, 2 * b2:2 * b2 + 2, :] if g == 0 else FTf[0:64, 2 * b2:2 * b2 + 2, 128:256]
                    kb.tt(MN1[0:64, 2 * b2:2 * b2 + 2, 0:128], pv1[:, :, 0:128], rsrc, ALU.add,
                          r=['ps%d' % (3 + b2), 'FT', 'RTs'], w=['MN1'])
                    for a2 in range(2):
                        hh = 2 * b2 + a2
                        h = 4 * g + hh
                        kb.stt(MN1[0:64, hh, 128:192], idf[0:64, 0:64], gC[0:64, h:h + 1], ps[0:64, 3 + b2, a2 * 192 + 128:a2 * 192 + 192],
                               ALU.mult, ALU.add, r=['ps%d' % (3 + b2), CK, 'idf'], w=['MN1'])
                    pv2 = ps[:, b2, 0:384].rearrange("p (a c) -> p a c", a=2)
                    kb.tt(MN2[:, 2 * b2:2 * b2 + 2, 0:128], pv2[:, :, 0:128], Grk[:, 2 * b2:2 * b2 + 2, :], ALU.add,
                          r=['ps%d' % b2, GRK], w=['MN2'])
                    h0 = 4 * g + 2 * b2
                    kb.tt(MN2[:, 2 * b2:2 * b2 + 2, 128:192], pv2[:, :, 128:192],
                          Kh[:, h0 * 64:(h0 + 2) * 64].rearrange("p (a c) -> p a c", a=2), ALU.add,
                          r=['ps%d' % b2, KK], w=['MN2'])
                sk = 'S0T%d' % cur
                for hh in range(4):
                    h = 4 * g + hh
                    hs = slice(h * 64, (h + 1) * 64)
                    ys_ = slice(256 + hh * 64, 256 + (hh + 1) * 64)
                    ss_ = slice(hh * 64, (hh + 1) * 64)
                    kb.mmr(ps[:, 2, ys_], MN1[:, hh, 0:128], S0T[cur][:, hs], start=True, stop=False, r=['MN1', sk], w=['ps2'])
                    kb.mmr(ps[:, 2, ys_], MN2[:, hh, 0:128], rkv[:, 1024 + h * 64:1024 + (h + 1) * 64], start=False, stop=True,
                           r=['MN2', RK], w=['ps2'])
                    kb.mmr(ps[0:64, 2, ss_], MN1[:, hh, 128:192], S0T[cur][:, hs], start=True, stop=False, r=['MN1', sk], w=['ps2'])
                    kb.mmr(ps[0:64, 2, ss_], MN2[:, hh, 128:192], rkv[:, 1024 + h * 64:1024 + (h + 1) * 64], start=False, stop=True,
                           r=['MN2', RK], w=['ps2'])
                if i == NT - 1:
                    zero_r(S0T[nxt][0:64, g * 256:(g + 1) * 256], 256, 'S0T%d' % nxt, rows=64)
                else:
                    kb.cp(S0T[nxt][0:64, g * 256:(g + 1) * 256], ps[0:64, 2, 0:256], r=['ps2'], w=['S0T%d' % nxt], eng='act')
                kb.cp(y[:, g * 256:(g + 1) * 256], ps[:, 2, 256:512], r=['ps2'], w=['y'], eng='act')
            state['cur'] = nxt
            kb.red(st1[:, 0:8], v3h(y[:]), ALU.add, r=['y'], w=['st1'])
            kb.tt(tmpP[:], y[:], y[:], ALU.mult, r=['y'], w=['tmpP'], eng='pool')
            kb.red(st2[:, 0:8], v3h(tmpP[:]), ALU.add, r=['tmpP'], w=['st2'])
            kb.ts(st1[:, 0:8], st1[:, 0:8], 1.0 / 64, ALU.mult, r=['st1'], w=['st1'])
            kb.tt(st3[:, 0:8], st1[:, 0:8], st1[:, 0:8], ALU.mult, r=['st1'], w=['st3'])
            kb.stt(st2[:, 0:8], st2[:, 0:8], 1.0 / 64, st3[:, 0:8], ALU.mult, ALU.subtract, r=['st2', 'st3'], w=['st2'])
            kb.af(st2[:, 0:8], st2[:, 0:8], AF.Sqrt, bias=LNX_EPS, r=['st2'], w=['st2'])
            kb.recip(st2[:, 0:8], st2[:, 0:8], r=['st2'], w=['st2'])
            kb.tt(v3h(y[:]), v3h(y[:]), bc(st1[:, 0:8], 2, [128, 8, 64]), ALU.subtract, r=['y', 'st1'], w=['y'])
            kb.tt(v3h(y[:]), v3h(y[:]), bc(st2[:, 0:8], 2, [128, 8, 64]), ALU.mult, r=['y', 'st2'], w=['y'])
            kb.tt(y[:], y[:], lw_b[:], ALU.mult, r=['y'] + CST, w=['y'])
            kb.tt(y[:], y[:], lb_b[:], ALU.add, r=['y'] + CST, w=['y'])
            kb.tt(tmpP[:], r_, k_, ALU.mult, r=[RK], w=['tmpP'], eng='pool')
            kb.tt(tmpP[:], tmpP[:], rk_b[:], ALU.mult, r=['tmpP'] + CST, w=['tmpP'], eng='pool')
            kb.red(st3[:, 0:8], v3h(tmpP[:]), ALU.add, r=['tmpP'], w=['st3'])
            kb.tt(v3h(tmpP[:]), v3h(v_), bc(st3[:, 0:8], 2, [128, 8, 64]), ALU.mult, r=[RK, 'st3'], w=['tmpP'])
            kb.tt(y[:], y[:], tmpP[:], ALU.add, r=['y', 'tmpP'], w=['y'])
            kb.tt(yo[:], y[:], g_t[:], ALU.mult, r=['y', GK], w=['yo'])
            kb.dma(yr[t0:t0 + 128, :], yo[:], r=['yo'], w=['yr'])

        def capture(fn, n):
            saved = S.ops
            S.ops = []
            fn(n)
            got = S.ops
            S.ops = saved
            return got

        prep(0)
        for n in range(len(tiles)):
            hv = capture(heavy, n)
            pr = capture(prep, n + 1) if n + 1 < len(tiles) else []
            ratio = (len(hv) // max(1, len(pr))) if pr else 0
            pi = 0
            for k, op_ in enumerate(hv):
                S.ops.append(op_)
                if pr and ratio > 0 and (k + 1) % ratio == 0 and pi < len(pr):
                    S.ops.append(pr[pi])
                    pi += 1
            S.ops.extend(pr[pi:])
        S.flush()


PARAM_SHAPES = {
    "ln1_g": [1, 1024], "w_in": [1, 1024, 2464], "b_attn": [1, 768], "mu_shift": [1, 1696],
    "w0": [1, 512], "w_up": [1, 32, 512], "a0": [1, 512], "a_up": [1, 32, 512], "g_up": [1, 96, 512],
    "k_k": [1, 512], "k_a": [1, 512], "r_k": [1, 8, 64], "lnx_w": [1, 512], "lnx_b": [1, 512],
    "attn_sinks": [1, 8], "attn_norm_g": [1, 512], "w_out": [1, 1024, 1024], "ln2_g": [1, 1024],
    "peer_wq": [1, 1024, 2048], "peer_subkeys": [1, 8, 2, 128, 128], "peer_u": [1, 16384, 1024],
    "peer_v": [1, 16384, 1024], "lnf_g": [1024],
}


def build_program(NSEQ, SEQ, phases=('prep', 'rwkv', 'attn', 'peer'), dbg=False, x1_in=False):
    nc = bass.Bass("TRN2", target_bir_lowering=False)
    NTOK = NSEQ * SEQ
    x = nc.dram_tensor("x", [NTOK, D], F32, kind="ExternalInput").ap()
    A = {k: nc.dram_tensor(k, s, F32, kind="ExternalInput").ap() for k, s in PARAM_SHAPES.items()}
    out = nc.dram_tensor("out", [NTOK, D], F32, kind="ExternalOutput").ap()
    sk = "ExternalOutput" if dbg else "Internal"
    yr = nc.dram_tensor("yr", [NTOK, 512], BF16, kind=sk).ap()
    x1 = nc.dram_tensor("x1", [NTOK, D], F32, kind=("ExternalInput" if x1_in else sk)).ap()
    uS = nc.dram_tensor("uS", [32, 128, 4 * 8 * 128], BF16, kind="Internal").ap()
    vS = nc.dram_tensor("vS", [16384, D], BF16, kind="Internal").ap()
    with ExitStack() as es:
        S = Sched(nc, es)
        kb = KB(nc, S)
        c = setup_consts(nc, kb, es)
        fused_prep = ('prep' in phases) and ('attn' in phases)
        if 'prep' in phases and not fused_prep:
            phase_prep(nc, kb, c, A, uS, vS)
        if 'rwkv' in phases:
            phase_rwkv(nc, kb, c, NSEQ, SEQ, x, A, yr)
        if 'attn' in phases:
            phase_attn(nc, kb, c, NSEQ, SEQ, x, A, yr, x1, prep=((uS, vS) if fused_prep else None))
        if 'peer' in phases:
            phase_peer(nc, kb, c, NTOK, x1, A, uS, vS, out)
        print("ops emitted:", S.n_emitted)
    return nc


def phase_attn(nc, kb, c, NSEQ, SEQ, x, A, yr, x1, prep=None):
    S = kb.S
    NT = SEQ // 128
    idf, idb, iot, pidx = c['idf'], c['idb'], c['iot'], c['pidx']
    with ExitStack() as es:
        sb = lambda name, shape, dt: es.enter_context(nc.sbuf_tensor(name, shape, dt))
        ps = es.enter_context(nc.psum_tensor('ps_b', [128, 8, 512], F32))
        Wat = sb('Wat', [128, 8, 768], BF16)
        Wout = sb('Wout', [128, 8, 1024], BF16)
        stage = [sb('bstage%d' % i, [128, 1024], F32) for i in range(2)]
        g1c = sb('g1cb', [128, 8], F32)
        gan = sb('gan', [128, 4], F32)
        bq8 = sb('bq8', [64, 8], F32)
        bkc = sb('bkc', [64, 2], F32)
        bv_b = sb('bv_b', [128, 128], F32)
        snk = sb('snk', [128, 8], F32)
        mPC = sb('mPC', [128, 256], F32)
        mF = sb('mF', [128, 256], F32)
        kb.dma(g1c[:, :], A['ln1_g'][0].rearrange("(c p) -> p c", p=128), w=['cst'], slow=True)
        kb.dma(gan[:, :], A['attn_norm_g'][0].rearrange("(c p) -> p c", p=128), w=['cst'], slow=True)
        kb.dma(bq8[:, :], A['b_attn'][0, 0:512].rearrange("(h d) -> d h", d=64), w=['cst'], slow=True)
        kb.dma(bkc[:, :], A['b_attn'][0, 512:640].rearrange("(h d) -> d h", d=64), w=['cst'], slow=True)
        kb.dma(bv_b[:], A['b_attn'][0:1, 640:768].partition_broadcast(128), w=['cst'])
        kb.dma(snk[:], A['attn_sinks'][0:1, :].partition_broadcast(128), w=['cst'])
        kb.ts(bq8[:], bq8[:], 0.125, ALU.mult, r=['cst'], w=['cst2'])
        for cc in range(8):
            st = stage[cc % 2]
            sk_ = 'bstage%d' % (cc % 2)
            kb.dma(st[:, 0:768], A['w_in'][0, cc * 128:(cc + 1) * 128, 1696:2464], w=[sk_])
            (lambda st_, cc_, sk__: S.act(lambda e: e.mul(out=Wat[:, cc_, :], in_=st_[:, 0:768], mul=g1c[:, cc_:cc_ + 1]),
                                          r=[sk__, 'cst'], w=['Wat']))(st, cc, sk_)
        for cc in range(8):
            st = stage[cc % 2]
            sk_ = 'bstage%d' % (cc % 2)
            kb.dma(st[:, :], A['w_out'][0, cc * 128:(cc + 1) * 128, :], w=[sk_])
            if cc < 4:
                kb.cp(Wout[:, cc, :], st[:, :], r=[sk_], w=['Wout'], eng='act')
            else:
                (lambda st_, cc_, sk__: S.act(lambda e: e.mul(out=Wout[:, cc_, :], in_=st_[:, :], mul=gan[:, cc_ - 4:cc_ - 3]),
                                              r=[sk__, 'cst'], w=['Wout']))(st, cc, sk_)
        kb.ts(mPC[:, 0:128], iot[:], pidx[:, 0:1], ALU.is_gt, r=['iot', 'pidx'], w=['cst3'])
        kb.ts(mPC[:, 128:256], iot[:], pidx[:, 0:1], ALU.is_le, r=['iot', 'pidx'], w=['cst3'])
        kb.ts(mPC[:], mPC[:], 1e30, ALU.mult, -1e30, ALU.add, r=['cst3'], w=['cst4'])
        kb.memset(mF[:, 0:128], NEG, w=['cst5'])
        kb.cp(mF[:, 128:256], mPC[:, 128:256], r=['cst4'], w=['cst5'])
        CST = ['cst', 'cst2', 'cst4', 'cst5']
        xt = sb('bxt', [128, 1024], F32)
        junk = sb('bjunk', [128, 1024], BF16)
        xn = sb('bxn', [128, 1024], BF16)
        hT = sb('bhT', [128, 1024], BF16)
        ss = sb('bss', [128, 8], F32)
        qT = sb('qT', [128, 8, 128], BF16)
        kT = [sb('kT%d' % i, [128, 2, 128], BF16) for i in range(2)]
        kb.memset(qT[:], 0.0, w=['qT'])
        vb = [sb('vb%d' % i, [128, 128], BF16) for i in range(2)]
        Sm = sb('Sm', [128, 8, 256], F32)
        E = sb('E', [128, 8, 256], BF16)
        ET = sb('ET', [128, 2048], BF16)
        mx = sb('mx', [128, 8], F32)
        nmx = sb('nmx', [128, 8], F32)
        rs = sb('rs', [128, 8], F32)
        esk = sb('esk', [128, 8], F32)
        o = sb('o', [128, 512], F32)
        ycat = sb('ycat', [128, 1024], BF16)
        ycT = sb('ycT', [128, 1024], BF16)
        for i in range(2):
            kb.memset(kT[i][:], 0.0, w=['kT%d' % i])
            kb.memset(vb[i][:], 0.0, w=['vb%d' % i])
        psT6 = ps[:, 6, :].bitcast(BF16)
        prep_emit = make_prep(nc, kb, c, A, prep[0], prep[1], es, ps, [7]) if prep is not None else None
        n_tiles_total = NSEQ * NT
        prep_units = [(jb, part) for jb in range(32) for part in range(2)] if prep is not None else []
        prep_done = 0
        par = 0
        STOP = 99
        for s in range(NSEQ):
            for i in range(NT):
                if STOP <= 1:
                    continue
                tile_idx = s * NT + i
                tile_start = len(S.ops)
                t0 = (s * NT + i) * 128
                first = (i == 0)
                kc, kp = 'kT%d' % par, 'kT%d' % (1 - par)
                vc, vp = 'vb%d' % par, 'vb%d' % (1 - par)
                kb.dma(xt[:], x[t0:t0 + 128, :], w=['xt'])
                kb.dma(ycat[:, 0:512], yr[t0:t0 + 128, :], r=['yr'], w=['ycatA'])
                kb.af(junk[:], xt[:], AF.Square, accum=ss[:, 0:1], r=['xt'], w=['junk', 'ss'])
                kb.af(ss[:, 1:2], ss[:, 0:1], AF.Sqrt, scale=1.0 / D, bias=RMS_EPS, r=['ss'], w=['ss1'])
                kb.recip(ss[:, 2:3], ss[:, 1:2], r=['ss1'], w=['ss2'])
                S.act(lambda e: e.mul(out=xn[:], in_=xt[:], mul=ss[:, 2:3]), r=['xt', 'ss2'], w=['xn'])
                for cc in range(8):
                    kb.tr(psT6[:, cc * 128:(cc + 1) * 128], xn[:, cc * 128:(cc + 1) * 128], idb[:], r=['xn', 'idb'], w=['ps6'])
                kb.cp(hT[:], psT6[:, :], r=['ps6'], w=['hT'])
                for h in range(8):
                    for cc in range(8):
                        kb.mm(ps[0:64, h // 4, (h % 4) * 128:(h % 4 + 1) * 128], Wat[:, cc, h * 64:(h + 1) * 64],
                              hT[:, cc * 128:(cc + 1) * 128], start=(cc == 0), stop=(cc == 7), r=['hT', 'Wat'], w=['ps%d' % (h // 4)])
                for kv in range(2):
                    for cc in range(8):
                        kb.mm(ps[0:64, 2, kv * 128:(kv + 1) * 128], Wat[:, cc, 512 + kv * 64:512 + (kv + 1) * 64],
                              hT[:, cc * 128:(cc + 1) * 128], start=(cc == 0), stop=(cc == 7), r=['hT', 'Wat'], w=['ps2'])
                for cc in range(8):
                    kb.mm(ps[:, 2, 256:384], hT[:, cc * 128:(cc + 1) * 128], Wat[:, cc, 640:768],
                          start=(cc == 0), stop=(cc == 7), r=['hT', 'Wat'], w=['ps2'])
                for b2 in range(2):
                    kb.stt(qT[0:64, 4 * b2:4 * b2 + 4, :], ps[0:64, b2, :].rearrange("p (a c) -> p a c", a=4), 0.125,
                           bc(bq8[0:64, 4 * b2:4 * b2 + 4], 2, [64, 4, 128]), ALU.mult, ALU.add, r=['ps%d' % b2] + CST, w=['qT'])
                kb.tt(kT[par][0:64, :, :], ps[0:64, 2, 0:256].rearrange("p (a c) -> p a c", a=2),
                      bc(bkc[0:64, 0:2], 2, [64, 2, 128]), ALU.add, r=['ps2'] + CST, w=[kc])
                kb.tt(vb[par][:], ps[:, 2, 256:384], bv_b[:], ALU.add, r=['ps2'] + CST, w=[vc])
                if STOP <= 2:
                    continue
                for h in range(8):
                    kv = h // 4
                    bk = 2 + h // 2
                    o_ = (h % 2) * 256
                    kprev = kT[par] if first else kT[1 - par]
                    kb.mm(ps[:, bk, o_:o_ + 128], qT[:, h, :], kprev[:, kv, :], r=['qT', kc, kp], w=['ps%d' % bk])
                    kb.mm(ps[:, bk, o_ + 128:o_ + 256], qT[:, h, :], kT[par][:, kv, :], r=['qT', kc], w=['ps%d' % bk])
                msk = mF if first else mPC
                for b2 in range(4):
                    kb.tt(Sm[:, 2 * b2:2 * b2 + 2, :], ps[:, 2 + b2, :].rearrange("p (a c) -> p a c", a=2),
                          bc(msk[:], 1, [128, 2, 256]), ALU.add, r=['ps%d' % (2 + b2)] + CST, w=['Sm'])
                if STOP <= 3:
                    continue
                kb.red(mx[:, 0:8], Sm[:], ALU.max, r=['Sm'], w=['mx'])
                kb.tt(mx[:, 0:8], mx[:, 0:8], snk[:, 0:8], ALU.max, r=['mx'] + CST, w=['mx'])
                kb.ts(nmx[:, 0:8], mx[:, 0:8], -1.0, ALU.mult, r=['mx'], w=['nmx'])
                for h in range(8):
                    kb.af(E[:, h, :], Sm[:, h, :], AF.Exp, bias=nmx[:, h:h + 1], accum=rs[:, h:h + 1], r=['Sm', 'nmx'], w=['E', 'rs'])
                kb.tt(esk[:, 0:8], snk[:, 0:8], mx[:, 0:8], ALU.subtract, r=['mx'] + CST, w=['esk'])
                kb.af(esk[:, 0:8], esk[:, 0:8], AF.Exp, r=['esk'], w=['esk'])
                kb.tt(rs[:, 0:8], rs[:, 0:8], esk[:, 0:8], ALU.add, r=['rs', 'esk'], w=['rs'])
                kb.recip(rs[:, 0:8], rs[:, 0:8], r=['rs'], w=['rs'])
                if STOP <= 4:
                    continue
                for hq in range(2):
                    for h in range(4 * hq, 4 * hq + 4):
                        for hf in range(2):
                            blk = h * 2 + hf
                            kb.tr(psT6[:, (blk % 8) * 128:(blk % 8 + 1) * 128], E[:, h, hf * 128:(hf + 1) * 128], idb[:],
                                  r=['E', 'idb'], w=['ps6'])
                    kb.cp(ET[:, hq * 1024:(hq + 1) * 1024], psT6[:, :], r=['ps6'], w=['ET'], eng=('act' if hq == 0 else 'dve'))
                for h in range(8):
                    kv = h // 4
                    kb.mm(ps[:, 0, h * 64:(h + 1) * 64], ET[:, (2 * h) * 128:(2 * h + 1) * 128], vb[1 - par][:, kv * 64:(kv + 1) * 64],
                          start=True, stop=False, r=['ET', vp], w=['ps0'])
                    kb.mm(ps[:, 0, h * 64:(h + 1) * 64], ET[:, (2 * h + 1) * 128:(2 * h + 2) * 128], vb[par][:, kv * 64:(kv + 1) * 64],
                          start=False, stop=True, r=['ET', vc], w=['ps0'])
                kb.tt(o[:].rearrange("p (h d) -> p h d", h=8), ps[:, 0, :].rearrange("p (h d) -> p h d", h=8),
                      bc(rs[:, 0:8], 2, [128, 8, 64]), ALU.mult, r=['ps0', 'rs'], w=['o'])
                kb.af(junk[:, 0:512], o[:], AF.Square, accum=ss[:, 3:4], r=['o'], w=['junk', 'ss3'])
                kb.af(ss[:, 4:5], ss[:, 3:4], AF.Sqrt, scale=1.0 / 512, bias=RMS_EPS, r=['ss3'], w=['ss4'])
                kb.recip(ss[:, 5:6], ss[:, 4:5], r=['ss4'], w=['ss5'])
                S.act(lambda e: e.mul(out=ycat[:, 512:1024], in_=o[:], mul=ss[:, 5:6]), r=['o', 'ss5'], w=['ycatB'])
                if STOP <= 5:
                    continue
                for cc in range(8):
                    kb.tr(psT6[:, cc * 128:(cc + 1) * 128], ycat[:, cc * 128:(cc + 1) * 128], idb[:],
                          r=['ycatA', 'ycatB', 'idb'], w=['ps6'])
                kb.cp(ycT[:], psT6[:, :], r=['ps6'], w=['ycT'])
                for n2 in range(2):
                    for cc in range(8):
                        kb.mm(ps[:, 2 + n2, :], ycT[:, cc * 128:(cc + 1) * 128], Wout[:, cc, n2 * 512:(n2 + 1) * 512],
                              start=(cc == 0), stop=(cc == 7), r=['ycT', 'Wout'], w=['ps%d' % (2 + n2)])
                kb.tt(xt[:, 0:512], xt[:, 0:512], ps[:, 2, :], ALU.add, r=['xt', 'ps2'], w=['xt'])
                kb.tt(xt[:, 512:1024], xt[:, 512:1024], ps[:, 3, :], ALU.add, r=['xt', 'ps3'], w=['xt'])
                kb.dma(x1[t0:t0 + 128, :], xt[:], r=['xt'], w=['x1'])
                par = 1 - par
                if prep_emit is not None:
                    want = ((tile_idx + 1) * len(prep_units)) // n_tiles_total
                    tile_ops = S.ops[tile_start:]
                    del S.ops[tile_start:]
                    saved = S.ops
                    S.ops = []
                    while prep_done < want:
                        prep_emit(*prep_units[prep_done])
                        prep_done += 1
                    pops = S.ops
                    S.ops = saved
                    ratio = max(1, len(tile_ops) // max(1, len(pops))) if pops else 0
                    pi = 0
                    for k_, op_ in enumerate(tile_ops):
                        S.ops.append(op_)
                        if pops and (k_ + 1) % ratio == 0 and pi < len(pops):
                            S.ops.append(pops[pi])
                            pi += 1
                    S.ops.extend(pops[pi:])
        if prep_emit is not None:
            while prep_done < len(prep_units):
                prep_emit(*prep_units[prep_done])
                prep_done += 1
        S.flush()


def make_prep(nc, kb, c, A, uS, vS, es, ps, banks):
    idf = c['idf']
    uv = A['peer_u'][0].rearrange("(i j) d -> i j d", j=128)
    vv = A['peer_v'][0].rearrange("(i j) d -> i j d", j=128)
    vSv = vS.rearrange("(i j) d -> i j d", j=128)
    sb = lambda name, shape, dt: es.enter_context(nc.sbuf_tensor(name, shape, dt))
    Uld = [sb('Uld%d' % i, [128, 4, 1024], F32) for i in range(2)]
    Vld = [sb('Vld%d' % i, [128, 4, 1024], F32) for i in range(2)]
    UT = [sb('UT%d' % i, [128, 4096], BF16) for i in range(2)]
    Vb = [sb('Vb%d' % i, [128, 4, 1024], BF16) for i in range(2)]
    nb = len(banks)

    def emit(jb, part):
        p = jb % 2
        if part == 0:
            kb.dma(Uld[p][:], uv[:, jb * 4:(jb + 1) * 4, :], w=['Uld%d' % p])
            kb.dma(Vld[p][:], vv[:, jb * 4:(jb + 1) * 4, :], w=['Vld%d' % p])
            for jj in range(4):
                for half in range(2):
                    bk = banks[(jj * 2 + half) % nb]
                    for q in range(4):
                        dc = half * 4 + q
                        kb.tr(ps[:, bk, q * 128:(q + 1) * 128], Uld[p][:, jj, dc * 128:(dc + 1) * 128], idf[:],
                              r=['Uld%d' % p, 'idf'], w=['ps%d' % bk])
                    o_ = (jj * 8 + half * 4) * 128
                    kb.cp(UT[p][:, o_:o_ + 512], ps[:, bk, :], r=['ps%d' % bk], w=['UT%d' % p],
                          eng=('act' if (nb > 1 and (jj + half) % 2 == 0) else 'dve') if nb > 1 else 'act')
            kb.dma(uS[jb], UT[p][:], r=['UT%d' % p], w=['uS'])
        else:
            kb.cp(Vb[p][:, 0:2, :], Vld[p][:, 0:2, :], r=['Vld%d' % p], w=['Vb%d' % p], eng='pool')
            kb.cp(Vb[p][:, 2:3, :], Vld[p][:, 2:3, :], r=['Vld%d' % p], w=['Vb%d' % p], eng='pool')
            kb.cp(Vb[p][:, 3:4, :], Vld[p][:, 3:4, :], r=['Vld%d' % p], w=['Vb%d' % p], eng='pool')
            kb.dma(vSv[:, jb * 4:(jb + 1) * 4, :], Vb[p][:], r=['Vb%d' % p], w=['vS'])
    return emit


def phase_prep(nc, kb, c, A, uS, vS):
    with ExitStack() as es:
        ps = es.enter_context(nc.psum_tensor('ps_p', [128, 8, 512], F32))
        emit = make_prep(nc, kb, c, A, uS, vS, es, ps, list(range(8)))
        for jb in range(32):
            emit(jb, 0)
            emit(jb, 1)
        kb.S.flush()


def phase_peer(nc, kb, c, NTOK, x1, A, uS, vS, out, TG=2):
    S = kb.S
    idf, idb, iot, pidx = c['idf'], c['idb'], c['iot'], c['pidx']
    NG = NTOK // (128 * TG)
    NTK = 128 * TG
    vSv = vS.rearrange("(i j) d -> i j d", j=128)
    with ExitStack() as es:
        sb = lambda name, shape, dt: es.enter_context(nc.sbuf_tensor(name, shape, dt))
        ps = es.enter_context(nc.psum_tensor('ps_c', [128, 8, 512], F32))
        wq = sb('wq', [128, 8, 2048], BF16)
        skT = sb('skT', [128, 16, 128], BF16)
        lnf_b = sb('lnf_b', [128, 1024], F32)
        g2c = sb('g2c', [128, 8], F32)
        iob = sb('iob', [128, 128], BF16)
        GG = sb('GG', [128, NTK, 128], BF16)
        h2T = [sb('h2T%d' % i, [128, 8, NTK], BF16) for i in range(2)]
        xt = [[sb('cxt%d_%d' % (i, t), [128, 1024], F32) for t in range(TG)] for i in range(2)]
        junk = sb('cjunk', [128, 1024], BF16)
        junk2 = sb('cjunk2', [128, 1024], BF16)
        xn = sb('cxn', [128, 1024], BF16)
        ss = sb('css', [128, 8], F32)
        ss2 = sb('css2', [128, 8], F32)
        NBUF = 3
        ut = [sb('ut%d' % i, [128, 2, 8, 128], BF16) for i in range(NBUF)]
        vt = [sb('vt%d' % i, [128, 2, 1024], BF16) for i in range(NBUF)]
        qTb = sb('qTb', [128, 16, 128], BF16)
        sc = sb('sc', [128, 16, 128], F32)
        scr = sb('scr', [128, 256], F32)
        tv = sb('tv', [128, 16, 16], F32)
        ti = sb('ti', [128, 16, 16], U32)
        tif = [sb('tif%d' % i, [128, 16, 16], F32) for i in range(TG)]
        cand = sb('cand', [128, 8, 256], F32)
        sel = cand[:].rearrange("p h (a b) -> p h a b", a=16)
        cv = [sb('cv%d' % i, [128, 8, 16], F32) for i in range(TG)]
        cpi = [sb('cpi%d' % i, [128, 8, 16], U32) for i in range(TG)]
        cu1 = sb('cu1', [128, 8, 16], U32)
        akf = sb('akf', [128, 8, 16], F32)
        bkf = sb('bkf', [128, 8, 16], F32)
        ge = sb('ge', [128, 8, 16], F32)
        gz = sb('gz', [128, 8], F32)
        idxi = sb('idxi', [128, 128], F32)
        idxj = sb('idxj', [128, 128], F32)
        gate = sb('gate', [128, 128], F32)
        slotT = sb('slotT', [128, TG, 3, 128], BF16)
        slotF = sb('slotF', [128, TG, 2, 128], F32)
        TB = 8
        Aoh = [sb('Aoh%d' % i, [128, TB, 128], BF16) for i in range(2)]
        Boh = [sb('Boh%d' % i, [128, TB, 128], BF16) for i in range(2)]
        ga = [sb('ga%d' % i, [128, NTK], BF16) for i in range(2)]
        coef = [sb('coef%d' % i, [128, NTK], BF16) for i in range(2)]
        fin = sb('fin', [128, 1024], F32)
        kb.dma(g2c[:, :], A['ln2_g'][0].rearrange("(c p) -> p c", p=128), w=['cst'], slow=True)
        kb.dma(lnf_b[:], A['lnf_g'].rearrange("(o d) -> o d", o=1).partition_broadcast(128), w=['cst'])
        kb.cp(iob[:], iot[:], r=['iot'], w=['cst'])
        stg = cand[:].rearrange("p a b -> p (a b)")
        for cc in range(8):
            kb.dma(stg, A['peer_wq'][0, cc * 128:(cc + 1) * 128, :], w=['cand'])
            kb.cp(wq[:, cc, :], stg, r=['cand'], w=['wq'], eng=('act' if cc % 2 == 0 else 'dve'))
        for grp in range(16):
            h, cx = grp // 2, grp % 2
            kb.dma(sc[:, grp, :], A['peer_subkeys'][0, h, cx], w=['sc'])
        for grp in range(16):
            kb.tr(ps[:, 4 + (grp // 4) % 2, (grp % 4) * 128:(grp % 4 + 1) * 128], sc[:, grp, :], idf[:], r=['sc', 'idf'],
                  w=['ps%d' % (4 + (grp // 4) % 2)])
            if grp % 4 == 3:
                g0 = grp - 3
                bk = 4 + (grp // 4) % 2
                kb.cp(skT[:, g0:g0 + 4, :], ps[:, bk, :].rearrange("p (a c) -> p a c", a=4), r=['ps%d' % bk], w=['skT'])
        CST = ['cst', 'wq', 'skT']
        psT = ps[:, 7, :].bitcast(BF16)

        def frontA(g):
            gp = g % 2
            hk = 'h2T%d' % gp
            for tl in range(TG):
                t0 = (g * TG + tl) * 128
                xk = 'xt%d_%d' % (gp, tl)
                xtt = xt[gp][tl]
                kb.dma(xtt[:], x1[t0:t0 + 128, :], r=['x1'], w=[xk])
                kb.af(junk[:], xtt[:], AF.Square, accum=ss[:, 0:1], r=[xk], w=['junk', 'ss'])
                kb.af(ss[:, 1:2], ss[:, 0:1], AF.Ln, scale=1.0 / D, bias=RMS_EPS, r=['ss'], w=['ss1'])
                kb.af(ss[:, 2:3], ss[:, 1:2], AF.Exp, scale=-0.5, r=['ss1'], w=['ss2'])
                (lambda xtt_, xk_: S.act(lambda e: e.mul(out=xn[:], in_=xtt_[:], mul=ss[:, 2:3]), r=[xk_, 'ss2'], w=['xn']))(xtt, xk)
                for cc in range(8):
                    kb.tr(psT[:, cc * 128:(cc + 1) * 128], xn[:, cc * 128:(cc + 1) * 128], idb[:], r=['xn', 'idb'], w=['ps7'])
                for cc in range(8):
                    (lambda cc_, tl_, gp_: S.act(lambda e: e.mul(out=h2T[gp_][:, cc_, tl_ * 128:(tl_ + 1) * 128],
                                                                in_=psT[:, cc_ * 128:(cc_ + 1) * 128], mul=g2c[:, cc_:cc_ + 1]),
                                                 r=['ps7'] + CST, w=[hk]))(cc, tl, gp)
                for qb in range(4):
                    for gi in range(4):
                        grp = qb * 4 + gi
                        for cc in range(8):
                            kb.mm(ps[:, 6, gi * 128:(gi + 1) * 128], wq[:, cc, grp * 128:(grp + 1) * 128],
                                  h2T[gp][:, cc, tl * 128:(tl + 1) * 128], start=(cc == 0), stop=(cc == 7), r=['wq', hk], w=['ps6'])
                    kb.cp(qTb[:, qb * 4:qb * 4 + 4, :], ps[:, 6, :].rearrange("p (a c) -> p a c", a=4), r=['ps6'], w=['qTb'],
                          eng='act')
                for qb in range(4):
                    bk = 6 + qb % 2
                    for gi in range(4):
                        grp = qb * 4 + gi
                        kb.mm(ps[:, bk, gi * 128:(gi + 1) * 128], qTb[:, grp, :], skT[:, grp, :], r=['qTb', 'skT'], w=['ps%d' % bk])
                    kb.cp(sc[:, qb * 4:qb * 4 + 4, :], ps[:, bk, :].rearrange("p (a c) -> p a c", a=4), r=['ps%d' % bk], w=['sc'],
                          eng='act')
                for grp in range(16):
                    S.dve(lambda e, grp=grp: e.max(out=tv[:, grp, 0:8], in_=sc[:, grp, :]), r=['sc'], w=['tv'])
                    S.dve(lambda e, grp=grp: e.max_index(out=ti[:, grp, 0:8], in_max=tv[:, grp, 0:8], in_values=sc[:, grp, :]),
                          r=['sc', 'tv'], w=['ti'])
                    S.dve(lambda e, grp=grp: e.match_replace(out=scr[:, 0:128], in_to_replace=tv[:, grp, 0:8], in_values=sc[:, grp, :],
                                                             imm_value=NEG), r=['sc', 'tv'], w=['scr'])
                    S.dve(lambda e, grp=grp: e.max(out=tv[:, grp, 8:16], in_=scr[:, 0:128]), r=['scr'], w=['tv'])
                    S.dve(lambda e, grp=grp: e.max_index(out=ti[:, grp, 8:16], in_max=tv[:, grp, 8:16], in_values=scr[:, 0:128]),
                          r=['scr', 'tv'], w=['ti'])
                kb.cp(tif[tl][:], ti[:], r=['ti'], w=['tif%d' % tl])
                tvv = tv[:].rearrange("p (h c) k -> p h c k", c=2)
                kb.tt(cand[:].rearrange("p h (a b) -> p h a b", a=16), bc(tvv[:, :, 0, :], 3, [128, 8, 16, 16]),
                      bc(tvv[:, :, 1, :], 2, [128, 8, 16, 16]), ALU.add, r=['tv'], w=['cand'])
                cvt, cpt = cv[tl], cpi[tl]
                ck, pk = 'cv%d' % tl, 'cpi%d' % tl
                for h in range(8):
                    S.dve(lambda e, h=h, cvt=cvt: e.max(out=cvt[:, h, 0:8], in_=cand[:, h, :]), r=['cand'], w=[ck])
                    S.dve(lambda e, h=h, cvt=cvt, cpt=cpt: e.max_index(out=cpt[:, h, 0:8], in_max=cvt[:, h, 0:8], in_values=cand[:, h, :]),
                          r=['cand', ck], w=[pk])
                    S.dve(lambda e, h=h, cvt=cvt: e.match_replace(out=scr[:, 0:256], in_to_replace=cvt[:, h, 0:8], in_values=cand[:, h, :],
                                                                  imm_value=NEG), r=['cand', ck], w=['scr'])
                    S.dve(lambda e, h=h, cvt=cvt: e.max(out=cvt[:, h, 8:16], in_=scr[:, 0:256]), r=['scr'], w=[ck])
                    S.dve(lambda e, h=h, cvt=cvt, cpt=cpt: e.max_index(out=cpt[:, h, 8:16], in_max=cvt[:, h, 8:16], in_values=scr[:, 0:256]),
                          r=['scr', ck], w=[pk])

        def frontB(g):
            for tl in range(TG):
                cvt, cpt = cv[tl], cpi[tl]
                ck, pk = 'cv%d' % tl, 'cpi%d' % tl
                tfv = tif[tl][:].rearrange("p (h c) k -> p h c k", c=2)
                kb.tt(ge[:], cvt[:], cvt[:, :, 0:1].broadcast_to([128, 8, 16]), ALU.subtract, r=[ck], w=['ge'])
                kb.af(ge[:], ge[:], AF.Exp, r=['ge'], w=['ge'])
                kb.red(gz[:, 0:8], ge[:], ALU.add, r=['ge'], w=['gz'])
                kb.recip(gz[:, 0:8], gz[:, 0:8], r=['gz'], w=['gz'])
                kb.tt(gate[:].rearrange("p (h k) -> p h k", h=8), ge[:], bc(gz[:, 0:8], 2, [128, 8, 16]), ALU.mult,
                      r=['ge', 'gz'], w=['gate'])
                S.dve(lambda e, cpt=cpt: e.tensor_single_scalar(out=cu1[:], in_=cpt[:], scalar=4, op=ALU.logical_shift_right), r=[pk], w=['cu1'])
                kb.cp(akf[:], cu1[:], r=['cu1'], w=['akf'])
                S.dve(lambda e, cpt=cpt: e.tensor_single_scalar(out=cu1[:], in_=cpt[:], scalar=15, op=ALU.bitwise_and), r=[pk, 'akf'], w=['cu1'])
                kb.cp(bkf[:], cu1[:], r=['cu1'], w=['bkf'])
                io16 = iot[:, 0:16].unsqueeze(1).unsqueeze(1).broadcast_to([128, 8, 16, 16])
                for (rk, cx, dst, dk) in ((akf, 0, idxi, 'idxi'), (bkf, 1, idxj, 'idxj')):
                    kb.tt(sel, io16, bc(rk[:], 3, [128, 8, 16, 16]), ALU.is_equal, r=['akf', 'bkf', 'iot', ck, pk], w=['cand'])
                    kb.tt(sel, sel, bc(tfv[:, :, cx, :], 2, [128, 8, 16, 16]), ALU.mult, r=['cand', 'tif%d' % tl], w=['cand'], eng='pool')
                    kb.red(dst[:].rearrange("p (h k) -> p h k", h=8), sel, ALU.add, r=['cand'], w=[dk])
                for q, (src, sk_) in enumerate(((idxi, 'idxi'), (idxj, 'idxj'), (gate, 'gate'))):
                    kb.tr(ps[:, 6, q * 128:(q + 1) * 128], src[:], idf[:], r=[sk_, 'idf'], w=['ps6'])
                kb.cp(slotT[:, tl, :, :], ps[:, 6, 0:384].rearrange("p (a c) -> p a c", a=3), r=['ps6'], w=['slotT'], eng='act')
                kb.cp(slotF[:, tl, :, :], ps[:, 6, 128:384].rearrange("p (a c) -> p a c", a=2), r=['ps6'], w=['slotF'], eng='act')

        def onehot(g):
            for tl in range(TG):
                for tb in range(128 // TB):
                    ts_ = slice(tb * TB, (tb + 1) * TB)
                    ob = tb % 2
                    Ao, Bo = Aoh[ob], Boh[ob]
                    ak_, bk_ = 'Aoh%d' % ob, 'Boh%d' % ob
                    io128 = iob[:].unsqueeze(1).broadcast_to([128, TB, 128])
                    kb.tt(Ao[:], io128, bc(slotT[:, tl, 0, ts_], 2, [128, TB, 128]), ALU.is_equal, r=['slotT'] + CST, w=[ak_])
                    bkeys = ['%s_%d' % (bk_, q_) for q_ in range(TB)]
                    for tt_ in range(TB):
                        tok = tb * TB + tt_
                        kb.stt(Bo[:, tt_, :], iob[:], slotF[:, tl, 0, tok:tok + 1], slotF[:, tl, 1, tok:tok + 1].broadcast_to([128, 128]),
                               ALU.is_equal, ALU.mult, r=['slotF'] + CST, w=[bkeys[tt_]])
                    for q4 in range(TB // 4):
                        bk = 4 + q4 % 2
                        for u in range(4):
                            tt_ = q4 * 4 + u
                            kb.mm(ps[:, bk, u * 128:(u + 1) * 128], Ao[:, tt_, :], Bo[:, tt_, :], r=[ak_, bkeys[tt_]], w=['ps%d' % bk])
                        tg0 = tl * 128 + tb * TB + q4 * 4
                        kb.cp(GG[:, tg0:tg0 + 4, :], ps[:, bk, :].rearrange("p (a c) -> p a c", a=4), r=['ps%d' % bk], w=['GG'],
                              eng='act')

        def experts(g, extra, extraB):
            gp = g % 2
            hk = 'h2T%d' % gp
            per = (len(extra) + PEER_A_STEPS - 1) // PEER_A_STEPS if extra else 0
            pos = [0]
            perB = (len(extraB) + 23) // 24 if extraB else 0
            posB = [0]

            def load(jh):
                p = jh % NBUF
                jb, half = jh // 2, jh % 2
                kb.dma(ut[p][:].rearrange("p a b c -> p (a b c)"), uS[jb][:, half * 2048:(half + 1) * 2048], r=['uS'], w=['ut%d' % p])
                kb.dma(vt[p][:], vSv[:, jh * 2:(jh + 1) * 2, :], r=['vS'], w=['vt%d' % p])

            def act(j):
                jh, jj = j // 2, j % 2
                p = jh % NBUF
                bkA = 4 + j % 2
                for dc in range(8):
                    kb.mm(ps[:, bkA, 0:NTK], ut[p][:, jj, dc, :], h2T[gp][:, dc, :], start=(dc == 0), stop=(dc == 7),
                          r=['ut%d' % p, hk], w=['ps%d' % bkA])

            if g == 0:
                load(0)
                load(1)
            act(0)
            for j in range(128):
                jh, jj = j // 2, j % 2
                p = jh % NBUF
                pa = j % 2
                bkA = 4 + pa
                if jj == 0 and jh + 2 < 64:
                    load(jh + 2)
                kb.af(ga[pa][:], ps[:, bkA, 0:NTK], AF.Gelu, r=['ps%d' % bkA], w=['ga%d' % pa])
                if j + 1 < 128:
                    act(j + 1)
                kb.tt(coef[pa][:], ga[pa][:], GG[:, :, j], ALU.mult, r=['ga%d' % pa, 'GG'], w=['coef%d' % pa], eng='pool')
                for tl in range(TG):
                    for n2 in range(2):
                        bkY = tl * 2 + n2
                        kb.mm(ps[:, bkY, :], coef[pa][:, tl * 128:(tl + 1) * 128], vt[p][:, jj, n2 * 512:(n2 + 1) * 512],
                              start=(j == 0), stop=(j == 127), r=['coef%d' % pa, 'vt%d' % p], w=['ps%d' % bkY])
                if extra and pos[0] < len(extra):
                    S.ops.extend(extra[pos[0]:pos[0] + per])
                    pos[0] += per
                if j >= PEER_B_START and extraB and posB[0] < len(extraB):
                    S.ops.extend(extraB[posB[0]:posB[0] + perB])
                    posB[0] += perB
            if extra and pos[0] < len(extra):
                S.ops.extend(extra[pos[0]:])
            if extraB and posB[0] < len(extraB):
                S.ops.extend(extraB[posB[0]:])
            if g + 1 < NG:
                load(0)
                load(1)

        def finish(g):
            gp = g % 2
            for tl in range(TG):
                t0 = (g * TG + tl) * 128
                xk = 'xt%d_%d' % (gp, tl)
                xtt = xt[gp][tl]
                kb.tt(fin[:, 0:512], xtt[:, 0:512], ps[:, tl * 2, :], ALU.add, r=[xk, 'ps%d' % (tl * 2)], w=['fin'])
                kb.tt(fin[:, 512:1024], xtt[:, 512:1024], ps[:, tl * 2 + 1, :], ALU.add, r=[xk, 'ps%d' % (tl * 2 + 1)], w=['fin'])
                kb.af(junk2[:], fin[:], AF.Square, accum=ss2[:, 3:4], r=['fin'], w=['junk2', 'ss3'])
                kb.af(ss2[:, 4:5], ss2[:, 3:4], AF.Sqrt, scale=1.0 / D, bias=RMS_EPS, r=['ss3'], w=['ss4'])
                kb.recip(ss2[:, 5:6], ss2[:, 4:5], r=['ss4'], w=['ss5'])
                kb.stt(fin[:], fin[:], ss2[:, 5:6], lnf_b[:], ALU.mult, ALU.mult, r=['fin', 'ss5'] + CST, w=['fin'])
                kb.dma(out[t0:t0 + 128, :], fin[:], r=['fin'], w=['out'])

        def cap(fn, *args):
            saved = S.ops
            S.ops = []
            fn(*args)
            got = S.ops
            S.ops = saved
            return got

        frontA(0)
        frontB(0)
        for g in range(NG):
            oh = cap(onehot, g)
            fn_ = cap(finish, g - 1) if g > 0 else []
            ratio = max(1, len(oh) // max(1, len(fn_))) if fn_ else 0
            pi = 0
            for k_, op_ in enumerate(oh):
                S.ops.append(op_)
                if fn_ and (k_ + 1) % ratio == 0 and pi < len(fn_):
                    S.ops.append(fn_[pi])
                    pi += 1
            S.ops.extend(fn_[pi:])
            extra, extraB = [], []
            if g + 1 < NG:
                saved = S.ops
                S.ops = []
                frontA(g + 1)
                extra = S.ops
                S.ops = []
                frontB(g + 1)
                extraB = S.ops
                S.ops = saved
            experts(g, extra, extraB)
        finish(NG - 1)
        S.flush()


NSEQ_CORE = 4
SEQ_LEN = 2048
N_CORES = 8


def kernel(**inputs):
    x = np.asarray(inputs["x"], dtype=np.float32)
    B, T, Dm = x.shape
    assert B == NSEQ_CORE * N_CORES and T == SEQ_LEN and Dm == D
    nc = build_program(NSEQ_CORE, SEQ_LEN)
    params = {k: np.ascontiguousarray(np.asarray(inputs[k], dtype=np.float32)) for k in PARAM_SHAPES}
    in_maps = []
    for c in range(N_CORES):
        m = dict(params)
        m["x"] = np.ascontiguousarray(x[c * NSEQ_CORE:(c + 1) * NSEQ_CORE].reshape(NSEQ_CORE * SEQ_LEN, D))
        in_maps.append(m)
    res = run_bass_kernel_spmd(nc, in_maps, core_ids=list(range(N_CORES)))
    outs = [np.asarray(r["out"], dtype=np.float32).reshape(NSEQ_CORE, SEQ_LEN, D) for r in res.results]
    return np.concatenate(outs, axis=0)
```

```python
from contextlib import ExitStack
import numpy as np
import concourse.bass as bass
import concourse.mybir as mybir

F32 = mybir.dt.float32
BF16 = mybir.dt.bfloat16
F32R = mybir.dt.float32r
U32 = mybir.dt.uint32
I32 = mybir.dt.int32
AF = mybir.ActivationFunctionType
ALU = mybir.AluOpType
AX = mybir.AxisListType

NSLOT = 8
PEER_A_STEPS = 80
PEER_B_START = 100
import os as _osx
SAME_ENGINE_INORDER = ('pe',)


class Sched:
    def __init__(self, nc, es):
        self.nc = nc
        self.engs = ['pe', 'act', 'dve', 'pool', 'sp']
        self.dq = ('sp', 'pool', 'act')
        self.esem = {e: es.enter_context(nc.semaphore('s_' + e)) for e in self.engs}
        self.dsem = {e: [es.enter_context(nc.semaphore('d_%s%d' % (e, s))) for s in range(NSLOT)]
                     for e in self.dq}
        self.cnt = {e: 0 for e in self.engs}
        self.dcnt = {e: 0 for e in self.dq}
        self.slot_uses = {e: [0] * NSLOT for e in self.dq}
        self.waited = {e: {} for e in self.engs}
        self.pending = None
        self.ops = []
        self.dummy = es.enter_context(nc.sbuf_tensor('sched_dummy', [128, 8], F32))
        self.n_emitted = 0

    def add(self, eng, fn, r=(), w=(), dma=False):
        self.ops.append(dict(eng=eng, fn=fn, r=tuple(r), w=tuple(w), dma=dma))

    def pe(self, fn, r=(), w=()):
        self.add('pe', fn, r, w)

    def act(self, fn, r=(), w=()):
        self.add('act', fn, r, w)

    def dve(self, fn, r=(), w=()):
        self.add('dve', fn, r, w)

    def pool(self, fn, r=(), w=()):
        self.add('pool', fn, r, w)

    def dma(self, fn, r=(), w=(), q='sp'):
        self.add(q, fn, r, w, dma=True)

    def flush(self):
        nc = self.nc
        dummy = self.dummy
        self.dve(lambda e: e.memset(dummy[:], 0.0), w=['__barrier__'])
        ops = self.ops
        self.ops = []
        self.n_emitted += len(ops)
        last_w = {}
        readers = {}
        for i, o in enumerate(ops):
            deps = set()
            for k in o['r']:
                if k in last_w:
                    deps.add(last_w[k])
            for k in o['w']:
                if k in last_w:
                    deps.add(last_w[k])
                deps.update(readers.get(k, ()))
            deps.discard(i)
            o['deps'] = deps
            for k in o['r']:
                readers.setdefault(k, []).append(i)
            for k in o['w']:
                last_w[k] = i
                readers[k] = []
        J = len(ops) - 1
        last_eng = {}
        for i, o in enumerate(ops[:-1]):
            if o['dma']:
                ops[J]['deps'].add(i)
            else:
                last_eng[o['eng']] = i
        ops[J]['deps'].update(last_eng.values())
        needed = {J}
        for o in ops:
            needed.update(o['deps'])
        slot_last = {e: [None] * NSLOT for e in self.dq}
        for i, o in enumerate(ops):
            e = o['eng']
            if o['dma']:
                s = self.dcnt[e] % NSLOT
                self.dcnt[e] += 1
                self.slot_uses[e][s] += 1
                o['sem'] = self.dsem[e][s]
                o['val'] = 16 * self.slot_uses[e][s]
                o['inc'] = 16
                if slot_last[e][s] is not None:
                    o['deps'].add(slot_last[e][s])
                slot_last[e][s] = i
            elif i in needed:
                self.cnt[e] += 1
                o['sem'] = self.esem[e]
                o['val'] = self.cnt[e]
                o['inc'] = 1
            else:
                o['sem'] = None
        per = {e: [] for e in self.engs}
        for i, o in enumerate(ops):
            per[o['eng']].append(i)
        pending = self.pending

        def run(ename, eng):
            waited = self.waited[ename]
            if pending is not None and ename != 'dve':
                if waited.get(id(pending[0]), 0) < pending[1]:
                    eng.wait_ge(pending[0], pending[1])
                    waited[id(pending[0])] = pending[1]
            for i in per[ename]:
                o = ops[i]
                need = {}
                for j in o['deps']:
                    p = ops[j]
                    if p['sem'] is None:
                        continue
                    if (not p['dma']) and p['eng'] == ename and ename in SAME_ENGINE_INORDER:
                        continue
                    key = id(p['sem'])
                    if key not in need or need[key][1] < p['val']:
                        need[key] = (p['sem'], p['val'])
                for key, (sem, val) in need.items():
                    if waited.get(key, 0) >= val:
                        continue
                    eng.wait_ge(sem, val)
                    waited[key] = val
                ins = o['fn'](eng)
                if o['sem'] is not None:
                    ins.then_inc(o['sem'], o['inc'])
            if ename in self.dsem:
                for s in range(NSLOT):
                    v = 16 * self.slot_uses[ename][s]
                    if v > 0 and waited.get(id(self.dsem[ename][s]), 0) < v:
                        eng.wait_ge(self.dsem[ename][s], v)
                        waited[id(self.dsem[ename][s])] = v

        with nc.Block() as block:
            @block.tensor
            def _(e):
                run('pe', e)

            @block.scalar
            def _(e):
                run('act', e)

            @block.vector
            def _(e):
                run('dve', e)

            @block.gpsimd
            def _(e):
                run('pool', e)

            @block.sync
            def _(e):
                run('sp', e)
        self.pending = (ops[J]['sem'], ops[J]['val'])

from concourse.bass_utils import run_bass_kernel_spmd

D = 1024
HD = 64
NEG = -1e30
C0 = -0.6065306597126334
RMS_EPS = 1e-6
LNX_EPS = 64e-5


class KB:
    def __init__(self, nc, S):
        self.nc = nc
        self.S = S

    def mm(self, out, lhsT, rhs, start=True, stop=True, r=(), w=()):
        self.S.pe(lambda e: e.matmul(out, lhsT=lhsT, rhs=rhs, start=start, stop=stop), r, w)

    def mmr(self, out, lhsT, rhs, start=True, stop=True, r=(), w=()):
        lt = lhsT if lhsT.dtype == F32R else lhsT.bitcast(F32R)
        rh = rhs if rhs.dtype == F32R else rhs.bitcast(F32R)
        self.S.pe(lambda e: e.matmul(out, lhsT=lt, rhs=rh, start=start, stop=stop), r, w)

    def tr(self, out, in_, ident, r=(), w=()):
        self.S.pe(lambda e: e.transpose(out, in_, ident), r, w)

    def tt(self, out, in0, in1, op, r=(), w=(), eng='dve'):
        self.S.add(eng, lambda e: e.tensor_tensor(out=out, in0=in0, in1=in1, op=op), r, w)

    def ts(self, out, in0, s1, op0, s2=None, op1=None, r=(), w=(), eng='dve'):
        if op1 is None:
            self.S.add(eng, lambda e: e.tensor_scalar(out=out, in0=in0, scalar1=s1, scalar2=None, op0=op0), r, w)
        else:
            self.S.add(eng, lambda e: e.tensor_scalar(out=out, in0=in0, scalar1=s1, scalar2=s2, op0=op0, op1=op1), r, w)

    def stt(self, out, in0, scalar, in1, op0, op1, r=(), w=(), eng='dve'):
        self.S.add(eng, lambda e: e.scalar_tensor_tensor(out=out, in0=in0, scalar=scalar, in1=in1, op0=op0, op1=op1), r, w)

    def af(self, out, in_, func, bias=None, scale=None, accum=None, r=(), w=()):
        kw = {}
        if bias is not None:
            kw['bias'] = bias
        if scale is not None:
            kw['scale'] = scale
        if accum is not None:
            kw['accum_out'] = accum
        self.S.act(lambda e: e.activation(out=out, in_=in_, func=func, **kw), r, w)

    def cp(self, out, in_, r=(), w=(), eng='dve'):
        if eng == 'act':
            self.S.act(lambda e: e.copy(out=out, in_=in_), r, w)
        else:
            self.S.add(eng, lambda e: e.tensor_copy(out=out, in_=in_), r, w)

    def red(self, out, in_, op, r=(), w=(), axis=AX.X):
        self.S.dve(lambda e: e.tensor_reduce(out=out, in_=in_, axis=axis, op=op), r, w)

    def recip(self, out, in_, r=(), w=()):
        self.S.dve(lambda e: e.reciprocal(out=out, in_=in_), r, w)

    def memset(self, ap, val, r=(), w=(), eng='dve'):
        self.S.add(eng, lambda e: e.memset(ap, val), r, w)

    def dma(self, out, in_, r=(), w=(), q='sp', slow=False):
        if slow:
            self.S.dma(lambda e: e.dma_start(out=out, in_=in_, allow_slow_non_contiguous=True), r, w, q=q)
        else:
            self.S.dma(lambda e: e.dma_start(out=out, in_=in_), r, w, q=q)


def bc(ap, axis, shape):
    return ap.unsqueeze(axis).broadcast_to(shape)


def setup_consts(nc, kb, es):
    sb = lambda name, shape, dt: es.enter_context(nc.sbuf_tensor(name, shape, dt))
    c = {}
    c['iot'] = sb('iot', [128, 128], F32)
    c['pidx'] = sb('pidx', [128, 1], F32)
    c['idf'] = sb('idf', [128, 128], F32)
    c['idb'] = sb('idb', [128, 128], BF16)
    iot, pidx, idf, idb = c['iot'], c['pidx'], c['idf'], c['idb']
    kb.S.pool(lambda e: e.iota(iot[:], pattern=[[1, 128]], base=0, channel_multiplier=0,
                               allow_small_or_imprecise_dtypes=True), w=['iot'])
    kb.S.pool(lambda e: e.iota(pidx[:], pattern=[[0, 1]], base=0, channel_multiplier=1,
                               allow_small_or_imprecise_dtypes=True), w=['pidx'])
    kb.ts(idf[:], iot[:], pidx[:, 0:1], ALU.is_equal, r=['iot', 'pidx'], w=['idf'])
    kb.cp(idb[:], idf[:], r=['idf'], w=['idb'])
    return c


def phase_rwkv(nc, kb, c, NSEQ, SEQ, x, A, yr):
    RDT = F32R
    S = kb.S
    NT = SEQ // 128
    idf, idb, iot, pidx = c['idf'], c['idb'], c['iot'], c['pidx']
    with ExitStack() as es:
        sb = lambda name, shape, dt: es.enter_context(nc.sbuf_tensor(name, shape, dt))
        ps = es.enter_context(nc.psum_tensor('ps_a', [128, 8, 512], F32))
        W1 = sb('W1', [128, 8, 1696], BF16)
        stage = [sb('stage0', [128, 1696], F32)] * 2
        g1c = sb('g1c', [128, 8], F32)
        w0_b = sb('w0_b', [128, 512], F32)
        a0_b = sb('a0_b', [128, 512], F32)
        kk_b = sb('kk_b', [128, 512], F32)
        ka_b = sb('ka_b', [128, 512], F32)
        lw_b = sb('lw_b', [128, 512], F32)
        lb_b = sb('lb_b', [128, 512], F32)
        rk_b = sb('rk_b', [128, 512], F32)
        mu_b = sb('mu_b', [128, 1536], F32)
        mucol = sb('mucol', [96, 3], F32)
        wup = sb('wup', [32, 512], F32)
        aup = sb('aup', [32, 512], F32)
        gup = sb('gup', [96, 512], F32)
        m_ab = sb('m_ab', [128, 256], F32)
        m_ls = sb('m_ls', [128, 256], F32)
        triI = sb('triI', [128, 128], F32)
        triE = sb('triE', [128, 128], F32)
        triT = sb('triT', [128, 128], F32)
        kb.dma(g1c[:, :], A['ln1_g'][0].rearrange("(c p) -> p c", p=128), w=['g1c'], slow=True)
        for cc in range(8):
            st = stage[cc % 2]
            kb.dma(st[:], A['w_in'][0, cc * 128:(cc + 1) * 128, 0:1696], w=['stage0'])
            (lambda st_, cc_: S.act(lambda e: e.mul(out=W1[:, cc_, :], in_=st_[:], mul=g1c[:, cc_:cc_ + 1]),
                                    r=['stage0', 'g1c'], w=['W1']))(st, cc)
        for t, name in ((w0_b, 'w0'), (a0_b, 'a0'), (kk_b, 'k_k'), (ka_b, 'k_a'), (lw_b, 'lnx_w'), (lb_b, 'lnx_b')):
            kb.dma(t[:], A[name][0:1, :].partition_broadcast(128), w=['cst'])
        kb.dma(rk_b[:], A['r_k'].rearrange("o h d -> o (h d)").partition_broadcast(128), w=['cst'])
        kb.dma(mu_b[:], A['mu_shift'][0:1, 0:1536].partition_broadcast(128), w=['cst'])
        kb.dma(mucol[0:32, 0:1], A['mu_shift'][0, 1536:1568].rearrange("(p o) -> p o", o=1), w=['cst'])
        kb.dma(mucol[0:32, 1:2], A['mu_shift'][0, 1568:1600].rearrange("(p o) -> p o", o=1), w=['cst'])
        kb.dma(mucol[0:96, 2:3], A['mu_shift'][0, 1600:1696].rearrange("(p o) -> p o", o=1), w=['cst'])
        kb.dma(wup[:], A['w_up'][0], w=['cst'])
        kb.dma(aup[:], A['a_up'][0], w=['cst'])
        kb.dma(gup[:], A['g_up'][0], w=['cst'])
        kb.ts(m_ab[:, 0:128], iot[:], pidx[:, 0:1], ALU.is_gt, r=['iot', 'pidx'], w=['cst'])
        kb.ts(m_ab[:, 128:256], iot[:], pidx[:, 0:1], ALU.is_ge, r=['iot', 'pidx'], w=['cst'])
        kb.ts(m_ls[:, 0:128], iot[:], pidx[:, 0:1], ALU.is_lt, r=['iot', 'pidx'], w=['cst'])
        kb.ts(m_ls[:, 128:256], iot[:], pidx[:, 0:1], ALU.is_lt, r=['iot', 'pidx'], w=['cst'])
        kb.ts(triE[:], m_ab[:, 0:128], C0, ALU.mult, r=['cst'], w=['cst2'])
        kb.ts(triI[:], m_ab[:, 128:256], C0, ALU.mult, r=['cst'], w=['cst2'])
        kb.memset(triT[:], C0, w=['cst2'])
        CST = ['cst', 'cst2']
        xt = sb('xt', [128, 1024], F32)
        xn = sb('xn', [128, 1024], BF16)
        hT = sb('hT', [128, 1024], BF16)
        ss = sb('ss', [128, 4], F32)
        rkv2 = [sb('rkv%d' % i, [128, 1536], RDT) for i in range(2)]
        pprev = sb('pprev', [128, 1536], F32)
        lastrow = sb('lastrow', [1, 1536], F32)
        lin = sb('lin', [96, 3, 129], F32)
        ldf = sb('ldf', [96, 3, 128], F32)
        lsh = sb('lsh', [96, 3, 128], F32)
        lact = sb('lact', [96, 2, 128], F32)
        tmpA = sb('tmpA', [128, 512], F32)
        Etot = tmpA
        tmpB = sb('tmpB', [128, 512], F32)
        sgw = sb('sgw', [128, 512], F32)
        a_t = sb('a_t', [128, 512], F32)
        g_t2 = [sb('g_t%d' % i, [128, 512], F32) for i in range(2)]
        kk = sb('kk', [128, 512], F32)
        b_t = sb('b_t', [128, 512], F32)
        TM2 = [sb('TM%d' % i, [128, 4, 512], RDT) for i in range(2)]
        Kh2 = [sb('Kh%d' % i, [128, 512], F32) for i in range(2)]
        Bh2 = [sb('Bh%d' % i, [128, 512], F32) for i in range(2)]
        ex0 = sb('ex0', [128, 512], F32)
        ex1 = sb('ex1', [128, 512], F32)
        n2 = sb('n2', [128, 8], F32)
        FT = sb('FT', [128, 4, 512], RDT)
        gC2 = [sb('gC%d' % i, [64, 8], F32) for i in range(2)]
        tmpP = sb('tmpP', [128, 512], F32)
        Gb2 = [sb('Gb%d' % i, [128, 4, 320], RDT) for i in range(2)]
        Grk2 = [sb('Grk%d' % i, [128, 4, 128], F32) for i in range(2)]
        RTs = sb('RTs', [64, 4, 128], F32)
        GT2 = [sb('GT%d' % i, [128, 4, 256], RDT) for i in range(2)]
        Nn = [sb('Nn%d' % i, [128, 4, 128], BF16) for i in range(2)]
        NTb = [sb('NTb%d' % i, [128, 4, 128], BF16) for i in range(2)]
        NTn = [sb('NTn%d' % i, [128, 4, 128], RDT) for i in range(2)]
        P2 = [sb('P%d' % i, [128, 4, 128], RDT) for i in range(2)]
        ZT = sb('ZT', [128, 4, 192], RDT)
        MN1 = sb('MN1', [128, 4, 192], RDT)
        MN2 = sb('MN2', [128, 4, 192], RDT)
        S0T = [sb('S0T%d' % i, [128, 512], RDT) for i in range(2)]
        y = sb('y', [128, 512], F32)
        yo = sb('yo', [128, 512], BF16)
        st1 = sb('st1', [128, 8], F32)
        st2 = sb('st2', [128, 8], F32)
        st3 = sb('st3', [128, 8], F32)
        def zero_r(ap2d, n, key, rows=128):
            kb.ts(ap2d, idf[0:rows, 0:1].broadcast_to([rows, n]), 0.0, ALU.mult, r=['idf'], w=[key])
        zero_r(FT[:].rearrange("p a b -> p (a b)"), 2048, 'FT')
        kb.memset(lin[:], 0.0, w=['lin', 'lin0'])
        zero_r(MN1[:].rearrange("p a b -> p (a b)"), 768, 'MN1')
        zero_r(S0T[0][:], 512, 'S0T0')
        zero_r(S0T[1][:], 512, 'S0T1')
        fv = lambda ap: (ap.bitcast(F32) if ap.dtype == F32R else ap)
        v3h = lambda ap: ap.rearrange("p (h d) -> p h d", h=8)
        FTf = fv(FT[:])
        psT = ps[:, 6, :].bitcast(BF16)
        tiles = [(s_, i_) for s_ in range(NSEQ) for i_ in range(NT)]

        def prep(n):
            s_, i = tiles[n]
            pb = n % 2
            t0 = n * 128
            first = (i == 0)
            rkv, g_t, TM, Kh, Bh, gC = rkv2[pb], g_t2[pb], TM2[pb], Kh2[pb], Bh2[pb], gC2[pb]
            RK, GK, TK, KK, BK, CK = 'rkv%d' % pb, 'g_t%d' % pb, 'TM%d' % pb, 'Kh%d' % pb, 'Bh%d' % pb, 'gC%d' % pb
            rkvf = fv(rkv[:])
            r_ = rkvf[:, 0:512]
            k_ = rkvf[:, 512:1024]
            k_w = rkv[:, 512:1024]
            kb.dma(xt[:], x[t0:t0 + 128, :], w=['xt'])
            kb.af(xn[:], xt[:], AF.Square, accum=ss[:, 0:1], r=['xt'], w=['xn', 'ss'])
            kb.af(ss[:, 1:2], ss[:, 0:1], AF.Ln, scale=1.0 / D, bias=RMS_EPS, r=['ss'], w=['ss1'])
            kb.af(ss[:, 2:3], ss[:, 1:2], AF.Exp, scale=-0.5, r=['ss1'], w=['ss2'])
            S.act(lambda e: e.mul(out=xn[:], in_=xt[:], mul=ss[:, 2:3]), r=['xt', 'ss2'], w=['xn'])
            for cc in range(8):
                kb.tr(psT[:, cc * 128:(cc + 1) * 128], xn[:, cc * 128:(cc + 1) * 128], idb[:], r=['xn', 'idb'], w=['ps6'])
            kb.cp(hT[:], psT[:, :], r=['ps6'], w=['hT'], eng='act')
            for j in range(3):
                bk = 7 if j % 2 == 0 else 6
                for cc in range(8):
                    kb.mm(ps[:, bk, :], hT[:, cc * 128:(cc + 1) * 128], W1[:, cc, j * 512:(j + 1) * 512],
                          start=(cc == 0), stop=(cc == 7), r=['hT', 'W1'], w=['ps%d' % bk])
                kb.cp(rkv[:, j * 512:(j + 1) * 512], ps[:, bk, :], r=['ps%d' % bk], w=[RK], eng=('act' if j != 1 else 'dve'))
            for (lo, nn, col) in ((1536, 32, 0), (1568, 32, 128), (1600, 96, 256)):
                for cc in range(8):
                    kb.mm(ps[0:nn, 6, col:col + 128], W1[:, cc, lo:lo + nn], hT[:, cc * 128:(cc + 1) * 128],
                          start=(cc == 0), stop=(cc == 7), r=['hT', 'W1'], w=['ps6'])
            if first:
                kb.memset(pprev[0:1, :], 0.0, w=['pprev0'])
            else:
                kb.dma(pprev[0:1, :], lastrow[0:1, :], r=['lastrow'], w=['pprev0'])
            kb.dma(pprev[1:128, :], rkvf[0:127, :], r=[RK], w=['pprevR'])
            kb.dma(lastrow[0:1, :], rkvf[127:128, :], r=[RK], w=['lastrow'])
            kb.tt(pprev[:], pprev[:], rkvf, ALU.subtract, r=['pprev0', 'pprevR', RK], w=['pprev0', 'pprevR'])
            kb.tt(pprev[:], pprev[:], mu_b[:], ALU.mult, r=['pprev0', 'pprevR'] + CST, w=['pprev0', 'pprevR'], eng='pool')
            kb.tt(rkv[:], rkvf, pprev[:], ALU.add, r=['pprev0', 'pprevR', RK], w=[RK])
            if first:
                kb.memset(lin[:, :, 0:1], 0.0, w=['lin0'])
            else:
                kb.cp(lin[:, :, 0:1], lin[:, :, 128:129], r=['lin'], w=['lin0'])
            kb.cp(lin[0:32, 0, 1:129], ps[0:32, 6, 0:128], r=['ps6', 'lin0'], w=['lin'], eng='act')
            kb.cp(lin[0:32, 1, 1:129], ps[0:32, 6, 128:256], r=['ps6', 'lin0'], w=['lin'], eng='act')
            kb.cp(lin[0:96, 2, 1:129], ps[0:96, 6, 256:384], r=['ps6', 'lin0'], w=['lin'], eng='act')
            for l, nn in ((0, 32), (1, 32), (2, 96)):
                kb.tt(ldf[0:nn, l, :], lin[0:nn, l, 0:128], lin[0:nn, l, 1:129], ALU.subtract, r=['lin', 'lin0'], w=['ldf'])
                kb.stt(lsh[0:nn, l, :], ldf[0:nn, l, :], mucol[0:nn, l:l + 1], lin[0:nn, l, 1:129], ALU.mult, ALU.add,
                       r=['ldf', 'lin'] + CST, w=['lsh'])
            kb.af(lact[0:32, 0, :], lsh[0:32, 0, :], AF.Tanh, r=['lsh'], w=['lact'])
            kb.af(lact[0:96, 1, :], lsh[0:96, 2, :], AF.Sigmoid, r=['lsh'], w=['lact'])
            kb.mm(ps[:, 7, :], lact[0:32, 0, :], wup[0:32, :], r=['lact'] + CST, w=['ps7'])
            kb.tt(tmpA[:], ps[:, 7, :], w0_b[:], ALU.add, r=['ps7'] + CST, w=['tmpA'])
            kb.mm(ps[:, 6, :], lsh[0:32, 1, :], aup[0:32, :], r=['lsh'] + CST, w=['ps6'])
            kb.tt(tmpB[:], ps[:, 6, :], a0_b[:], ALU.add, r=['ps6'] + CST, w=['tmpB'])
            kb.mm(ps[:, 7, :], lact[0:96, 1, :], gup[0:96, :], r=['lact'] + CST, w=['ps7'])
            kb.cp(g_t[:], ps[:, 7, :], r=['ps7'], w=[GK], eng='act')
            kb.af(sgw[:], tmpA[:], AF.Sigmoid, r=['tmpA'], w=['sgw'])
            kb.af(a_t[:], tmpB[:], AF.Sigmoid, r=['tmpB'], w=['a_t'])
            kb.tt(kk[:], k_, kk_b[:], ALU.mult, r=[RK] + CST, w=['kk'])
            kb.tt(tmpA[:], kk[:], kk[:], ALU.mult, r=['kk'], w=['tmpA'], eng='pool')
            kb.red(n2[:, 0:8], v3h(tmpA[:]), ALU.add, r=['tmpA'], w=['n2'])
            kb.af(n2[:, 0:8], n2[:, 0:8], AF.Sqrt, r=['n2'], w=['n2'])
            kb.ts(n2[:, 0:8], n2[:, 0:8], 1e-12, ALU.max, r=['n2'], w=['n2'])
            kb.recip(n2[:, 0:8], n2[:, 0:8], r=['n2'], w=['n2'])
            kb.tt(v3h(kk[:]), v3h(kk[:]), bc(n2[:, 0:8], 2, [128, 8, 64]), ALU.mult, r=['kk', 'n2'], w=['kk'])
            kb.stt(tmpB[:], a_t[:], -1.0, ka_b[:], ALU.add, ALU.mult, r=['a_t'] + CST, w=['tmpB'])
            kb.stt(k_w, tmpB[:], 1.0, k_, ALU.add, ALU.mult, r=['tmpB', RK], w=[RK])
            kb.tt(b_t[:], kk[:], a_t[:], ALU.mult, r=['kk', 'a_t'], w=['b_t'], eng='pool')
            kb.mm(ps[:, 6, :], triI[:], sgw[:], r=['sgw'] + CST, w=['ps6'])
            kb.mm(ps[:, 7, :], triE[:], sgw[:], r=['sgw'] + CST, w=['ps7'])
            kb.af(ex0[:], ps[:, 6, :], AF.Exp, r=['ps6'], w=['ex0'])
            kb.tt(TM[:, 1, :], r_, ex0[:], ALU.mult, r=[RK, 'ex0'], w=[TK])
            kb.af(ex1[:], ps[:, 7, :], AF.Exp, r=['ps7'], w=['ex1'])
            kb.stt(TM[:, 0, :], kk[:], -1.0, ex1[:], ALU.mult, ALU.mult, r=['kk', 'ex1'], w=[TK])
            kb.af(ex0[:], ps[:, 6, :], AF.Exp, scale=-1.0, r=['ps6'], w=['ex0'])
            kb.mm(ps[:, 7, :], triT[:], sgw[:], r=['sgw'] + CST, w=['ps7'])
            kb.tt(TM[:, 2, :], b_t[:], ex0[:], ALU.mult, r=['b_t', 'ex0'], w=[TK])
            kb.tt(TM[:, 3, :], k_, ex0[:], ALU.mult, r=[RK, 'ex0'], w=[TK])
            kb.af(Etot[:], ps[:, 7, :], AF.Exp, r=['ps7'], w=['tmpA'])
            kb.tt(ex1[:], Etot[:], ex0[:], ALU.mult, r=['tmpA', 'ex0'], w=['ex1'], eng='pool')
            kb.tt(Kh[:], k_, ex1[:], ALU.mult, r=[RK, 'ex1'], w=[KK], eng='pool')
            kb.tt(Bh[:], b_t[:], ex1[:], ALU.mult, r=['b_t', 'ex1'], w=[BK])
            for h in range(8):
                kb.tr(ps[0:64, 6, h * 32:(h + 1) * 32], Etot[0:32, h * 64:(h + 1) * 64], idf[0:32, 0:32],
                      r=['tmpA', 'idf'], w=['ps6'])
            kb.cp(gC[0:64, 0:8], ps[0:64, 6, 0:256].rearrange("p (h c) -> p h c", c=32)[:, :, 0], r=['ps6'], w=[CK], eng='act')

        state = {'cur': 0}

        def heavy(n):
            s_, i = tiles[n]
            pb = n % 2
            t0 = n * 128
            cur = state['cur']
            rkv, g_t, TM, Kh, Bh, gC = rkv2[pb], g_t2[pb], TM2[pb], Kh2[pb], Bh2[pb], gC2[pb]
            RK, GK, TK, KK, BK, CK = 'rkv%d' % pb, 'g_t%d' % pb, 'TM%d' % pb, 'Kh%d' % pb, 'Bh%d' % pb, 'gC%d' % pb
            rkvf = fv(rkv[:])
            r_ = rkvf[:, 0:512]
            k_ = rkvf[:, 512:1024]
            v_ = rkvf[:, 1024:1536]
            TMf = fv(TM[:])
            nxt = 1 - cur
            for g in range(2):
                Gb, GT, Grk, P = Gb2[g], GT2[g], Grk2[g], P2[g]
                GBK, GTK, GRK, PK = 'Gb%d' % g, 'GT%d' % g, 'Grk%d' % g, 'P%d' % g
                for hh in range(4):
                    h = 4 * g + hh
                    for q in range(4):
                        kb.tr(ps[0:64, 5, q * 128:(q + 1) * 128], TMf[:, q, h * 64:(h + 1) * 64], idf[:],
                              r=[TK, 'idf'], w=['ps5'])
                    kb.cp(FT[0:64, hh, :], ps[0:64, 5, :], r=['ps5'], w=['FT'], eng='act')
                for hh in range(4):
                    kb.mmr(ps[:, hh // 2, (hh % 2) * 256:(hh % 2) * 256 + 256], FT[:, hh, 256:384], FT[:, hh, 0:256],
                           r=['FT'], w=['ps%d' % (hh // 2)])
                    kb.mmr(ps[:, 2, hh * 128:(hh + 1) * 128], FT[:, hh, 384:512], FT[:, hh, 128:256], r=['FT'], w=['ps2'])
                    kb.mmr(ps[:, 3 + hh // 2, (hh % 2) * 256:(hh % 2) * 256 + 256], FT[:, hh, 0:128], FT[:, hh, 256:512],
                           r=['FT'], w=['ps%d' % (3 + hh // 2)])
                if g == 0:
                    kb.cp(RTs[0:64, :, :], FTf[0:64, :, 128:256], r=['FT'], w=['RTs'])
                for b2 in range(2):
                    kb.tt(Gb[:, 2 * b2:2 * b2 + 2, 0:256], ps[:, b2, :].rearrange("p (a c) -> p a c", a=2),
                          bc(m_ab[:], 1, [128, 2, 256]), ALU.mult, r=['ps%d' % b2] + CST, w=[GBK])
                    kb.tt(GT[:, 2 * b2:2 * b2 + 2, :], ps[:, 3 + b2, :].rearrange("p (a c) -> p a c", a=2),
                          bc(m_ls[:], 1, [128, 2, 256]), ALU.mult, r=['ps%d' % (3 + b2)] + CST, w=[GTK])
                kb.tt(Grk[:], ps[:, 2, :].rearrange("p (a c) -> p a c", a=4), bc(m_ab[:, 128:256], 1, [128, 4, 128]),
                      ALU.mult, r=['ps2'] + CST, w=[GRK])
                kb.cp(Gb[:, :, 256:320], Bh[:, g * 256:(g + 1) * 256].rearrange("p (a c) -> p a c", a=4), r=[BK], w=[GBK])
                kb.tt(P[:], fv(Gb[:])[:, :, 0:128], bc(idf[:], 1, [128, 4, 128]), ALU.add, r=[GBK, 'idf'], w=[PK])
            for it in range(6):
                for g in range(2):
                    Gb, GT, P = Gb2[g], GT2[g], P2[g]
                    PK = 'P%d' % g
                    bN, bT, bP = 3 * g, 3 * g + 1, 3 * g + 2
                    if it == 0:
                        Ncur = (lambda Gb_: (lambda hh: Gb_[:, hh, 0:128]))(Gb)
                        NTcur = (lambda GT_: (lambda hh: GT_[:, hh, 0:128]))(GT)
                        nk, ntk = 'Gb%d' % g, 'GT%d' % g
                    else:
                        Ncur = (lambda g_: (lambda hh: Nn[g_][:, hh, :]))(g)
                        NTcur = (lambda g_: (lambda hh: NTb[g_][:, hh, :]))(g)
                        nk, ntk = 'Nn%d' % g, 'NTb%d' % g
                    mmf = kb.mmr if it == 0 else kb.mm
                    if it < 5:
                        for hh in range(4):
                            mmf(ps[:, bN, hh * 128:(hh + 1) * 128], NTcur(hh), Ncur(hh), r=[nk, ntk], w=['ps%d' % bN])
                    for hh in range(4):
                        mmf(ps[:, bT, hh * 128:(hh + 1) * 128], Ncur(hh), NTcur(hh), r=[nk, ntk], w=['ps%d' % bT])
                    if it < 5:
                        kb.cp(Nn[g][:], ps[:, bN, :].rearrange("p (a c) -> p a c", a=4), r=['ps%d' % bN], w=['Nn%d' % g], eng='act')
                        kb.cp(NTb[g][:], ps[:, bT, :].rearrange("p (a c) -> p a c", a=4), r=['ps%d' % bT], w=['NTb%d' % g], eng='act')
                    kb.cp(NTn[g][:], ps[:, bT, :].rearrange("p (a c) -> p a c", a=4), r=['ps%d' % bT, 'NTb%d' % g], w=['NTn%d' % g], eng='act')
                    for hh in range(4):
                        kb.mmr(ps[:, bP, hh * 128:(hh + 1) * 128], NTn[g][:, hh, :], P[:, hh, :], r=['NTn%d' % g, PK], w=['ps%d' % bP])
                    kb.tt(P[:], fv(P[:]), ps[:, bP, :].rearrange("p (a c) -> p a c", a=4), ALU.add, r=[PK, 'ps%d' % bP], w=[PK])
            for g in range(2):
                Gb, GT, Grk, P = Gb2[g], GT2[g], Grk2[g], P2[g]
                GBK, GTK, GRK, PK = 'Gb%d' % g, 'GT%d' % g, 'Grk%d' % g, 'P%d' % g
                for hh in range(4):
                    h = 4 * g + hh
                    o = (hh % 2) * 192
                    kb.mmr(ps[:, hh // 2, o:o + 64], P[:, hh, :], TM[:, 0, h * 64:(h + 1) * 64], r=[PK, TK], w=['ps%d' % (hh // 2)])
                    kb.mmr(ps[:, hh // 2, o + 64:o + 192], P[:, hh, :], GT[:, hh, 128:256], r=[PK, GTK], w=['ps%d' % (hh // 2)])
                for b2 in range(2):
                    kb.cp(ZT[:, 2 * b2:2 * b2 + 2, :], ps[:, b2, 0:384].rearrange("p (a c) -> p a c", a=2),
                          r=['ps%d' % b2], w=['ZT'], eng=('act' if b2 == 0 else 'dve'))
                for hh in range(4):
                    o = (hh % 2) * 192
                    kb.mmr(ps[0:64, 3 + hh // 2, o:o + 192], ZT[:, hh, 0:64], Gb[:, hh, 128:320], r=['ZT', GBK], w=['ps%d' % (3 + hh // 2)])
                for hh in range(4):
                    o = (hh % 2) * 192
                    kb.mmr(ps[:, hh // 2, o:o + 192], ZT[:, hh, 64:192], Gb[:, hh, 128:320], r=['ZT', GBK], w=['ps%d' % (hh // 2)])
                for b2 in range(2):
                    pv1 = ps[0:64, 3 + b2, 0:384].rearrange("p (a c) -> p a c", a=2)
                    rsrc = RTs[0:64, 2 * b2:2 * b2 + 2, :] if g == 0 else FTf[0:64, 2 * b2:2 * b2 + 2, 128:256]
                    kb.tt(MN1[0:64, 2 * b2:2 * b2 + 2, 0:128], pv1[:, :, 0:128], rsrc, ALU.add,
                          r=['ps%d' % (3 + b2), 'FT', 'RTs'], w=['MN1'])
                    for a2 in range(2):
                        hh = 2 * b2 + a2
                        h = 4 * g + hh
                        kb.stt(MN1[0:64, hh, 128:192], idf[0:64, 0:64], gC[0:64, h:h + 1], ps[0:64, 3 + b2, a2 * 192 + 128:a2 * 192 + 192],
                               ALU.mult, ALU.add, r=['ps%d' % (3 + b2), CK, 'idf'], w=['MN1'])
                    pv2 = ps[:, b2, 0:384].rearrange("p (a c) -> p a c", a=2)
                    kb.tt(MN2[:, 2 * b2:2 * b2 + 2, 0:128], pv2[:, :, 0:128], Grk[:, 2 * b2:2 * b2 + 2, :], ALU.add,
                          r=['ps%d' % b2, GRK], w=['MN2'])
                    h0 = 4 * g + 2 * b2
                    kb.tt(MN2[:, 2 * b2:2 * b2 + 2, 128:192], pv2[:, :, 128:192],
                          Kh[:, h0 * 64:(h0 + 2) * 64].rearrange("p (a c) -> p a c", a=2), ALU.add,
                          r=['ps%d' % b2, KK], w=['MN2'])
                sk = 'S0T%d' % cur
                for hh in range(4):
                    h = 4 * g + hh
                    hs = slice(h * 64, (h + 1) * 64)
                    ys_ = slice(256 + hh * 64, 256 + (hh + 1) * 64)
                    ss_ = slice(hh * 64, (hh + 1) * 64)
                    kb.mmr(ps[:, 2, ys_], MN1[:, hh, 0:128], S0T[cur][:, hs], start=True, stop=False, r=['MN1', sk], w=['ps2'])
                    kb.mmr(ps[:, 2, ys_], MN2[:, hh, 0:128], rkv[:, 1024 + h * 64:1024 + (h + 1) * 64], start=False, stop=True,
                           r=['MN2', RK], w=['ps2'])
                    kb.mmr(ps[0:64, 2, ss_], MN1[:, hh, 128:192], S0T[cur][:, hs], start=True, stop=False, r=['MN1', sk], w=['ps2'])
                    kb.mmr(ps[0:64, 2, ss_], MN2[:, hh, 128:192], rkv[:, 1024 + h * 64:1024 + (h + 1) * 64], start=False, stop=True,
                           r=['MN2', RK], w=['ps2'])
                if i == NT - 1:
                    zero_r(S0T[nxt][0:64, g * 256:(g + 1) * 256], 256, 'S0T%d' % nxt, rows=64)
                else:
                    kb.cp(S0T[nxt][0:64, g * 256:(g + 1) * 256], ps[0:64, 2, 0:256], r=['ps2'], w=['S0T%d' % nxt], eng='act')
                kb.cp(y[:, g * 256:(g + 1) * 256], ps[:, 2, 256:512], r=['ps2'], w=['y'], eng='act')
            state['cur'] = nxt
            kb.red(st1[:, 0:8], v3h(y[:]), ALU.add, r=['y'], w=['st1'])
            kb.tt(tmpP[:], y[:], y[:], ALU.mult, r=['y'], w=['tmpP'], eng='pool')
            kb.red(st2[:, 0:8], v3h(tmpP[:]), ALU.add, r=['tmpP'], w=['st2'])
            kb.ts(st1[:, 0:8], st1[:, 0:8], 1.0 / 64, ALU.mult, r=['st1'], w=['st1'])
            kb.tt(st3[:, 0:8], st1[:, 0:8], st1[:, 0:8], ALU.mult, r=['st1'], w=['st3'])
            kb.stt(st2[:, 0:8], st2[:, 0:8], 1.0 / 64, st3[:, 0:8], ALU.mult, ALU.subtract, r=['st2', 'st3'], w=['st2'])
            kb.af(st2[:, 0:8], st2[:, 0:8], AF.Sqrt, bias=LNX_EPS, r=['st2'], w=['st2'])
            kb.recip(st2[:, 0:8], st2[:, 0:8], r=['st2'], w=['st2'])
            kb.tt(v3h(y[:]), v3h(y[:]), bc(st1[:, 0:8], 2, [128, 8, 64]), ALU.subtract, r=['y', 'st1'], w=['y'])
            kb.tt(v3h(y[:]), v3h(y[:]), bc(st2[:, 0:8], 2, [128, 8, 64]), ALU.mult, r=['y', 'st2'], w=['y'])
            kb.tt(y[:], y[:], lw_b[:], ALU.mult, r=['y'] + CST, w=['y'])
            kb.tt(y[:], y[:], lb_b[:], ALU.add, r=['y'] + CST, w=['y'])
            kb.tt(tmpP[:], r_, k_, ALU.mult, r=[RK], w=['tmpP'], eng='pool')
            kb.tt(tmpP[:], tmpP[:], rk_b[:], ALU.mult, r=['tmpP'] + CST, w=['tmpP'], eng='pool')
            kb.red(st3[:, 0:8], v3h(tmpP[:]), ALU.add, r=['tmpP'], w=['st3'])
            kb.tt(v3h(tmpP[:]), v3h(v_), bc(st3[:, 0:8], 2, [128, 8, 64]), ALU.mult, r=[RK, 'st3'], w=['tmpP'])
            kb.tt(y[:], y[:], tmpP[:], ALU.add, r=['y', 'tmpP'], w=['y'])
            kb.tt(yo[:], y[:], g_t[:], ALU.mult, r=['y', GK], w=['yo'])
            kb.dma(yr[t0:t0 + 128, :], yo[:], r=['yo'], w=['yr'])

        def capture(fn, n):
            saved = S.ops
            S.ops = []
            fn(n)
            got = S.ops
            S.ops = saved
            return got

        prep(0)
        for n in range(len(tiles)):
            hv = capture(heavy, n)
            pr = capture(prep, n + 1) if n + 1 < len(tiles) else []
            ratio = (len(hv) // max(1, len(pr))) if pr else 0
            pi = 0
            for k, op_ in enumerate(hv):
                S.ops.append(op_)
                if pr and ratio > 0 and (k + 1) % ratio == 0 and pi < len(pr):
                    S.ops.append(pr[pi])
                    pi += 1
            S.ops.extend(pr[pi:])
        S.flush()


PARAM_SHAPES = {
    "ln1_g": [1, 1024], "w_in": [1, 1024, 2464], "b_attn": [1, 768], "mu_shift": [1, 1696],
    "w0": [1, 512], "w_up": [1, 32, 512], "a0": [1, 512], "a_up": [1, 32, 512], "g_up": [1, 96, 512],
    "k_k": [1, 512], "k_a": [1, 512], "r_k": [1, 8, 64], "lnx_w": [1, 512], "lnx_b": [1, 512],
    "attn_sinks": [1, 8], "attn_norm_g": [1, 512], "w_out": [1, 1024, 1024], "ln2_g": [1, 1024],
    "peer_wq": [1, 1024, 2048], "peer_subkeys": [1, 8, 2, 128, 128], "peer_u": [1, 16384, 1024],
    "peer_v": [1, 16384, 1024], "lnf_g": [1024],
}


def build_program(NSEQ, SEQ, phases=('prep', 'rwkv', 'attn', 'peer'), dbg=False, x1_in=False):
    nc = bass.Bass("TRN2", target_bir_lowering=False)
    NTOK = NSEQ * SEQ
    x = nc.dram_tensor("x", [NTOK, D], F32, kind="ExternalInput").ap()
    A = {k: nc.dram_tensor(k, s, F32, kind="ExternalInput").ap() for k, s in PARAM_SHAPES.items()}
    out = nc.dram_tensor("out", [NTOK, D], F32, kind="ExternalOutput").ap()
    sk = "ExternalOutput" if dbg else "Internal"
    yr = nc.dram_tensor("yr", [NTOK, 512], BF16, kind=sk).ap()
    x1 = nc.dram_tensor("x1", [NTOK, D], F32, kind=("ExternalInput" if x1_in else sk)).ap()
    uS = nc.dram_tensor("uS", [32, 128, 4 * 8 * 128], BF16, kind="Internal").ap()
    vS = nc.dram_tensor("vS", [16384, D], BF16, kind="Internal").ap()
    with ExitStack() as es:
        S = Sched(nc, es)
        kb = KB(nc, S)
        c = setup_consts(nc, kb, es)
        fused_prep = ('prep' in phases) and ('attn' in phases)
        if 'prep' in phases and not fused_prep:
            phase_prep(nc, kb, c, A, uS, vS)
        if 'rwkv' in phases:
            phase_rwkv(nc, kb, c, NSEQ, SEQ, x, A, yr)
        if 'attn' in phases:
            phase_attn(nc, kb, c, NSEQ, SEQ, x, A, yr, x1, prep=((uS, vS) if fused_prep else None))
        if 'peer' in phases:
            phase_peer(nc, kb, c, NTOK, x1, A, uS, vS, out)
        print("ops emitted:", S.n_emitted)
    return nc


def phase_attn(nc, kb, c, NSEQ, SEQ, x, A, yr, x1, prep=None):
    S = kb.S
    NT = SEQ // 128
    idf, idb, iot, pidx = c['idf'], c['idb'], c['iot'], c['pidx']
    with ExitStack() as es:
        sb = lambda name, shape, dt: es.enter_context(nc.sbuf_tensor(name, shape, dt))
        ps = es.enter_context(nc.psum_tensor('ps_b', [128, 8, 512], F32))
        Wat = sb('Wat', [128, 8, 768], BF16)
        Wout = sb('Wout', [128, 8, 1024], BF16)
        stage = [sb('bstage%d' % i, [128, 1024], F32) for i in range(2)]
        g1c = sb('g1cb', [128, 8], F32)
        gan = sb('gan', [128, 4], F32)
        bq8 = sb('bq8', [64, 8], F32)
        bkc = sb('bkc', [64, 2], F32)
        bv_b = sb('bv_b', [128, 128], F32)
        snk = sb('snk', [128, 8], F32)
        mPC = sb('mPC', [128, 256], F32)
        mF = sb('mF', [128, 256], F32)
        kb.dma(g1c[:, :], A['ln1_g'][0].rearrange("(c p) -> p c", p=128), w=['cst'], slow=True)
        kb.dma(gan[:, :], A['attn_norm_g'][0].rearrange("(c p) -> p c", p=128), w=['cst'], slow=True)
        kb.dma(bq8[:, :], A['b_attn'][0, 0:512].rearrange("(h d) -> d h", d=64), w=['cst'], slow=True)
        kb.dma(bkc[:, :], A['b_attn'][0, 512:640].rearrange("(h d) -> d h", d=64), w=['cst'], slow=True)
        kb.dma(bv_b[:], A['b_attn'][0:1, 640:768].partition_broadcast(128), w=['cst'])
        kb.dma(snk[:], A['attn_sinks'][0:1, :].partition_broadcast(128), w=['cst'])
        kb.ts(bq8[:], bq8[:], 0.125, ALU.mult, r=['cst'], w=['cst2'])
        for cc in range(8):
            st = stage[cc % 2]
            sk_ = 'bstage%d' % (cc % 2)
            kb.dma(st[:, 0:768], A['w_in'][0, cc * 128:(cc + 1) * 128, 1696:2464], w=[sk_])
            (lambda st_, cc_, sk__: S.act(lambda e: e.mul(out=Wat[:, cc_, :], in_=st_[:, 0:768], mul=g1c[:, cc_:cc_ + 1]),
                                          r=[sk__, 'cst'], w=['Wat']))(st, cc, sk_)
        for cc in range(8):
            st = stage[cc % 2]
            sk_ = 'bstage%d' % (cc % 2)
            kb.dma(st[:, :], A['w_out'][0, cc * 128:(cc + 1) * 128, :], w=[sk_])
            if cc < 4:
                kb.cp(Wout[:, cc, :], st[:, :], r=[sk_], w=['Wout'], eng='act')
            else:
                (lambda st_, cc_, sk__: S.act(lambda e: e.mul(out=Wout[:, cc_, :], in_=st_[:, :], mul=gan[:, cc_ - 4:cc_ - 3]),
                                              r=[sk__, 'cst'], w=['Wout']))(st, cc, sk_)
        kb.ts(mPC[:, 0:128], iot[:], pidx[:, 0:1], ALU.is_gt, r=['iot', 'pidx'], w=['cst3'])
        kb.ts(mPC[:, 128:256], iot[:], pidx[:, 0:1], ALU.is_le, r=['iot', 'pidx'], w=['cst3'])
        kb.ts(mPC[:], mPC[:], 1e30, ALU.mult, -1e30, ALU.add, r=['cst3'], w=['cst4'])
        kb.memset(mF[:, 0:128], NEG, w=['cst5'])
        kb.cp(mF[:, 128:256], mPC[:, 128:256], r=['cst4'], w=['cst5'])
        CST = ['cst', 'cst2', 'cst4', 'cst5']
        xt = sb('bxt', [128, 1024], F32)
        junk = sb('bjunk', [128, 1024], BF16)
        xn = sb('bxn', [128, 1024], BF16)
        hT = sb('bhT', [128, 1024], BF16)
        ss = sb('bss', [128, 8], F32)
        qT = sb('qT', [128, 8, 128], BF16)
        kT = [sb('kT%d' % i, [128, 2, 128], BF16) for i in range(2)]
        kb.memset(qT[:], 0.0, w=['qT'])
        vb = [sb('vb%d' % i, [128, 128], BF16) for i in range(2)]
        Sm = sb('Sm', [128, 8, 256], F32)
        E = sb('E', [128, 8, 256], BF16)
        ET = sb('ET', [128, 2048], BF16)
        mx = sb('mx', [128, 8], F32)
        nmx = sb('nmx', [128, 8], F32)
        rs = sb('rs', [128, 8], F32)
        esk = sb('esk', [128, 8], F32)
        o = sb('o', [128, 512], F32)
        ycat = sb('ycat', [128, 1024], BF16)
        ycT = sb('ycT', [128, 1024], BF16)
        for i in range(2):
            kb.memset(kT[i][:], 0.0, w=['kT%d' % i])
            kb.memset(vb[i][:], 0.0, w=['vb%d' % i])
        psT6 = ps[:, 6, :].bitcast(BF16)
        prep_emit = make_prep(nc, kb, c, A, prep[0], prep[1], es, ps, [7]) if prep is not None else None
        n_tiles_total = NSEQ * NT
        prep_units = [(jb, part) for jb in range(32) for part in range(2)] if prep is not None else []
        prep_done = 0
        par = 0
        STOP = 99
        for s in range(NSEQ):
            for i in range(NT):
                if STOP <= 1:
                    continue
                tile_idx = s * NT + i
                tile_start = len(S.ops)
                t0 = (s * NT + i) * 128
                first = (i == 0)
                kc, kp = 'kT%d' % par, 'kT%d' % (1 - par)
                vc, vp = 'vb%d' % par, 'vb%d' % (1 - par)
                kb.dma(xt[:], x[t0:t0 + 128, :], w=['xt'])
                kb.dma(ycat[:, 0:512], yr[t0:t0 + 128, :], r=['yr'], w=['ycatA'])
                kb.af(junk[:], xt[:], AF.Square, accum=ss[:, 0:1], r=['xt'], w=['junk', 'ss'])
                kb.af(ss[:, 1:2], ss[:, 0:1], AF.Sqrt, scale=1.0 / D, bias=RMS_EPS, r=['ss'], w=['ss1'])
                kb.recip(ss[:, 2:3], ss[:, 1:2], r=['ss1'], w=['ss2'])
                S.act(lambda e: e.mul(out=xn[:], in_=xt[:], mul=ss[:, 2:3]), r=['xt', 'ss2'], w=['xn'])
                for cc in range(8):
                    kb.tr(psT6[:, cc * 128:(cc + 1) * 128], xn[:, cc * 128:(cc + 1) * 128], idb[:], r=['xn', 'idb'], w=['ps6'])
                kb.cp(hT[:], psT6[:, :], r=['ps6'], w=['hT'])
                for h in range(8):
                    for cc in range(8):
                        kb.mm(ps[0:64, h // 4, (h % 4) * 128:(h % 4 + 1) * 128], Wat[:, cc, h * 64:(h + 1) * 64],
                              hT[:, cc * 128:(cc + 1) * 128], start=(cc == 0), stop=(cc == 7), r=['hT', 'Wat'], w=['ps%d' % (h // 4)])
                for kv in range(2):
                    for cc in range(8):
                        kb.mm(ps[0:64, 2, kv * 128:(kv + 1) * 128], Wat[:, cc, 512 + kv * 64:512 + (kv + 1) * 64],
                              hT[:, cc * 128:(cc + 1) * 128], start=(cc == 0), stop=(cc == 7), r=['hT', 'Wat'], w=['ps2'])
                for cc in range(8):
                    kb.mm(ps[:, 2, 256:384], hT[:, cc * 128:(cc + 1) * 128], Wat[:, cc, 640:768],
                          start=(cc == 0), stop=(cc == 7), r=['hT', 'Wat'], w=['ps2'])
                for b2 in range(2):
                    kb.stt(qT[0:64, 4 * b2:4 * b2 + 4, :], ps[0:64, b2, :].rearrange("p (a c) -> p a c", a=4), 0.125,
                           bc(bq8[0:64, 4 * b2:4 * b2 + 4], 2, [64, 4, 128]), ALU.mult, ALU.add, r=['ps%d' % b2] + CST, w=['qT'])
                kb.tt(kT[par][0:64, :, :], ps[0:64, 2, 0:256].rearrange("p (a c) -> p a c", a=2),
                      bc(bkc[0:64, 0:2], 2, [64, 2, 128]), ALU.add, r=['ps2'] + CST, w=[kc])
                kb.tt(vb[par][:], ps[:, 2, 256:384], bv_b[:], ALU.add, r=['ps2'] + CST, w=[vc])
                if STOP <= 2:
                    continue
                for h in range(8):
                    kv = h // 4
                    bk = 2 + h // 2
                    o_ = (h % 2) * 256
                    kprev = kT[par] if first else kT[1 - par]
                    kb.mm(ps[:, bk, o_:o_ + 128], qT[:, h, :], kprev[:, kv, :], r=['qT', kc, kp], w=['ps%d' % bk])
                    kb.mm(ps[:, bk, o_ + 128:o_ + 256], qT[:, h, :], kT[par][:, kv, :], r=['qT', kc], w=['ps%d' % bk])
                msk = mF if first else mPC
                for b2 in range(4):
                    kb.tt(Sm[:, 2 * b2:2 * b2 + 2, :], ps[:, 2 + b2, :].rearrange("p (a c) -> p a c", a=2),
                          bc(msk[:], 1, [128, 2, 256]), ALU.add, r=['ps%d' % (2 + b2)] + CST, w=['Sm'])
                if STOP <= 3:
                    continue
                kb.red(mx[:, 0:8], Sm[:], ALU.max, r=['Sm'], w=['mx'])
                kb.tt(mx[:, 0:8], mx[:, 0:8], snk[:, 0:8], ALU.max, r=['mx'] + CST, w=['mx'])
                kb.ts(nmx[:, 0:8], mx[:, 0:8], -1.0, ALU.mult, r=['mx'], w=['nmx'])
                for h in range(8):
                    kb.af(E[:, h, :], Sm[:, h, :], AF.Exp, bias=nmx[:, h:h + 1], accum=rs[:, h:h + 1], r=['Sm', 'nmx'], w=['E', 'rs'])
                kb.tt(esk[:, 0:8], snk[:, 0:8], mx[:, 0:8], ALU.subtract, r=['mx'] + CST, w=['esk'])
                kb.af(esk[:, 0:8], esk[:, 0:8], AF.Exp, r=['esk'], w=['esk'])
                kb.tt(rs[:, 0:8], rs[:, 0:8], esk[:, 0:8], ALU.add, r=['rs', 'esk'], w=['rs'])
                kb.recip(rs[:, 0:8], rs[:, 0:8], r=['rs'], w=['rs'])
                if STOP <= 4:
                    continue
                for hq in range(2):
                    for h in range(4 * hq, 4 * hq + 4):
                        for hf in range(2):
                            blk = h * 2 + hf
                            kb.tr(psT6[:, (blk % 8) * 128:(blk % 8 + 1) * 128], E[:, h, hf * 128:(hf + 1) * 128], idb[:],
                                  r=['E', 'idb'], w=['ps6'])
                    kb.cp(ET[:, hq * 1024:(hq + 1) * 1024], psT6[:, :], r=['ps6'], w=['ET'], eng=('act' if hq == 0 else 'dve'))
                for h in range(8):
                    kv = h // 4
                    kb.mm(ps[:, 0, h * 64:(h + 1) * 64], ET[:, (2 * h) * 128:(2 * h + 1) * 128], vb[1 - par][:, kv * 64:(kv + 1) * 64],
                          start=True, stop=False, r=['ET', vp], w=['ps0'])
                    kb.mm(ps[:, 0, h * 64:(h + 1) * 64], ET[:, (2 * h + 1) * 128:(2 * h + 2) * 128], vb[par][:, kv * 64:(kv + 1) * 64],
                          start=False, stop=True, r=['ET', vc], w=['ps0'])
                kb.tt(o[:].rearrange("p (h d) -> p h d", h=8), ps[:, 0, :].rearrange("p (h d) -> p h d", h=8),
                      bc(rs[:, 0:8], 2, [128, 8, 64]), ALU.mult, r=['ps0', 'rs'], w=['o'])
                kb.af(junk[:, 0:512], o[:], AF.Square, accum=ss[:, 3:4], r=['o'], w=['junk', 'ss3'])
                kb.af(ss[:, 4:5], ss[:, 3:4], AF.Sqrt, scale=1.0 / 512, bias=RMS_EPS, r=['ss3'], w=['ss4'])
                kb.recip(ss[:, 5:6], ss[:, 4:5], r=['ss4'], w=['ss5'])
                S.act(lambda e: e.mul(out=ycat[:, 512:1024], in_=o[:], mul=ss[:, 5:6]), r=['o', 'ss5'], w=['ycatB'])
                if STOP <= 5:
                    continue
                for cc in range(8):
                    kb.tr(psT6[:, cc * 128:(cc + 1) * 128], ycat[:, cc * 128:(cc + 1) * 128], idb[:],
                          r=['ycatA', 'ycatB', 'idb'], w=['ps6'])
                kb.cp(ycT[:], psT6[:, :], r=['ps6'], w=['ycT'])
                for n2 in range(2):
                    for cc in range(8):
                        kb.mm(ps[:, 2 + n2, :], ycT[:, cc * 128:(cc + 1) * 128], Wout[:, cc, n2 * 512:(n2 + 1) * 512],
                              start=(cc == 0), stop=(cc == 7), r=['ycT', 'Wout'], w=['ps%d' % (2 + n2)])
                kb.tt(xt[:, 0:512], xt[:, 0:512], ps[:, 2, :], ALU.add, r=['xt', 'ps2'], w=['xt'])
                kb.tt(xt[:, 512:1024], xt[:, 512:1024], ps[:, 3, :], ALU.add, r=['xt', 'ps3'], w=['xt'])
                kb.dma(x1[t0:t0 + 128, :], xt[:], r=['xt'], w=['x1'])
                par = 1 - par
                if prep_emit is not None:
                    want = ((tile_idx + 1) * len(prep_units)) // n_tiles_total
                    tile_ops = S.ops[tile_start:]
                    del S.ops[tile_start:]
                    saved = S.ops
                    S.ops = []
                    while prep_done < want:
                        prep_emit(*prep_units[prep_done])
                        prep_done += 1
                    pops = S.ops
                    S.ops = saved
                    ratio = max(1, len(tile_ops) // max(1, len(pops))) if pops else 0
                    pi = 0
                    for k_, op_ in enumerate(tile_ops):
                        S.ops.append(op_)
                        if pops and (k_ + 1) % ratio == 0 and pi < len(pops):
                            S.ops.append(pops[pi])
                            pi += 1
                    S.ops.extend(pops[pi:])
        if prep_emit is not None:
            while prep_done < len(prep_units):
                prep_emit(*prep_units[prep_done])
                prep_done += 1
        S.flush()


def make_prep(nc, kb, c, A, uS, vS, es, ps, banks):
    idf = c['idf']
    uv = A['peer_u'][0].rearrange("(i j) d -> i j d", j=128)
    vv = A['peer_v'][0].rearrange("(i j) d -> i j d", j=128)
    vSv = vS.rearrange("(i j) d -> i j d", j=128)
    sb = lambda name, shape, dt: es.enter_context(nc.sbuf_tensor(name, shape, dt))
    Uld = [sb('Uld%d' % i, [128, 4, 1024], F32) for i in range(2)]
    Vld = [sb('Vld%d' % i, [128, 4, 1024], F32) for i in range(2)]
    UT = [sb('UT%d' % i, [128, 4096], BF16) for i in range(2)]
    Vb = [sb('Vb%d' % i, [128, 4, 1024], BF16) for i in range(2)]
    nb = len(banks)

    def emit(jb, part):
        p = jb % 2
        if part == 0:
            kb.dma(Uld[p][:], uv[:, jb * 4:(jb + 1) * 4, :], w=['Uld%d' % p])
            kb.dma(Vld[p][:], vv[:, jb * 4:(jb + 1) * 4, :], w=['Vld%d' % p])
            for jj in range(4):
                for half in range(2):
                    bk = banks[(jj * 2 + half) % nb]
                    for q in range(4):
                        dc = half * 4 + q
                        kb.tr(ps[:, bk, q * 128:(q + 1) * 128], Uld[p][:, jj, dc * 128:(dc + 1) * 128], idf[:],
                              r=['Uld%d' % p, 'idf'], w=['ps%d' % bk])
                    o_ = (jj * 8 + half * 4) * 128
                    kb.cp(UT[p][:, o_:o_ + 512], ps[:, bk, :], r=['ps%d' % bk], w=['UT%d' % p],
                          eng=('act' if (nb > 1 and (jj + half) % 2 == 0) else 'dve') if nb > 1 else 'act')
            kb.dma(uS[jb], UT[p][:], r=['UT%d' % p], w=['uS'])
        else:
            kb.cp(Vb[p][:, 0:2, :], Vld[p][:, 0:2, :], r=['Vld%d' % p], w=['Vb%d' % p], eng='pool')
            kb.cp(Vb[p][:, 2:3, :], Vld[p][:, 2:3, :], r=['Vld%d' % p], w=['Vb%d' % p], eng='pool')
            kb.cp(Vb[p][:, 3:4, :], Vld[p][:, 3:4, :], r=['Vld%d' % p], w=['Vb%d' % p], eng='pool')
            kb.dma(vSv[:, jb * 4:(jb + 1) * 4, :], Vb[p][:], r=['Vb%d' % p], w=['vS'])
    return emit


def phase_prep(nc, kb, c, A, uS, vS):
    with ExitStack() as es:
        ps = es.enter_context(nc.psum_tensor('ps_p', [128, 8, 512], F32))
        emit = make_prep(nc, kb, c, A, uS, vS, es, ps, list(range(8)))
        for jb in range(32):
            emit(jb, 0)
            emit(jb, 1)
        kb.S.flush()


def phase_peer(nc, kb, c, NTOK, x1, A, uS, vS, out, TG=2):
    S = kb.S
    idf, idb, iot, pidx = c['idf'], c['idb'], c['iot'], c['pidx']
    NG = NTOK // (128 * TG)
    NTK = 128 * TG
    vSv = vS.rearrange("(i j) d -> i j d", j=128)
    with ExitStack() as es:
        sb = lambda name, shape, dt: es.enter_context(nc.sbuf_tensor(name, shape, dt))
        ps = es.enter_context(nc.psum_tensor('ps_c', [128, 8, 512], F32))
        wq = sb('wq', [128, 8, 2048], BF16)
        skT = sb('skT', [128, 16, 128], BF16)
        lnf_b = sb('lnf_b', [128, 1024], F32)
        g2c = sb('g2c', [128, 8], F32)
        iob = sb('iob', [128, 128], BF16)
        GG = sb('GG', [128, NTK, 128], BF16)
        h2T = [sb('h2T%d' % i, [128, 8, NTK], BF16) for i in range(2)]
        xt = [[sb('cxt%d_%d' % (i, t), [128, 1024], F32) for t in range(TG)] for i in range(2)]
        junk = sb('cjunk', [128, 1024], BF16)
        junk2 = sb('cjunk2', [128, 1024], BF16)
        xn = sb('cxn', [128, 1024], BF16)
        ss = sb('css', [128, 8], F32)
        ss2 = sb('css2', [128, 8], F32)
        NBUF = 3
        ut = [sb('ut%d' % i, [128, 2, 8, 128], BF16) for i in range(NBUF)]
        vt = [sb('vt%d' % i, [128, 2, 1024], BF16) for i in range(NBUF)]
        qTb = sb('qTb', [128, 16, 128], BF16)
        sc = sb('sc', [128, 16, 128], F32)
        scr = sb('scr', [128, 256], F32)
        tv = sb('tv', [128, 16, 16], F32)
        ti = sb('ti', [128, 16, 16], U32)
        tif = [sb('tif%d' % i, [128, 16, 16], F32) for i in range(TG)]
        cand = sb('cand', [128, 8, 256], F32)
        sel = cand[:].rearrange("p h (a b) -> p h a b", a=16)
        cv = [sb('cv%d' % i, [128, 8, 16], F32) for i in range(TG)]
        cpi = [sb('cpi%d' % i, [128, 8, 16], U32) for i in range(TG)]
        cu1 = sb('cu1', [128, 8, 16], U32)
        akf = sb('akf', [128, 8, 16], F32)
        bkf = sb('bkf', [128, 8, 16], F32)
        ge = sb('ge', [128, 8, 16], F32)
        gz = sb('gz', [128, 8], F32)
        idxi = sb('idxi', [128, 128], F32)
        idxj = sb('idxj', [128, 128], F32)
        gate = sb('gate', [128, 128], F32)
        slotT = sb('slotT', [128, TG, 3, 128], BF16)
        slotF = sb('slotF', [128, TG, 2, 128], F32)
        TB = 8
        Aoh = [sb('Aoh%d' % i, [128, TB, 128], BF16) for i in range(2)]
        Boh = [sb('Boh%d' % i, [128, TB, 128], BF16) for i in range(2)]
        ga = [sb('ga%d' % i, [128, NTK], BF16) for i in range(2)]
        coef = [sb('coef%d' % i, [128, NTK], BF16) for i in range(2)]
        fin = sb('fin', [128, 1024], F32)
        kb.dma(g2c[:, :], A['ln2_g'][0].rearrange("(c p) -> p c", p=128), w=['cst'], slow=True)
        kb.dma(lnf_b[:], A['lnf_g'].rearrange("(o d) -> o d", o=1).partition_broadcast(128), w=['cst'])
        kb.cp(iob[:], iot[:], r=['iot'], w=['cst'])
        stg = cand[:].rearrange("p a b -> p (a b)")
        for cc in range(8):
            kb.dma(stg, A['peer_wq'][0, cc * 128:(cc + 1) * 128, :], w=['cand'])
            kb.cp(wq[:, cc, :], stg, r=['cand'], w=['wq'], eng=('act' if cc % 2 == 0 else 'dve'))
        for grp in range(16):
            h, cx = grp // 2, grp % 2
            kb.dma(sc[:, grp, :], A['peer_subkeys'][0, h, cx], w=['sc'])
        for grp in range(16):
            kb.tr(ps[:, 4 + (grp // 4) % 2, (grp % 4) * 128:(grp % 4 + 1) * 128], sc[:, grp, :], idf[:], r=['sc', 'idf'],
                  w=['ps%d' % (4 + (grp // 4) % 2)])
            if grp % 4 == 3:
                g0 = grp - 3
                bk = 4 + (grp // 4) % 2
                kb.cp(skT[:, g0:g0 + 4, :], ps[:, bk, :].rearrange("p (a c) -> p a c", a=4), r=['ps%d' % bk], w=['skT'])
        CST = ['cst', 'wq', 'skT']
        psT = ps[:, 7, :].bitcast(BF16)

        def frontA(g):
            gp = g % 2
            hk = 'h2T%d' % gp
            for tl in range(TG):
                t0 = (g * TG + tl) * 128
                xk = 'xt%d_%d' % (gp, tl)
                xtt = xt[gp][tl]
                kb.dma(xtt[:], x1[t0:t0 + 128, :], r=['x1'], w=[xk])
                kb.af(junk[:], xtt[:], AF.Square, accum=ss[:, 0:1], r=[xk], w=['junk', 'ss'])
                kb.af(ss[:, 1:2], ss[:, 0:1], AF.Ln, scale=1.0 / D, bias=RMS_EPS, r=['ss'], w=['ss1'])
                kb.af(ss[:, 2:3], ss[:, 1:2], AF.Exp, scale=-0.5, r=['ss1'], w=['ss2'])
                (lambda xtt_, xk_: S.act(lambda e: e.mul(out=xn[:], in_=xtt_[:], mul=ss[:, 2:3]), r=[xk_, 'ss2'], w=['xn']))(xtt, xk)
                for cc in range(8):
                    kb.tr(psT[:, cc * 128:(cc + 1) * 128], xn[:, cc * 128:(cc + 1) * 128], idb[:], r=['xn', 'idb'], w=['ps7'])
                for cc in range(8):
                    (lambda cc_, tl_, gp_: S.act(lambda e: e.mul(out=h2T[gp_][:, cc_, tl_ * 128:(tl_ + 1) * 128],
                                                                in_=psT[:, cc_ * 128:(cc_ + 1) * 128], mul=g2c[:, cc_:cc_ + 1]),
                                                 r=['ps7'] + CST, w=[hk]))(cc, tl, gp)
                for qb in range(4):
                    for gi in range(4):
                        grp = qb * 4 + gi
                        for cc in range(8):
                            kb.mm(ps[:, 6, gi * 128:(gi + 1) * 128], wq[:, cc, grp * 128:(grp + 1) * 128],
                                  h2T[gp][:, cc, tl * 128:(tl + 1) * 128], start=(cc == 0), stop=(cc == 7), r=['wq', hk], w=['ps6'])
                    kb.cp(qTb[:, qb * 4:qb * 4 + 4, :], ps[:, 6, :].rearrange("p (a c) -> p a c", a=4), r=['ps6'], w=['qTb'],
                          eng='act')
                for qb in range(4):
                    bk = 6 + qb % 2
                    for gi in range(4):
                        grp = qb * 4 + gi
                        kb.mm(ps[:, bk, gi * 128:(gi + 1) * 128], qTb[:, grp, :], skT[:, grp, :], r=['qTb', 'skT'], w=['ps%d' % bk])
                    kb.cp(sc[:, qb * 4:qb * 4 + 4, :], ps[:, bk, :].rearrange("p (a c) -> p a c", a=4), r=['ps%d' % bk], w=['sc'],
                          eng='act')
                for grp in range(16):
                    S.dve(lambda e, grp=grp: e.max(out=tv[:, grp, 0:8], in_=sc[:, grp, :]), r=['sc'], w=['tv'])
                    S.dve(lambda e, grp=grp: e.max_index(out=ti[:, grp, 0:8], in_max=tv[:, grp, 0:8], in_values=sc[:, grp, :]),
                          r=['sc', 'tv'], w=['ti'])
                    S.dve(lambda e, grp=grp: e.match_replace(out=scr[:, 0:128], in_to_replace=tv[:, grp, 0:8], in_values=sc[:, grp, :],
                                                             imm_value=NEG), r=['sc', 'tv'], w=['scr'])
                    S.dve(lambda e, grp=grp: e.max(out=tv[:, grp, 8:16], in_=scr[:, 0:128]), r=['scr'], w=['tv'])
                    S.dve(lambda e, grp=grp: e.max_index(out=ti[:, grp, 8:16], in_max=tv[:, grp, 8:16], in_values=scr[:, 0:128]),
                          r=['scr', 'tv'], w=['ti'])
                kb.cp(tif[tl][:], ti[:], r=['ti'], w=['tif%d' % tl])
                tvv = tv[:].rearrange("p (h c) k -> p h c k", c=2)
                kb.tt(cand[:].rearrange("p h (a b) -> p h a b", a=16), bc(tvv[:, :, 0, :], 3, [128, 8, 16, 16]),
                      bc(tvv[:, :, 1, :], 2, [128, 8, 16, 16]), ALU.add, r=['tv'], w=['cand'])
                cvt, cpt = cv[tl], cpi[tl]
                ck, pk = 'cv%d' % tl, 'cpi%d' % tl
                for h in range(8):
                    S.dve(lambda e, h=h, cvt=cvt: e.max(out=cvt[:, h, 0:8], in_=cand[:, h, :]), r=['cand'], w=[ck])
                    S.dve(lambda e, h=h, cvt=cvt, cpt=cpt: e.max_index(out=cpt[:, h, 0:8], in_max=cvt[:, h, 0:8], in_values=cand[:, h, :]),
                          r=['cand', ck], w=[pk])
                    S.dve(lambda e, h=h, cvt=cvt: e.match_replace(out=scr[:, 0:256], in_to_replace=cvt[:, h, 0:8], in_values=cand[:, h, :],
                                                                  imm_value=NEG), r=['cand', ck], w=['scr'])
                    S.dve(lambda e, h=h, cvt=cvt: e.max(out=cvt[:, h, 8:16], in_=scr[:, 0:256]), r=['scr'], w=[ck])
                    S.dve(lambda e, h=h, cvt=cvt, cpt=cpt: e.max_index(out=cpt[:, h, 8:16], in_max=cvt[:, h, 8:16], in_values=scr[:, 0:256]),
                          r=['scr', ck], w=[pk])

        def frontB(g):
            for tl in range(TG):
                cvt, cpt = cv[tl], cpi[tl]
                ck, pk = 'cv%d' % tl, 'cpi%d' % tl
                tfv = tif[tl][:].rearrange("p (h c) k -> p h c k", c=2)
                kb.tt(ge[:], cvt[:], cvt[:, :, 0:1].broadcast_to([128, 8, 16]), ALU.subtract, r=[ck], w=['ge'])
                kb.af(ge[:], ge[:], AF.Exp, r=['ge'], w=['ge'])
                kb.red(gz[:, 0:8], ge[:], ALU.add, r=['ge'], w=['gz'])
                kb.recip(gz[:, 0:8], gz[:, 0:8], r=['gz'], w=['gz'])
                kb.tt(gate[:].rearrange("p (h k) -> p h k", h=8), ge[:], bc(gz[:, 0:8], 2, [128, 8, 16]), ALU.mult,
                      r=['ge', 'gz'], w=['gate'])
                S.dve(lambda e, cpt=cpt: e.tensor_single_scalar(out=cu1[:], in_=cpt[:], scalar=4, op=ALU.logical_shift_right), r=[pk], w=['cu1'])
                kb.cp(akf[:], cu1[:], r=['cu1'], w=['akf'])
                S.dve(lambda e, cpt=cpt: e.tensor_single_scalar(out=cu1[:], in_=cpt[:], scalar=15, op=ALU.bitwise_and), r=[pk, 'akf'], w=['cu1'])
                kb.cp(bkf[:], cu1[:], r=['cu1'], w=['bkf'])
                io16 = iot[:, 0:16].unsqueeze(1).unsqueeze(1).broadcast_to([128, 8, 16, 16])
                for (rk, cx, dst, dk) in ((akf, 0, idxi, 'idxi'), (bkf, 1, idxj, 'idxj')):
                    kb.tt(sel, io16, bc(rk[:], 3, [128, 8, 16, 16]), ALU.is_equal, r=['akf', 'bkf', 'iot', ck, pk], w=['cand'])
                    kb.tt(sel, sel, bc(tfv[:, :, cx, :], 2, [128, 8, 16, 16]), ALU.mult, r=['cand', 'tif%d' % tl], w=['cand'], eng='pool')
                    kb.red(dst[:].rearrange("p (h k) -> p h k", h=8), sel, ALU.add, r=['cand'], w=[dk])
                for q, (src, sk_) in enumerate(((idxi, 'idxi'), (idxj, 'idxj'), (gate, 'gate'))):
                    kb.tr(ps[:, 6, q * 128:(q + 1) * 128], src[:], idf[:], r=[sk_, 'idf'], w=['ps6'])
                kb.cp(slotT[:, tl, :, :], ps[:, 6, 0:384].rearrange("p (a c) -> p a c", a=3), r=['ps6'], w=['slotT'], eng='act')
                kb.cp(slotF[:, tl, :, :], ps[:, 6, 128:384].rearrange("p (a c) -> p a c", a=2), r=['ps6'], w=['slotF'], eng='act')

        def onehot(g):
            for tl in range(TG):
                for tb in range(128 // TB):
                    ts_ = slice(tb * TB, (tb + 1) * TB)
                    ob = tb % 2
                    Ao, Bo = Aoh[ob], Boh[ob]
                    ak_, bk_ = 'Aoh%d' % ob, 'Boh%d' % ob
                    io128 = iob[:].unsqueeze(1).broadcast_to([128, TB, 128])
                    kb.tt(Ao[:], io128, bc(slotT[:, tl, 0, ts_], 2, [128, TB, 128]), ALU.is_equal, r=['slotT'] + CST, w=[ak_])
                    bkeys = ['%s_%d' % (bk_, q_) for q_ in range(TB)]
                    for tt_ in range(TB):
                        tok = tb * TB + tt_
                        kb.stt(Bo[:, tt_, :], iob[:], slotF[:, tl, 0, tok:tok + 1], slotF[:, tl, 1, tok:tok + 1].broadcast_to([128, 128]),
                               ALU.is_equal, ALU.mult, r=['slotF'] + CST, w=[bkeys[tt_]])
                    for q4 in range(TB // 4):
                        bk = 4 + q4 % 2
                        for u in range(4):
                            tt_ = q4 * 4 + u
                            kb.mm(ps[:, bk, u * 128:(u + 1) * 128], Ao[:, tt_, :], Bo[:, tt_, :], r=[ak_, bkeys[tt_]], w=['ps%d' % bk])
                        tg0 = tl * 128 + tb * TB + q4 * 4
                        kb.cp(GG[:, tg0:tg0 + 4, :], ps[:, bk, :].rearrange("p (a c) -> p a c", a=4), r=['ps%d' % bk], w=['GG'],
                              eng='act')

        def experts(g, extra, extraB):
            gp = g % 2
            hk = 'h2T%d' % gp
            per = (len(extra) + PEER_A_STEPS - 1) // PEER_A_STEPS if extra else 0
            pos = [0]
            perB = (len(extraB) + 23) // 24 if extraB else 0
            posB = [0]

            def load(jh):
                p = jh % NBUF
                jb, half = jh // 2, jh % 2
                kb.dma(ut[p][:].rearrange("p a b c -> p (a b c)"), uS[jb][:, half * 2048:(half + 1) * 2048], r=['uS'], w=['ut%d' % p])
                kb.dma(vt[p][:], vSv[:, jh * 2:(jh + 1) * 2, :], r=['vS'], w=['vt%d' % p])

            def act(j):
                jh, jj = j // 2, j % 2
                p = jh % NBUF
                bkA = 4 + j % 2
                for dc in range(8):
                    kb.mm(ps[:, bkA, 0:NTK], ut[p][:, jj, dc, :], h2T[gp][:, dc, :], start=(dc == 0), stop=(dc == 7),
                          r=['ut%d' % p, hk], w=['ps%d' % bkA])

            load(0)
            load(1)
            act(0)
            for j in range(128):
                jh, jj = j // 2, j % 2
                p = jh % NBUF
                pa = j % 2
                bkA = 4 + pa
                if jj == 0 and jh + 2 < 64:
                    load(jh + 2)
                kb.af(ga[pa][:], ps[:, bkA, 0:NTK], AF.Gelu, r=['ps%d' % bkA], w=['ga%d' % pa])
                if j + 1 < 128:
                    act(j + 1)
                kb.tt(coef[pa][:], ga[pa][:], GG[:, :, j], ALU.mult, r=['ga%d' % pa, 'GG'], w=['coef%d' % pa], eng='pool')
                for tl in range(TG):
                    for n2 in range(2):
                        bkY = tl * 2 + n2
                        kb.mm(ps[:, bkY, :], coef[pa][:, tl * 128:(tl + 1) * 128], vt[p][:, jj, n2 * 512:(n2 + 1) * 512],
                              start=(j == 0), stop=(j == 127), r=['coef%d' % pa, 'vt%d' % p], w=['ps%d' % bkY])
                if extra and pos[0] < len(extra):
                    S.ops.extend(extra[pos[0]:pos[0] + per])
                    pos[0] += per
                if j >= PEER_B_START and extraB and posB[0] < len(extraB):
                    S.ops.extend(extraB[posB[0]:posB[0] + perB])
                    posB[0] += perB
            if extra and pos[0] < len(extra):
                S.ops.extend(extra[pos[0]:])
            if extraB and posB[0] < len(extraB):
                S.ops.extend(extraB[posB[0]:])

        def finish(g):
            gp = g % 2
            for tl in range(TG):
                t0 = (g * TG + tl) * 128
                xk = 'xt%d_%d' % (gp, tl)
                xtt = xt[gp][tl]
                kb.tt(fin[:, 0:512], xtt[:, 0:512], ps[:, tl * 2, :], ALU.add, r=[xk, 'ps%d' % (tl * 2)], w=['fin'])
                kb.tt(fin[:, 512:1024], xtt[:, 512:1024], ps[:, tl * 2 + 1, :], ALU.add, r=[xk, 'ps%d' % (tl * 2 + 1)], w=['fin'])
                kb.af(junk2[:], fin[:], AF.Square, accum=ss2[:, 3:4], r=['fin'], w=['junk2', 'ss3'])
                kb.af(ss2[:, 4:5], ss2[:, 3:4], AF.Sqrt, scale=1.0 / D, bias=RMS_EPS, r=['ss3'], w=['ss4'])
                kb.recip(ss2[:, 5:6], ss2[:, 4:5], r=['ss4'], w=['ss5'])
                kb.stt(fin[:], fin[:], ss2[:, 5:6], lnf_b[:], ALU.mult, ALU.mult, r=['fin', 'ss5'] + CST, w=['fin'])
                kb.dma(out[t0:t0 + 128, :], fin[:], r=['fin'], w=['out'])

        def cap(fn, *args):
            saved = S.ops
            S.ops = []
            fn(*args)
            got = S.ops
            S.ops = saved
            return got

        frontA(0)
        frontB(0)
        for g in range(NG):
            oh = cap(onehot, g)
            fn_ = cap(finish, g - 1) if g > 0 else []
            ratio = max(1, len(oh) // max(1, len(fn_))) if fn_ else 0
            pi = 0
            for k_, op_ in enumerate(oh):
                S.ops.append(op_)
                if fn_ and (k_ + 1) % ratio == 0 and pi < len(fn_):
                    S.ops.append(fn_[pi])
                    pi += 1
            S.ops.extend(fn_[pi:])
            extra, extraB = [], []
            if g + 1 < NG:
                saved = S.ops
                S.ops = []
                frontA(g + 1)
                extra = S.ops
                S.ops = []
                frontB(g + 1)
                extraB = S.ops
                S.ops = saved
            experts(g, extra, extraB)
        finish(NG - 1)
        S.flush()


NSEQ_CORE = 4
SEQ_LEN = 2048
N_CORES = 8


def kernel(**inputs):
    x = np.asarray(inputs["x"], dtype=np.float32)
    B, T, Dm = x.shape
    assert B == NSEQ_CORE * N_CORES and T == SEQ_LEN and Dm == D
    nc = build_program(NSEQ_CORE, SEQ_LEN)
    params = {k: np.ascontiguousarray(np.asarray(inputs[k], dtype=np.float32)) for k in PARAM_SHAPES}
    in_maps = []
    for c in range(N_CORES):
        m = dict(params)
        m["x"] = np.ascontiguousarray(x[c * NSEQ_CORE:(c + 1) * NSEQ_CORE].reshape(NSEQ_CORE * SEQ_LEN, D))
        in_maps.append(m)
    res = run_bass_kernel_spmd(nc, in_maps, core_ids=list(range(N_CORES)))
    outs = [np.asarray(r["out"], dtype=np.float32).reshape(NSEQ_CORE, SEQ_LEN, D) for r in res.results]
    return np.concatenate(outs, axis=0)
```

```python
from contextlib import ExitStack
import numpy as np
import concourse.bass as bass
import concourse.mybir as mybir

F32 = mybir.dt.float32
BF16 = mybir.dt.bfloat16
F32R = mybir.dt.float32r
U32 = mybir.dt.uint32
I32 = mybir.dt.int32
AF = mybir.ActivationFunctionType
ALU = mybir.AluOpType
AX = mybir.AxisListType

NSLOT = 16
PEER_A_STEPS = 80
PEER_B_START = 100
import os as _osx
SAME_ENGINE_INORDER = ('pe',)


class Sched:
    def __init__(self, nc, es):
        self.nc = nc
        self.engs = ['pe', 'act', 'dve', 'pool', 'sp']
        self.dq = ('sp', 'pool', 'act')
        self.esem = {e: es.enter_context(nc.semaphore('s_' + e)) for e in self.engs}
        self.dsem = {e: [es.enter_context(nc.semaphore('d_%s%d' % (e, s))) for s in range(NSLOT)]
                     for e in self.dq}
        self.cnt = {e: 0 for e in self.engs}
        self.dcnt = {e: 0 for e in self.dq}
        self.slot_uses = {e: [0] * NSLOT for e in self.dq}
        self.waited = {e: {} for e in self.engs}
        self.pending = None
        self.ops = []
        self.dummy = es.enter_context(nc.sbuf_tensor('sched_dummy', [128, 8], F32))
        self.n_emitted = 0

    def add(self, eng, fn, r=(), w=(), dma=False):
        self.ops.append(dict(eng=eng, fn=fn, r=tuple(r), w=tuple(w), dma=dma))

    def pe(self, fn, r=(), w=()):
        self.add('pe', fn, r, w)

    def act(self, fn, r=(), w=()):
        self.add('act', fn, r, w)

    def dve(self, fn, r=(), w=()):
        self.add('dve', fn, r, w)

    def pool(self, fn, r=(), w=()):
        self.add('pool', fn, r, w)

    def dma(self, fn, r=(), w=(), q='sp'):
        self.add(q, fn, r, w, dma=True)

    def flush(self):
        nc = self.nc
        dummy = self.dummy
        self.dve(lambda e: e.memset(dummy[:], 0.0), w=['__barrier__'])
        ops = self.ops
        self.ops = []
        self.n_emitted += len(ops)
        last_w = {}
        readers = {}
        for i, o in enumerate(ops):
            deps = set()
            for k in o['r']:
                if k in last_w:
                    deps.add(last_w[k])
            for k in o['w']:
                if k in last_w:
                    deps.add(last_w[k])
                deps.update(readers.get(k, ()))
            deps.discard(i)
            o['deps'] = deps
            for k in o['r']:
                readers.setdefault(k, []).append(i)
            for k in o['w']:
                last_w[k] = i
                readers[k] = []
        J = len(ops) - 1
        last_eng = {}
        for i, o in enumerate(ops[:-1]):
            if o['dma']:
                ops[J]['deps'].add(i)
            else:
                last_eng[o['eng']] = i
        ops[J]['deps'].update(last_eng.values())
        needed = {J}
        for o in ops:
            needed.update(o['deps'])
        slot_last = {e: [None] * NSLOT for e in self.dq}
        for i, o in enumerate(ops):
            e = o['eng']
            if o['dma']:
                s = self.dcnt[e] % NSLOT
                self.dcnt[e] += 1
                self.slot_uses[e][s] += 1
                o['sem'] = self.dsem[e][s]
                o['val'] = 16 * self.slot_uses[e][s]
                o['inc'] = 16
                if slot_last[e][s] is not None:
                    o['deps'].add(slot_last[e][s])
                slot_last[e][s] = i
            elif i in needed:
                self.cnt[e] += 1
                o['sem'] = self.esem[e]
                o['val'] = self.cnt[e]
                o['inc'] = 1
            else:
                o['sem'] = None
        per = {e: [] for e in self.engs}
        for i, o in enumerate(ops):
            per[o['eng']].append(i)
        pending = self.pending

        def run(ename, eng):
            waited = self.waited[ename]
            if pending is not None and ename != 'dve':
                if waited.get(id(pending[0]), 0) < pending[1]:
                    eng.wait_ge(pending[0], pending[1])
                    waited[id(pending[0])] = pending[1]
            for i in per[ename]:
                o = ops[i]
                need = {}
                for j in o['deps']:
                    p = ops[j]
                    if p['sem'] is None:
                        continue
                    if (not p['dma']) and p['eng'] == ename and ename in SAME_ENGINE_INORDER:
                        continue
                    key = id(p['sem'])
                    if key not in need or need[key][1] < p['val']:
                        need[key] = (p['sem'], p['val'])
                for key, (sem, val) in need.items():
                    if waited.get(key, 0) >= val:
                        continue
                    eng.wait_ge(sem, val)
                    waited[key] = val
                ins = o['fn'](eng)
                if o['sem'] is not None:
                    ins.then_inc(o['sem'], o['inc'])
            if ename in self.dsem:
                for s in range(NSLOT):
                    v = 16 * self.slot_uses[ename][s]
                    if v > 0 and waited.get(id(self.dsem[ename][s]), 0) < v:
                        eng.wait_ge(self.dsem[ename][s], v)
                        waited[id(self.dsem[ename][s])] = v

        with nc.Block() as block:
            @block.tensor
            def _(e):
                run('pe', e)

            @block.scalar
            def _(e):
                run('act', e)

            @block.vector
            def _(e):
                run('dve', e)

            @block.gpsimd
            def _(e):
                run('pool', e)

            @block.sync
            def _(e):
                run('sp', e)
        self.pending = (ops[J]['sem'], ops[J]['val'])

from concourse.bass_utils import run_bass_kernel_spmd

D = 1024
HD = 64
NEG = -1e30
C0 = -0.6065306597126334
RMS_EPS = 1e-6
LNX_EPS = 64e-5


class KB:
    def __init__(self, nc, S):
        self.nc = nc
        self.S = S

    def mm(self, out, lhsT, rhs, start=True, stop=True, r=(), w=()):
        self.S.pe(lambda e: e.matmul(out, lhsT=lhsT, rhs=rhs, start=start, stop=stop), r, w)

    def mmr(self, out, lhsT, rhs, start=True, stop=True, r=(), w=()):
        lt = lhsT if lhsT.dtype == F32R else lhsT.bitcast(F32R)
        rh = rhs if rhs.dtype == F32R else rhs.bitcast(F32R)
        self.S.pe(lambda e: e.matmul(out, lhsT=lt, rhs=rh, start=start, stop=stop), r, w)

    def tr(self, out, in_, ident, r=(), w=()):
        self.S.pe(lambda e: e.transpose(out, in_, ident), r, w)

    def tt(self, out, in0, in1, op, r=(), w=(), eng='dve'):
        self.S.add(eng, lambda e: e.tensor_tensor(out=out, in0=in0, in1=in1, op=op), r, w)

    def ts(self, out, in0, s1, op0, s2=None, op1=None, r=(), w=(), eng='dve'):
        if op1 is None:
            self.S.add(eng, lambda e: e.tensor_scalar(out=out, in0=in0, scalar1=s1, scalar2=None, op0=op0), r, w)
        else:
            self.S.add(eng, lambda e: e.tensor_scalar(out=out, in0=in0, scalar1=s1, scalar2=s2, op0=op0, op1=op1), r, w)

    def stt(self, out, in0, scalar, in1, op0, op1, r=(), w=(), eng='dve'):
        self.S.add(eng, lambda e: e.scalar_tensor_tensor(out=out, in0=in0, scalar=scalar, in1=in1, op0=op0, op1=op1), r, w)

    def af(self, out, in_, func, bias=None, scale=None, accum=None, r=(), w=()):
        kw = {}
        if bias is not None:
            kw['bias'] = bias
        if scale is not None:
            kw['scale'] = scale
        if accum is not None:
            kw['accum_out'] = accum
        self.S.act(lambda e: e.activation(out=out, in_=in_, func=func, **kw), r, w)

    def cp(self, out, in_, r=(), w=(), eng='dve'):
        if eng == 'act':
            self.S.act(lambda e: e.copy(out=out, in_=in_), r, w)
        else:
            self.S.add(eng, lambda e: e.tensor_copy(out=out, in_=in_), r, w)

    def red(self, out, in_, op, r=(), w=(), axis=AX.X):
        self.S.dve(lambda e: e.tensor_reduce(out=out, in_=in_, axis=axis, op=op), r, w)

    def recip(self, out, in_, r=(), w=()):
        self.S.dve(lambda e: e.reciprocal(out=out, in_=in_), r, w)

    def memset(self, ap, val, r=(), w=(), eng='dve'):
        self.S.add(eng, lambda e: e.memset(ap, val), r, w)

    def dma(self, out, in_, r=(), w=(), q='sp', slow=False):
        if slow:
            self.S.dma(lambda e: e.dma_start(out=out, in_=in_, allow_slow_non_contiguous=True), r, w, q=q)
        else:
            self.S.dma(lambda e: e.dma_start(out=out, in_=in_), r, w, q=q)


def bc(ap, axis, shape):
    return ap.unsqueeze(axis).broadcast_to(shape)


def setup_consts(nc, kb, es):
    sb = lambda name, shape, dt: es.enter_context(nc.sbuf_tensor(name, shape, dt))
    c = {}
    c['iot'] = sb('iot', [128, 128], F32)
    c['pidx'] = sb('pidx', [128, 1], F32)
    c['idf'] = sb('idf', [128, 128], F32)
    c['idb'] = sb('idb', [128, 128], BF16)
    iot, pidx, idf, idb = c['iot'], c['pidx'], c['idf'], c['idb']
    kb.S.pool(lambda e: e.iota(iot[:], pattern=[[1, 128]], base=0, channel_multiplier=0,
                               allow_small_or_imprecise_dtypes=True), w=['iot'])
    kb.S.pool(lambda e: e.iota(pidx[:], pattern=[[0, 1]], base=0, channel_multiplier=1,
                               allow_small_or_imprecise_dtypes=True), w=['pidx'])
    kb.ts(idf[:], iot[:], pidx[:, 0:1], ALU.is_equal, r=['iot', 'pidx'], w=['idf'])
    kb.cp(idb[:], idf[:], r=['idf'], w=['idb'])
    return c


def phase_rwkv(nc, kb, c, NSEQ, SEQ, x, A, yr):
    RDT = F32R
    S = kb.S
    NT = SEQ // 128
    idf, idb, iot, pidx = c['idf'], c['idb'], c['iot'], c['pidx']
    with ExitStack() as es:
        sb = lambda name, shape, dt: es.enter_context(nc.sbuf_tensor(name, shape, dt))
        ps = es.enter_context(nc.psum_tensor('ps_a', [128, 8, 512], F32))
        W1 = sb('W1', [128, 8, 1696], BF16)
        stage = [sb('stage0', [128, 1696], F32)] * 2
        g1c = sb('g1c', [128, 8], F32)
        w0_b = sb('w0_b', [128, 512], F32)
        a0_b = sb('a0_b', [128, 512], F32)
        kk_b = sb('kk_b', [128, 512], F32)
        ka_b = sb('ka_b', [128, 512], F32)
        lw_b = sb('lw_b', [128, 512], F32)
        lb_b = sb('lb_b', [128, 512], F32)
        rk_b = sb('rk_b', [128, 512], F32)
        mu_b = sb('mu_b', [128, 1536], F32)
        mucol = sb('mucol', [96, 3], F32)
        wup = sb('wup', [32, 512], F32)
        aup = sb('aup', [32, 512], F32)
        gup = sb('gup', [96, 512], F32)
        m_ab = sb('m_ab', [128, 256], F32)
        m_ls = sb('m_ls', [128, 256], F32)
        triI = sb('triI', [128, 128], F32)
        triE = sb('triE', [128, 128], F32)
        triT = sb('triT', [128, 128], F32)
        kb.dma(g1c[:, :], A['ln1_g'][0].rearrange("(c p) -> p c", p=128), w=['g1c'], slow=True)
        for cc in range(8):
            st = stage[cc % 2]
            kb.dma(st[:], A['w_in'][0, cc * 128:(cc + 1) * 128, 0:1696], w=['stage0'])
            (lambda st_, cc_: S.act(lambda e: e.mul(out=W1[:, cc_, :], in_=st_[:], mul=g1c[:, cc_:cc_ + 1]),
                                    r=['stage0', 'g1c'], w=['W1']))(st, cc)
        for t, name in ((w0_b, 'w0'), (a0_b, 'a0'), (kk_b, 'k_k'), (ka_b, 'k_a'), (lw_b, 'lnx_w'), (lb_b, 'lnx_b')):
            kb.dma(t[:], A[name][0:1, :].partition_broadcast(128), w=['cst'])
        kb.dma(rk_b[:], A['r_k'].rearrange("o h d -> o (h d)").partition_broadcast(128), w=['cst'])
        kb.dma(mu_b[:], A['mu_shift'][0:1, 0:1536].partition_broadcast(128), w=['cst'])
        kb.dma(mucol[0:32, 0:1], A['mu_shift'][0, 1536:1568].rearrange("(p o) -> p o", o=1), w=['cst'])
        kb.dma(mucol[0:32, 1:2], A['mu_shift'][0, 1568:1600].rearrange("(p o) -> p o", o=1), w=['cst'])
        kb.dma(mucol[0:96, 2:3], A['mu_shift'][0, 1600:1696].rearrange("(p o) -> p o", o=1), w=['cst'])
        kb.dma(wup[:], A['w_up'][0], w=['cst'])
        kb.dma(aup[:], A['a_up'][0], w=['cst'])
        kb.dma(gup[:], A['g_up'][0], w=['cst'])
        kb.ts(m_ab[:, 0:128], iot[:], pidx[:, 0:1], ALU.is_gt, r=['iot', 'pidx'], w=['cst'])
        kb.ts(m_ab[:, 128:256], iot[:], pidx[:, 0:1], ALU.is_ge, r=['iot', 'pidx'], w=['cst'])
        kb.ts(m_ls[:, 0:128], iot[:], pidx[:, 0:1], ALU.is_lt, r=['iot', 'pidx'], w=['cst'])
        kb.ts(m_ls[:, 128:256], iot[:], pidx[:, 0:1], ALU.is_lt, r=['iot', 'pidx'], w=['cst'])
        kb.ts(triE[:], m_ab[:, 0:128], C0, ALU.mult, r=['cst'], w=['cst2'])
        kb.ts(triI[:], m_ab[:, 128:256], C0, ALU.mult, r=['cst'], w=['cst2'])
        kb.memset(triT[:], C0, w=['cst2'])
        CST = ['cst', 'cst2']
        xt = sb('xt', [128, 1024], F32)
        xn = sb('xn', [128, 1024], BF16)
        hT = sb('hT', [128, 1024], BF16)
        ss = sb('ss', [128, 4], F32)
        rkv2 = [sb('rkv%d' % i, [128, 1536], RDT) for i in range(2)]
        pprev = sb('pprev', [128, 1536], F32)
        lastrow = sb('lastrow', [1, 1536], F32)
        lin = sb('lin', [96, 3, 129], F32)
        ldf = sb('ldf', [96, 3, 128], F32)
        lsh = sb('lsh', [96, 3, 128], F32)
        lact = sb('lact', [96, 2, 128], F32)
        tmpA = sb('tmpA', [128, 512], F32)
        Etot = tmpA
        tmpB = sb('tmpB', [128, 512], F32)
        sgw = sb('sgw', [128, 512], F32)
        a_t = sb('a_t', [128, 512], F32)
        g_t2 = [sb('g_t%d' % i, [128, 512], F32) for i in range(2)]
        kk = sb('kk', [128, 512], F32)
        b_t = sb('b_t', [128, 512], F32)
        TM2 = [sb('TM%d' % i, [128, 4, 512], RDT) for i in range(2)]
        Kh2 = [sb('Kh%d' % i, [128, 512], F32) for i in range(2)]
        Bh2 = [sb('Bh%d' % i, [128, 512], F32) for i in range(2)]
        ex0 = sb('ex0', [128, 512], F32)
        ex1 = sb('ex1', [128, 512], F32)
        n2 = sb('n2', [128, 8], F32)
        FT = sb('FT', [128, 4, 512], RDT)
        gC2 = [sb('gC%d' % i, [64, 8], F32) for i in range(2)]
        tmpP = sb('tmpP', [128, 512], F32)
        Gb2 = [sb('Gb%d' % i, [128, 4, 320], RDT) for i in range(2)]
        Grk2 = [sb('Grk%d' % i, [128, 4, 128], F32) for i in range(2)]
        RTs = sb('RTs', [64, 4, 128], F32)
        GT2 = [sb('GT%d' % i, [128, 4, 256], RDT) for i in range(2)]
        Nn = [sb('Nn%d' % i, [128, 4, 128], BF16) for i in range(2)]
        NTb = [sb('NTb%d' % i, [128, 4, 128], BF16) for i in range(2)]
        NTn = [sb('NTn%d' % i, [128, 4, 128], RDT) for i in range(2)]
        P2 = [sb('P%d' % i, [128, 4, 128], RDT) for i in range(2)]
        ZT = sb('ZT', [128, 4, 192], RDT)
        MN1 = sb('MN1', [128, 4, 192], RDT)
        MN2 = sb('MN2', [128, 4, 192], RDT)
        S0T = [sb('S0T%d' % i, [128, 512], RDT) for i in range(2)]
        y = sb('y', [128, 512], F32)
        yo = sb('yo', [128, 512], BF16)
        st1 = sb('st1', [128, 8], F32)
        st2 = sb('st2', [128, 8], F32)
        st3 = sb('st3', [128, 8], F32)
        def zero_r(ap2d, n, key, rows=128):
            kb.ts(ap2d, idf[0:rows, 0:1].broadcast_to([rows, n]), 0.0, ALU.mult, r=['idf'], w=[key])
        zero_r(FT[:].rearrange("p a b -> p (a b)"), 2048, 'FT')
        kb.memset(lin[:], 0.0, w=['lin', 'lin0'])
        zero_r(MN1[:].rearrange("p a b -> p (a b)"), 768, 'MN1')
        zero_r(S0T[0][:], 512, 'S0T0')
        zero_r(S0T[1][:], 512, 'S0T1')
        fv = lambda ap: (ap.bitcast(F32) if ap.dtype == F32R else ap)
        v3h = lambda ap: ap.rearrange("p (h d) -> p h d", h=8)
        FTf = fv(FT[:])
        psT = ps[:, 6, :].bitcast(BF16)
        tiles = [(s_, i_) for s_ in range(NSEQ) for i_ in range(NT)]

        def prep(n):
            s_, i = tiles[n]
            pb = n % 2
            t0 = n * 128
            first = (i == 0)
            rkv, g_t, TM, Kh, Bh, gC = rkv2[pb], g_t2[pb], TM2[pb], Kh2[pb], Bh2[pb], gC2[pb]
            RK, GK, TK, KK, BK, CK = 'rkv%d' % pb, 'g_t%d' % pb, 'TM%d' % pb, 'Kh%d' % pb, 'Bh%d' % pb, 'gC%d' % pb
            rkvf = fv(rkv[:])
            r_ = rkvf[:, 0:512]
            k_ = rkvf[:, 512:1024]
            k_w = rkv[:, 512:1024]
            kb.dma(xt[:], x[t0:t0 + 128, :], w=['xt'])
            kb.af(xn[:], xt[:], AF.Square, accum=ss[:, 0:1], r=['xt'], w=['xn', 'ss'])
            kb.af(ss[:, 1:2], ss[:, 0:1], AF.Ln, scale=1.0 / D, bias=RMS_EPS, r=['ss'], w=['ss1'])
            kb.af(ss[:, 2:3], ss[:, 1:2], AF.Exp, scale=-0.5, r=['ss1'], w=['ss2'])
            S.act(lambda e: e.mul(out=xn[:], in_=xt[:], mul=ss[:, 2:3]), r=['xt', 'ss2'], w=['xn'])
            for cc in range(8):
                kb.tr(psT[:, cc * 128:(cc + 1) * 128], xn[:, cc * 128:(cc + 1) * 128], idb[:], r=['xn', 'idb'], w=['ps6'])
            kb.cp(hT[:], psT[:, :], r=['ps6'], w=['hT'], eng='act')
            for j in range(3):
                bk = 7 if j % 2 == 0 else 6
                for cc in range(8):
                    kb.mm(ps[:, bk, :], hT[:, cc * 128:(cc + 1) * 128], W1[:, cc, j * 512:(j + 1) * 512],
                          start=(cc == 0), stop=(cc == 7), r=['hT', 'W1'], w=['ps%d' % bk])
                kb.cp(rkv[:, j * 512:(j + 1) * 512], ps[:, bk, :], r=['ps%d' % bk], w=[RK], eng=('act' if j != 1 else 'dve'))
            for (lo, nn, col) in ((1536, 32, 0), (1568, 32, 128), (1600, 96, 256)):
                for cc in range(8):
                    kb.mm(ps[0:nn, 6, col:col + 128], W1[:, cc, lo:lo + nn], hT[:, cc * 128:(cc + 1) * 128],
                          start=(cc == 0), stop=(cc == 7), r=['hT', 'W1'], w=['ps6'])
            if first:
                kb.memset(pprev[0:1, :], 0.0, w=['pprev0'])
            else:
                kb.dma(pprev[0:1, :], lastrow[0:1, :], r=['lastrow'], w=['pprev0'])
            kb.dma(pprev[1:128, :], rkvf[0:127, :], r=[RK], w=['pprevR'])
            kb.dma(lastrow[0:1, :], rkvf[127:128, :], r=[RK], w=['lastrow'])
            kb.tt(pprev[:], pprev[:], rkvf, ALU.subtract, r=['pprev0', 'pprevR', RK], w=['pprev0', 'pprevR'])
            kb.tt(pprev[:], pprev[:], mu_b[:], ALU.mult, r=['pprev0', 'pprevR'] + CST, w=['pprev0', 'pprevR'], eng='pool')
            kb.tt(rkv[:], rkvf, pprev[:], ALU.add, r=['pprev0', 'pprevR', RK], w=[RK])
            if first:
                kb.memset(lin[:, :, 0:1], 0.0, w=['lin0'])
            else:
                kb.cp(lin[:, :, 0:1], lin[:, :, 128:129], r=['lin'], w=['lin0'])
            kb.cp(lin[0:32, 0, 1:129], ps[0:32, 6, 0:128], r=['ps6', 'lin0'], w=['lin'], eng='act')
            kb.cp(lin[0:32, 1, 1:129], ps[0:32, 6, 128:256], r=['ps6', 'lin0'], w=['lin'], eng='act')
            kb.cp(lin[0:96, 2, 1:129], ps[0:96, 6, 256:384], r=['ps6', 'lin0'], w=['lin'], eng='act')
            for l, nn in ((0, 32), (1, 32), (2, 96)):
                kb.tt(ldf[0:nn, l, :], lin[0:nn, l, 0:128], lin[0:nn, l, 1:129], ALU.subtract, r=['lin', 'lin0'], w=['ldf'])
                kb.stt(lsh[0:nn, l, :], ldf[0:nn, l, :], mucol[0:nn, l:l + 1], lin[0:nn, l, 1:129], ALU.mult, ALU.add,
                       r=['ldf', 'lin'] + CST, w=['lsh'])
            kb.af(lact[0:32, 0, :], lsh[0:32, 0, :], AF.Tanh, r=['lsh'], w=['lact'])
            kb.af(lact[0:96, 1, :], lsh[0:96, 2, :], AF.Sigmoid, r=['lsh'], w=['lact'])
            kb.mm(ps[:, 7, :], lact[0:32, 0, :], wup[0:32, :], r=['lact'] + CST, w=['ps7'])
            kb.tt(tmpA[:], ps[:, 7, :], w0_b[:], ALU.add, r=['ps7'] + CST, w=['tmpA'])
            kb.mm(ps[:, 6, :], lsh[0:32, 1, :], aup[0:32, :], r=['lsh'] + CST, w=['ps6'])
            kb.tt(tmpB[:], ps[:, 6, :], a0_b[:], ALU.add, r=['ps6'] + CST, w=['tmpB'])
            kb.mm(ps[:, 7, :], lact[0:96, 1, :], gup[0:96, :], r=['lact'] + CST, w=['ps7'])
            kb.cp(g_t[:], ps[:, 7, :], r=['ps7'], w=[GK], eng='act')
            kb.af(sgw[:], tmpA[:], AF.Sigmoid, r=['tmpA'], w=['sgw'])
            kb.af(a_t[:], tmpB[:], AF.Sigmoid, r=['tmpB'], w=['a_t'])
            kb.tt(kk[:], k_, kk_b[:], ALU.mult, r=[RK] + CST, w=['kk'])
            kb.tt(tmpA[:], kk[:], kk[:], ALU.mult, r=['kk'], w=['tmpA'], eng='pool')
            kb.red(n2[:, 0:8], v3h(tmpA[:]), ALU.add, r=['tmpA'], w=['n2'])
            kb.af(n2[:, 0:8], n2[:, 0:8], AF.Sqrt, r=['n2'], w=['n2'])
            kb.ts(n2[:, 0:8], n2[:, 0:8], 1e-12, ALU.max, r=['n2'], w=['n2'])
            kb.recip(n2[:, 0:8], n2[:, 0:8], r=['n2'], w=['n2'])
            kb.tt(v3h(kk[:]), v3h(kk[:]), bc(n2[:, 0:8], 2, [128, 8, 64]), ALU.mult, r=['kk', 'n2'], w=['kk'])
            kb.stt(tmpB[:], a_t[:], -1.0, ka_b[:], ALU.add, ALU.mult, r=['a_t'] + CST, w=['tmpB'])
            kb.stt(k_w, tmpB[:], 1.0, k_, ALU.add, ALU.mult, r=['tmpB', RK], w=[RK])
            kb.tt(b_t[:], kk[:], a_t[:], ALU.mult, r=['kk', 'a_t'], w=['b_t'], eng='pool')
            kb.mm(ps[:, 6, :], triI[:], sgw[:], r=['sgw'] + CST, w=['ps6'])
            kb.mm(ps[:, 7, :], triE[:], sgw[:], r=['sgw'] + CST, w=['ps7'])
            kb.af(ex0[:], ps[:, 6, :], AF.Exp, r=['ps6'], w=['ex0'])
            kb.tt(TM[:, 1, :], r_, ex0[:], ALU.mult, r=[RK, 'ex0'], w=[TK])
            kb.af(ex1[:], ps[:, 7, :], AF.Exp, r=['ps7'], w=['ex1'])
            kb.stt(TM[:, 0, :], kk[:], -1.0, ex1[:], ALU.mult, ALU.mult, r=['kk', 'ex1'], w=[TK])
            kb.af(ex0[:], ps[:, 6, :], AF.Exp, scale=-1.0, r=['ps6'], w=['ex0'])
            kb.mm(ps[:, 7, :], triT[:], sgw[:], r=['sgw'] + CST, w=['ps7'])
            kb.tt(TM[:, 2, :], b_t[:], ex0[:], ALU.mult, r=['b_t', 'ex0'], w=[TK])
            kb.tt(TM[:, 3, :], k_, ex0[:], ALU.mult, r=[RK, 'ex0'], w=[TK])
            kb.af(Etot[:], ps[:, 7, :], AF.Exp, r=['ps7'], w=['tmpA'])
            kb.tt(ex1[:], Etot[:], ex0[:], ALU.mult, r=['tmpA', 'ex0'], w=['ex1'], eng='pool')
            kb.tt(Kh[:], k_, ex1[:], ALU.mult, r=[RK, 'ex1'], w=[KK], eng='pool')
            kb.tt(Bh[:], b_t[:], ex1[:], ALU.mult, r=['b_t', 'ex1'], w=[BK])
            for h in range(8):
                kb.tr(ps[0:64, 6, h * 32:(h + 1) * 32], Etot[0:32, h * 64:(h + 1) * 64], idf[0:32, 0:32],
                      r=['tmpA', 'idf'], w=['ps6'])
            kb.cp(gC[0:64, 0:8], ps[0:64, 6, 0:256].rearrange("p (h c) -> p h c", c=32)[:, :, 0], r=['ps6'], w=[CK], eng='act')

        state = {'cur': 0}

        def heavy(n):
            s_, i = tiles[n]
            pb = n % 2
            t0 = n * 128
            cur = state['cur']
            rkv, g_t, TM, Kh, Bh, gC = rkv2[pb], g_t2[pb], TM2[pb], Kh2[pb], Bh2[pb], gC2[pb]
            RK, GK, TK, KK, BK, CK = 'rkv%d' % pb, 'g_t%d' % pb, 'TM%d' % pb, 'Kh%d' % pb, 'Bh%d' % pb, 'gC%d' % pb
            rkvf = fv(rkv[:])
            r_ = rkvf[:, 0:512]
            k_ = rkvf[:, 512:1024]
            v_ = rkvf[:, 1024:1536]
            TMf = fv(TM[:])
            nxt = 1 - cur
            for g in range(2):
                Gb, GT, Grk, P = Gb2[g], GT2[g], Grk2[g], P2[g]
                GBK, GTK, GRK, PK = 'Gb%d' % g, 'GT%d' % g, 'Grk%d' % g, 'P%d' % g
                for hh in range(4):
                    h = 4 * g + hh
                    for q in range(4):
                        kb.tr(ps[0:64, 5, q * 128:(q + 1) * 128], TMf[:, q, h * 64:(h + 1) * 64], idf[:],
                              r=[TK, 'idf'], w=['ps5'])
                    kb.cp(FT[0:64, hh, :], ps[0:64, 5, :], r=['ps5'], w=['FT'], eng=('act' if hh % 2 == 0 else 'dve'))
                for hh in range(4):
                    kb.mmr(ps[:, hh // 2, (hh % 2) * 256:(hh % 2) * 256 + 256], FT[:, hh, 256:384], FT[:, hh, 0:256],
                           r=['FT'], w=['ps%d' % (hh // 2)])
                    kb.mmr(ps[:, 2, hh * 128:(hh + 1) * 128], FT[:, hh, 384:512], FT[:, hh, 128:256], r=['FT'], w=['ps2'])
                    kb.mmr(ps[:, 3 + hh // 2, (hh % 2) * 256:(hh % 2) * 256 + 256], FT[:, hh, 0:128], FT[:, hh, 256:512],
                           r=['FT'], w=['ps%d' % (3 + hh // 2)])
                if g == 0:
                    kb.cp(RTs[0:64, :, :], FTf[0:64, :, 128:256], r=['FT'], w=['RTs'])
                for b2 in range(2):
                    kb.tt(Gb[:, 2 * b2:2 * b2 + 2, 0:256], ps[:, b2, :].rearrange("p (a c) -> p a c", a=2),
                          bc(m_ab[:], 1, [128, 2, 256]), ALU.mult, r=['ps%d' % b2] + CST, w=[GBK])
                    kb.tt(GT[:, 2 * b2:2 * b2 + 2, :], ps[:, 3 + b2, :].rearrange("p (a c) -> p a c", a=2),
                          bc(m_ls[:], 1, [128, 2, 256]), ALU.mult, r=['ps%d' % (3 + b2)] + CST, w=[GTK])
                kb.tt(Grk[:], ps[:, 2, :].rearrange("p (a c) -> p a c", a=4), bc(m_ab[:, 128:256], 1, [128, 4, 128]),
                      ALU.mult, r=['ps2'] + CST, w=[GRK])
                kb.cp(Gb[:, :, 256:320], Bh[:, g * 256:(g + 1) * 256].rearrange("p (a c) -> p a c", a=4), r=[BK], w=[GBK])
                kb.tt(P[:], fv(Gb[:])[:, :, 0:128], bc(idf[:], 1, [128, 4, 128]), ALU.add, r=[GBK, 'idf'], w=[PK])
            for it in range(6):
                for g in range(2):
                    Gb, GT, P = Gb2[g], GT2[g], P2[g]
                    PK = 'P%d' % g
                    bN, bT, bP = 3 * g, 3 * g + 1, 3 * g + 2
                    if it == 0:
                        Ncur = (lambda Gb_: (lambda hh: Gb_[:, hh, 0:128]))(Gb)
                        NTcur = (lambda GT_: (lambda hh: GT_[:, hh, 0:128]))(GT)
                        nk, ntk = 'Gb%d' % g, 'GT%d' % g
                    else:
                        Ncur = (lambda g_: (lambda hh: Nn[g_][:, hh, :]))(g)
                        NTcur = (lambda g_: (lambda hh: NTb[g_][:, hh, :]))(g)
                        nk, ntk = 'Nn%d' % g, 'NTb%d' % g
                    mmf = kb.mmr if it == 0 else kb.mm
                    if it < 5:
                        for hh in range(4):
                            mmf(ps[:, bN, hh * 128:(hh + 1) * 128], NTcur(hh), Ncur(hh), r=[nk, ntk], w=['ps%d' % bN])
                    for hh in range(4):
                        mmf(ps[:, bT, hh * 128:(hh + 1) * 128], Ncur(hh), NTcur(hh), r=[nk, ntk], w=['ps%d' % bT])
                    if it < 5:
                        kb.cp(Nn[g][:], ps[:, bN, :].rearrange("p (a c) -> p a c", a=4), r=['ps%d' % bN], w=['Nn%d' % g], eng='act')
                        kb.cp(NTb[g][:], ps[:, bT, :].rearrange("p (a c) -> p a c", a=4), r=['ps%d' % bT], w=['NTb%d' % g], eng='act')
                    kb.cp(NTn[g][:], ps[:, bT, :].rearrange("p (a c) -> p a c", a=4), r=['ps%d' % bT, 'NTb%d' % g], w=['NTn%d' % g], eng='act')
                    for hh in range(4):
                        kb.mmr(ps[:, bP, hh * 128:(hh + 1) * 128], NTn[g][:, hh, :], P[:, hh, :], r=['NTn%d' % g, PK], w=['ps%d' % bP])
                    kb.tt(P[:], fv(P[:]), ps[:, bP, :].rearrange("p (a c) -> p a c", a=4), ALU.add, r=[PK, 'ps%d' % bP], w=[PK])
            for g in range(2):
                Gb, GT, Grk, P = Gb2[g], GT2[g], Grk2[g], P2[g]
                GBK, GTK, GRK, PK = 'Gb%d' % g, 'GT%d' % g, 'Grk%d' % g, 'P%d' % g
                for hh in range(4):
                    h = 4 * g + hh
                    o = (hh % 2) * 192
                    kb.mmr(ps[:, hh // 2, o:o + 64], P[:, hh, :], TM[:, 0, h * 64:(h + 1) * 64], r=[PK, TK], w=['ps%d' % (hh // 2)])
                    kb.mmr(ps[:, hh // 2, o + 64:o + 192], P[:, hh, :], GT[:, hh, 128:256], r=[PK, GTK], w=['ps%d' % (hh // 2)])
                for b2 in range(2):
                    kb.cp(ZT[:, 2 * b2:2 * b2 + 2, :], ps[:, b2, 0:384].rearrange("p (a c) -> p a c", a=2),
                          r=['ps%d' % b2], w=['ZT'], eng=('act' if b2 == 0 else 'dve'))
                for hh in range(4):
                    o = (hh % 2) * 192
                    kb.mmr(ps[0:64, 3 + hh // 2, o:o + 192], ZT[:, hh, 0:64], Gb[:, hh, 128:320], r=['ZT', GBK], w=['ps%d' % (3 + hh // 2)])
                for hh in range(4):
                    o = (hh % 2) * 192
                    kb.mmr(ps[:, hh // 2, o:o + 192], ZT[:, hh, 64:192], Gb[:, hh, 128:320], r=['ZT', GBK], w=['ps%d' % (hh // 2)])
                for b2 in range(2):
                    pv1 = ps[0:64, 3 + b2, 0:384].rearrange("p (a c) -> p a c", a=2)
                    rsrc = RTs[0:64, 2 * b2:2 * b2 + 2, :] if g == 0 else FTf[0:64, 2 * b2:2 * b2 + 2, 128:256]
                    kb.tt(MN1[0:64, 2 * b2:2 * b2 + 2, 0:128], pv1[:, :, 0:128], rsrc, ALU.add,
                          r=['ps%d' % (3 + b2), 'FT', 'RTs'], w=['MN1'])
                    for a2 in range(2):
                        hh = 2 * b2 + a2
                        h = 4 * g + hh
                        kb.stt(MN1[0:64, hh, 128:192], idf[0:64, 0:64], gC[0:64, h:h + 1], ps[0:64, 3 + b2, a2 * 192 + 128:a2 * 192 + 192],
                               ALU.mult, ALU.add, r=['ps%d' % (3 + b2), CK, 'idf'], w=['MN1'])
                    pv2 = ps[:, b2, 0:384].rearrange("p (a c) -> p a c", a=2)
                    kb.tt(MN2[:, 2 * b2:2 * b2 + 2, 0:128], pv2[:, :, 0:128], Grk[:, 2 * b2:2 * b2 + 2, :], ALU.add,
                          r=['ps%d' % b2, GRK], w=['MN2'])
                    h0 = 4 * g + 2 * b2
                    kb.tt(MN2[:, 2 * b2:2 * b2 + 2, 128:192], pv2[:, :, 128:192],
                          Kh[:, h0 * 64:(h0 + 2) * 64].rearrange("p (a c) -> p a c", a=2), ALU.add,
                          r=['ps%d' % b2, KK], w=['MN2'])
                sk = 'S0T%d' % cur
                for hh in range(4):
                    h = 4 * g + hh
                    hs = slice(h * 64, (h + 1) * 64)
                    ys_ = slice(256 + hh * 64, 256 + (hh + 1) * 64)
                    ss_ = slice(hh * 64, (hh + 1) * 64)
                    kb.mmr(ps[:, 2, ys_], MN1[:, hh, 0:128], S0T[cur][:, hs], start=True, stop=False, r=['MN1', sk], w=['ps2'])
                    kb.mmr(ps[:, 2, ys_], MN2[:, hh, 0:128], rkv[:, 1024 + h * 64:1024 + (h + 1) * 64], start=False, stop=True,
                           r=['MN2', RK], w=['ps2'])
                    kb.mmr(ps[0:64, 2, ss_], MN1[:, hh, 128:192], S0T[cur][:, hs], start=True, stop=False, r=['MN1', sk], w=['ps2'])
                    kb.mmr(ps[0:64, 2, ss_], MN2[:, hh, 128:192], rkv[:, 1024 + h * 64:1024 + (h + 1) * 64], start=False, stop=True,
                           r=['MN2', RK], w=['ps2'])
                if i == NT - 1:
                    zero_r(S0T[nxt][0:64, g * 256:(g + 1) * 256], 256, 'S0T%d' % nxt, rows=64)
                else:
                    kb.cp(S0T[nxt][0:64, g * 256:(g + 1) * 256], ps[0:64, 2, 0:256], r=['ps2'], w=['S0T%d' % nxt], eng='act')
                kb.cp(y[:, g * 256:(g + 1) * 256], ps[:, 2, 256:512], r=['ps2'], w=['y'], eng='act')
            state['cur'] = nxt
            kb.red(st1[:, 0:8], v3h(y[:]), ALU.add, r=['y'], w=['st1'])
            kb.tt(tmpP[:], y[:], y[:], ALU.mult, r=['y'], w=['tmpP'], eng='pool')
            kb.red(st2[:, 0:8], v3h(tmpP[:]), ALU.add, r=['tmpP'], w=['st2'])
            kb.ts(st1[:, 0:8], st1[:, 0:8], 1.0 / 64, ALU.mult, r=['st1'], w=['st1'])
            kb.tt(st3[:, 0:8], st1[:, 0:8], st1[:, 0:8], ALU.mult, r=['st1'], w=['st3'])
            kb.stt(st2[:, 0:8], st2[:, 0:8], 1.0 / 64, st3[:, 0:8], ALU.mult, ALU.subtract, r=['st2', 'st3'], w=['st2'])
            kb.af(st2[:, 0:8], st2[:, 0:8], AF.Sqrt, bias=LNX_EPS, r=['st2'], w=['st2'])
            kb.recip(st2[:, 0:8], st2[:, 0:8], r=['st2'], w=['st2'])
            kb.tt(v3h(y[:]), v3h(y[:]), bc(st1[:, 0:8], 2, [128, 8, 64]), ALU.subtract, r=['y', 'st1'], w=['y'])
            kb.tt(v3h(y[:]), v3h(y[:]), bc(st2[:, 0:8], 2, [128, 8, 64]), ALU.mult, r=['y', 'st2'], w=['y'])
            kb.tt(y[:], y[:], lw_b[:], ALU.mult, r=['y'] + CST, w=['y'])
            kb.tt(y[:], y[:], lb_b[:], ALU.add, r=['y'] + CST, w=['y'])
            kb.tt(tmpP[:], r_, k_, ALU.mult, r=[RK], w=['tmpP'], eng='pool')
            kb.tt(tmpP[:], tmpP[:], rk_b[:], ALU.mult, r=['tmpP'] + CST, w=['tmpP'], eng='pool')
            kb.red(st3[:, 0:8], v3h(tmpP[:]), ALU.add, r=['tmpP'], w=['st3'])
            kb.tt(v3h(tmpP[:]), v3h(v_), bc(st3[:, 0:8], 2, [128, 8, 64]), ALU.mult, r=[RK, 'st3'], w=['tmpP'])
            kb.tt(y[:], y[:], tmpP[:], ALU.add, r=['y', 'tmpP'], w=['y'])
            kb.tt(yo[:], y[:], g_t[:], ALU.mult, r=['y', GK], w=['yo'])
            kb.dma(yr[t0:t0 + 128, :], yo[:], r=['yo'], w=['yr'])

        def capture(fn, n):
            saved = S.ops
            S.ops = []
            fn(n)
            got = S.ops
            S.ops = saved
            return got

        prep(0)
        for n in range(len(tiles)):
            hv = capture(heavy, n)
            pr = capture(prep, n + 1) if n + 1 < len(tiles) else []
            ratio = (len(hv) // max(1, len(pr))) if pr else 0
            pi = 0
            for k, op_ in enumerate(hv):
                S.ops.append(op_)
                if pr and ratio > 0 and (k + 1) % ratio == 0 and pi < len(pr):
                    S.ops.append(pr[pi])
                    pi += 1
            S.ops.extend(pr[pi:])
        S.flush()


PARAM_SHAPES = {
    "ln1_g": [1, 1024], "w_in": [1, 1024, 2464], "b_attn": [1, 768], "mu_shift": [1, 1696],
    "w0": [1, 512], "w_up": [1, 32, 512], "a0": [1, 512], "a_up": [1, 32, 512], "g_up": [1, 96, 512],
    "k_k": [1, 512], "k_a": [1, 512], "r_k": [1, 8, 64], "lnx_w": [1, 512], "lnx_b": [1, 512],
    "attn_sinks": [1, 8], "attn_norm_g": [1, 512], "w_out": [1, 1024, 1024], "ln2_g": [1, 1024],
    "peer_wq": [1, 1024, 2048], "peer_subkeys": [1, 8, 2, 128, 128], "peer_u": [1, 16384, 1024],
    "peer_v": [1, 16384, 1024], "lnf_g": [1024],
}


def build_program(NSEQ, SEQ, phases=('prep', 'rwkv', 'attn', 'peer'), dbg=False, x1_in=False):
    nc = bass.Bass("TRN2", target_bir_lowering=False)
    NTOK = NSEQ * SEQ
    x = nc.dram_tensor("x", [NTOK, D], F32, kind="ExternalInput").ap()
    A = {k: nc.dram_tensor(k, s, F32, kind="ExternalInput").ap() for k, s in PARAM_SHAPES.items()}
    out = nc.dram_tensor("out", [NTOK, D], F32, kind="ExternalOutput").ap()
    sk = "ExternalOutput" if dbg else "Internal"
    yr = nc.dram_tensor("yr", [NTOK, 512], BF16, kind=sk).ap()
    x1 = nc.dram_tensor("x1", [NTOK, D], F32, kind=("ExternalInput" if x1_in else sk)).ap()
    uS = nc.dram_tensor("uS", [32, 128, 4 * 8 * 128], BF16, kind="Internal").ap()
    vS = nc.dram_tensor("vS", [16384, D], BF16, kind="Internal").ap()
    with ExitStack() as es:
        S = Sched(nc, es)
        kb = KB(nc, S)
        c = setup_consts(nc, kb, es)
        fused_prep = ('prep' in phases) and ('attn' in phases)
        if 'prep' in phases and not fused_prep:
            phase_prep(nc, kb, c, A, uS, vS)
        if 'rwkv' in phases:
            phase_rwkv(nc, kb, c, NSEQ, SEQ, x, A, yr)
        if 'attn' in phases:
            phase_attn(nc, kb, c, NSEQ, SEQ, x, A, yr, x1, prep=((uS, vS) if fused_prep else None))
        if 'peer' in phases:
            phase_peer(nc, kb, c, NTOK, x1, A, uS, vS, out)
        print("ops emitted:", S.n_emitted)
    return nc


def phase_attn(nc, kb, c, NSEQ, SEQ, x, A, yr, x1, prep=None):
    S = kb.S
    NT = SEQ // 128
    idf, idb, iot, pidx = c['idf'], c['idb'], c['iot'], c['pidx']
    with ExitStack() as es:
        sb = lambda name, shape, dt: es.enter_context(nc.sbuf_tensor(name, shape, dt))
        ps = es.enter_context(nc.psum_tensor('ps_b', [128, 8, 512], F32))
        Wat = sb('Wat', [128, 8, 768], BF16)
        Wout = sb('Wout', [128, 8, 1024], BF16)
        stage = [sb('bstage%d' % i, [128, 1024], F32) for i in range(2)]
        g1c = sb('g1cb', [128, 8], F32)
        gan = sb('gan', [128, 4], F32)
        bq8 = sb('bq8', [64, 8], F32)
        bkc = sb('bkc', [64, 2], F32)
        bv_b = sb('bv_b', [128, 128], F32)
        snk = sb('snk', [128, 8], F32)
        mPC = sb('mPC', [128, 256], F32)
        mF = sb('mF', [128, 256], F32)
        kb.dma(g1c[:, :], A['ln1_g'][0].rearrange("(c p) -> p c", p=128), w=['cst'], slow=True)
        kb.dma(gan[:, :], A['attn_norm_g'][0].rearrange("(c p) -> p c", p=128), w=['cst'], slow=True)
        kb.dma(bq8[:, :], A['b_attn'][0, 0:512].rearrange("(h d) -> d h", d=64), w=['cst'], slow=True)
        kb.dma(bkc[:, :], A['b_attn'][0, 512:640].rearrange("(h d) -> d h", d=64), w=['cst'], slow=True)
        kb.dma(bv_b[:], A['b_attn'][0:1, 640:768].partition_broadcast(128), w=['cst'])
        kb.dma(snk[:], A['attn_sinks'][0:1, :].partition_broadcast(128), w=['cst'])
        kb.ts(bq8[:], bq8[:], 0.125, ALU.mult, r=['cst'], w=['cst2'])
        for cc in range(8):
            st = stage[cc % 2]
            sk_ = 'bstage%d' % (cc % 2)
            kb.dma(st[:, 0:768], A['w_in'][0, cc * 128:(cc + 1) * 128, 1696:2464], w=[sk_])
            (lambda st_, cc_, sk__: S.act(lambda e: e.mul(out=Wat[:, cc_, :], in_=st_[:, 0:768], mul=g1c[:, cc_:cc_ + 1]),
                                          r=[sk__, 'cst'], w=['Wat']))(st, cc, sk_)
        for cc in range(8):
            st = stage[cc % 2]
            sk_ = 'bstage%d' % (cc % 2)
            kb.dma(st[:, :], A['w_out'][0, cc * 128:(cc + 1) * 128, :], w=[sk_])
            if cc < 4:
                kb.cp(Wout[:, cc, :], st[:, :], r=[sk_], w=['Wout'], eng='act')
            else:
                (lambda st_, cc_, sk__: S.act(lambda e: e.mul(out=Wout[:, cc_, :], in_=st_[:, :], mul=gan[:, cc_ - 4:cc_ - 3]),
                                              r=[sk__, 'cst'], w=['Wout']))(st, cc, sk_)
        kb.ts(mPC[:, 0:128], iot[:], pidx[:, 0:1], ALU.is_gt, r=['iot', 'pidx'], w=['cst3'])
        kb.ts(mPC[:, 128:256], iot[:], pidx[:, 0:1], ALU.is_le, r=['iot', 'pidx'], w=['cst3'])
        kb.ts(mPC[:], mPC[:], 1e30, ALU.mult, -1e30, ALU.add, r=['cst3'], w=['cst4'])
        kb.memset(mF[:, 0:128], NEG, w=['cst5'])
        kb.cp(mF[:, 128:256], mPC[:, 128:256], r=['cst4'], w=['cst5'])
        CST = ['cst', 'cst2', 'cst4', 'cst5']
        xt = sb('bxt', [128, 1024], F32)
        junk = sb('bjunk', [128, 1024], BF16)
        xn = sb('bxn', [128, 1024], BF16)
        hT = sb('bhT', [128, 1024], BF16)
        ss = sb('bss', [128, 8], F32)
        qT = sb('qT', [128, 8, 128], BF16)
        kT = [sb('kT%d' % i, [128, 2, 128], BF16) for i in range(2)]
        kb.memset(qT[:], 0.0, w=['qT'])
        vb = [sb('vb%d' % i, [128, 128], BF16) for i in range(2)]
        Sm = sb('Sm', [128, 8, 256], F32)
        E = sb('E', [128, 8, 256], BF16)
        ET = sb('ET', [128, 2048], BF16)
        mx = sb('mx', [128, 8], F32)
        nmx = sb('nmx', [128, 8], F32)
        rs = sb('rs', [128, 8], F32)
        esk = sb('esk', [128, 8], F32)
        o = sb('o', [128, 512], F32)
        ycat = sb('ycat', [128, 1024], BF16)
        ycT = sb('ycT', [128, 1024], BF16)
        for i in range(2):
            kb.memset(kT[i][:], 0.0, w=['kT%d' % i])
            kb.memset(vb[i][:], 0.0, w=['vb%d' % i])
        psT6 = ps[:, 6, :].bitcast(BF16)
        prep_emit = make_prep(nc, kb, c, A, prep[0], prep[1], es, ps, [7]) if prep is not None else None
        n_tiles_total = NSEQ * NT
        prep_units = [(jb, part) for jb in range(32) for part in range(2)] if prep is not None else []
        prep_done = 0
        par = 0
        STOP = 99
        for s in range(NSEQ):
            for i in range(NT):
                if STOP <= 1:
                    continue
                tile_idx = s * NT + i
                tile_start = len(S.ops)
                t0 = (s * NT + i) * 128
                first = (i == 0)
                kc, kp = 'kT%d' % par, 'kT%d' % (1 - par)
                vc, vp = 'vb%d' % par, 'vb%d' % (1 - par)
                kb.dma(xt[:], x[t0:t0 + 128, :], w=['xt'])
                kb.dma(ycat[:, 0:512], yr[t0:t0 + 128, :], r=['yr'], w=['ycatA'])
                kb.af(junk[:], xt[:], AF.Square, accum=ss[:, 0:1], r=['xt'], w=['junk', 'ss'])
                kb.af(ss[:, 1:2], ss[:, 0:1], AF.Sqrt, scale=1.0 / D, bias=RMS_EPS, r=['ss'], w=['ss1'])
                kb.recip(ss[:, 2:3], ss[:, 1:2], r=['ss1'], w=['ss2'])
                S.act(lambda e: e.mul(out=xn[:], in_=xt[:], mul=ss[:, 2:3]), r=['xt', 'ss2'], w=['xn'])
                for cc in range(8):
                    kb.tr(psT6[:, cc * 128:(cc + 1) * 128], xn[:, cc * 128:(cc + 1) * 128], idb[:], r=['xn', 'idb'], w=['ps6'])
                kb.cp(hT[:], psT6[:, :], r=['ps6'], w=['hT'])
                for h in range(8):
                    for cc in range(8):
                        kb.mm(ps[0:64, h // 4, (h % 4) * 128:(h % 4 + 1) * 128], Wat[:, cc, h * 64:(h + 1) * 64],
                              hT[:, cc * 128:(cc + 1) * 128], start=(cc == 0), stop=(cc == 7), r=['hT', 'Wat'], w=['ps%d' % (h // 4)])
                for kv in range(2):
                    for cc in range(8):
                        kb.mm(ps[0:64, 2, kv * 128:(kv + 1) * 128], Wat[:, cc, 512 + kv * 64:512 + (kv + 1) * 64],
                              hT[:, cc * 128:(cc + 1) * 128], start=(cc == 0), stop=(cc == 7), r=['hT', 'Wat'], w=['ps2'])
                for cc in range(8):
                    kb.mm(ps[:, 2, 256:384], hT[:, cc * 128:(cc + 1) * 128], Wat[:, cc, 640:768],
                          start=(cc == 0), stop=(cc == 7), r=['hT', 'Wat'], w=['ps2'])
                for b2 in range(2):
                    kb.stt(qT[0:64, 4 * b2:4 * b2 + 4, :], ps[0:64, b2, :].rearrange("p (a c) -> p a c", a=4), 0.125,
                           bc(bq8[0:64, 4 * b2:4 * b2 + 4], 2, [64, 4, 128]), ALU.mult, ALU.add, r=['ps%d' % b2] + CST, w=['qT'])
                kb.tt(kT[par][0:64, :, :], ps[0:64, 2, 0:256].rearrange("p (a c) -> p a c", a=2),
                      bc(bkc[0:64, 0:2], 2, [64, 2, 128]), ALU.add, r=['ps2'] + CST, w=[kc])
                kb.tt(vb[par][:], ps[:, 2, 256:384], bv_b[:], ALU.add, r=['ps2'] + CST, w=[vc])
                if STOP <= 2:
                    continue
                for h in range(8):
                    kv = h // 4
                    bk = 2 + h // 2
                    o_ = (h % 2) * 256
                    kprev = kT[par] if first else kT[1 - par]
                    kb.mm(ps[:, bk, o_:o_ + 128], qT[:, h, :], kprev[:, kv, :], r=['qT', kc, kp], w=['ps%d' % bk])
                    kb.mm(ps[:, bk, o_ + 128:o_ + 256], qT[:, h, :], kT[par][:, kv, :], r=['qT', kc], w=['ps%d' % bk])
                msk = mF if first else mPC
                for b2 in range(4):
                    kb.tt(Sm[:, 2 * b2:2 * b2 + 2, :], ps[:, 2 + b2, :].rearrange("p (a c) -> p a c", a=2),
                          bc(msk[:], 1, [128, 2, 256]), ALU.add, r=['ps%d' % (2 + b2)] + CST, w=['Sm'])
                if STOP <= 3:
                    continue
                kb.red(mx[:, 0:8], Sm[:], ALU.max, r=['Sm'], w=['mx'])
                kb.tt(mx[:, 0:8], mx[:, 0:8], snk[:, 0:8], ALU.max, r=['mx'] + CST, w=['mx'])
                kb.ts(nmx[:, 0:8], mx[:, 0:8], -1.0, ALU.mult, r=['mx'], w=['nmx'])
                for h in range(8):
                    kb.af(E[:, h, :], Sm[:, h, :], AF.Exp, bias=nmx[:, h:h + 1], accum=rs[:, h:h + 1], r=['Sm', 'nmx'], w=['E', 'rs'])
                kb.tt(esk[:, 0:8], snk[:, 0:8], mx[:, 0:8], ALU.subtract, r=['mx'] + CST, w=['esk'])
                kb.af(esk[:, 0:8], esk[:, 0:8], AF.Exp, r=['esk'], w=['esk'])
                kb.tt(rs[:, 0:8], rs[:, 0:8], esk[:, 0:8], ALU.add, r=['rs', 'esk'], w=['rs'])
                kb.recip(rs[:, 0:8], rs[:, 0:8], r=['rs'], w=['rs'])
                if STOP <= 4:
                    continue
                for hq in range(2):
                    for h in range(4 * hq, 4 * hq + 4):
                        for hf in range(2):
                            blk = h * 2 + hf
                            kb.tr(psT6[:, (blk % 8) * 128:(blk % 8 + 1) * 128], E[:, h, hf * 128:(hf + 1) * 128], idb[:],
                                  r=['E', 'idb'], w=['ps6'])
                    kb.cp(ET[:, hq * 1024:(hq + 1) * 1024], psT6[:, :], r=['ps6'], w=['ET'], eng=('act' if hq == 0 else 'dve'))
                for h in range(8):
                    kv = h // 4
                    kb.mm(ps[:, 0, h * 64:(h + 1) * 64], ET[:, (2 * h) * 128:(2 * h + 1) * 128], vb[1 - par][:, kv * 64:(kv + 1) * 64],
                          start=True, stop=False, r=['ET', vp], w=['ps0'])
                    kb.mm(ps[:, 0, h * 64:(h + 1) * 64], ET[:, (2 * h + 1) * 128:(2 * h + 2) * 128], vb[par][:, kv * 64:(kv + 1) * 64],
                          start=False, stop=True, r=['ET', vc], w=['ps0'])
                kb.tt(o[:].rearrange("p (h d) -> p h d", h=8), ps[:, 0, :].rearrange("p (h d) -> p h d", h=8),
                      bc(rs[:, 0:8], 2, [128, 8, 64]), ALU.mult, r=['ps0', 'rs'], w=['o'])
                kb.af(junk[:, 0:512], o[:], AF.Square, accum=ss[:, 3:4], r=['o'], w=['junk', 'ss3'])
                kb.af(ss[:, 4:5], ss[:, 3:4], AF.Sqrt, scale=1.0 / 512, bias=RMS_EPS, r=['ss3'], w=['ss4'])
                kb.recip(ss[:, 5:6], ss[:, 4:5], r=['ss4'], w=['ss5'])
                S.act(lambda e: e.mul(out=ycat[:, 512:1024], in_=o[:], mul=ss[:, 5:6]), r=['o', 'ss5'], w=['ycatB'])
                if STOP <= 5:
                    continue
                for cc in range(8):
                    kb.tr(psT6[:, cc * 128:(cc + 1) * 128], ycat[:, cc * 128:(cc + 1) * 128], idb[:],
                          r=['ycatA', 'ycatB', 'idb'], w=['ps6'])
                kb.cp(ycT[:], psT6[:, :], r=['ps6'], w=['ycT'])
                for n2 in range(2):
                    for cc in range(8):
                        kb.mm(ps[:, 2 + n2, :], ycT[:, cc * 128:(cc + 1) * 128], Wout[:, cc, n2 * 512:(n2 + 1) * 512],
                              start=(cc == 0), stop=(cc == 7), r=['ycT', 'Wout'], w=['ps%d' % (2 + n2)])
                kb.tt(xt[:, 0:512], xt[:, 0:512], ps[:, 2, :], ALU.add, r=['xt', 'ps2'], w=['xt'])
                kb.tt(xt[:, 512:1024], xt[:, 512:1024], ps[:, 3, :], ALU.add, r=['xt', 'ps3'], w=['xt'])
                kb.dma(x1[t0:t0 + 128, :], xt[:], r=['xt'], w=['x1'])
                par = 1 - par
                if prep_emit is not None:
                    want = ((tile_idx + 1) * len(prep_units)) // n_tiles_total
                    tile_ops = S.ops[tile_start:]
                    del S.ops[tile_start:]
                    saved = S.ops
                    S.ops = []
                    while prep_done < want:
                        prep_emit(*prep_units[prep_done])
                        prep_done += 1
                    pops = S.ops
                    S.ops = saved
                    ratio = max(1, len(tile_ops) // max(1, len(pops))) if pops else 0
                    pi = 0
                    for k_, op_ in enumerate(tile_ops):
                        S.ops.append(op_)
                        if pops and (k_ + 1) % ratio == 0 and pi < len(pops):
                            S.ops.append(pops[pi])
                            pi += 1
                    S.ops.extend(pops[pi:])
        if prep_emit is not None:
            while prep_done < len(prep_units):
                prep_emit(*prep_units[prep_done])
                prep_done += 1
        S.flush()


def make_prep(nc, kb, c, A, uS, vS, es, ps, banks):
    idf = c['idf']
    uv = A['peer_u'][0].rearrange("(i j) d -> i j d", j=128)
    vv = A['peer_v'][0].rearrange("(i j) d -> i j d", j=128)
    vSv = vS.rearrange("(i j) d -> i j d", j=128)
    sb = lambda name, shape, dt: es.enter_context(nc.sbuf_tensor(name, shape, dt))
    Uld = [sb('Uld%d' % i, [128, 4, 1024], F32) for i in range(2)]
    Vld = [sb('Vld%d' % i, [128, 4, 1024], F32) for i in range(2)]
    UT = [sb('UT%d' % i, [128, 4096], BF16) for i in range(2)]
    Vb = [sb('Vb%d' % i, [128, 4, 1024], BF16) for i in range(2)]
    nb = len(banks)

    def emit(jb, part):
        p = jb % 2
        if part == 0:
            kb.dma(Uld[p][:], uv[:, jb * 4:(jb + 1) * 4, :], w=['Uld%d' % p])
            kb.dma(Vld[p][:], vv[:, jb * 4:(jb + 1) * 4, :], w=['Vld%d' % p])
            for jj in range(4):
                for half in range(2):
                    bk = banks[(jj * 2 + half) % nb]
                    for q in range(4):
                        dc = half * 4 + q
                        kb.tr(ps[:, bk, q * 128:(q + 1) * 128], Uld[p][:, jj, dc * 128:(dc + 1) * 128], idf[:],
                              r=['Uld%d' % p, 'idf'], w=['ps%d' % bk])
                    o_ = (jj * 8 + half * 4) * 128
                    kb.cp(UT[p][:, o_:o_ + 512], ps[:, bk, :], r=['ps%d' % bk], w=['UT%d' % p],
                          eng=('act' if (nb > 1 and (jj + half) % 2 == 0) else 'dve') if nb > 1 else 'act')
            kb.dma(uS[jb], UT[p][:], r=['UT%d' % p], w=['uS'])
        else:
            kb.cp(Vb[p][:, 0:2, :], Vld[p][:, 0:2, :], r=['Vld%d' % p], w=['Vb%d' % p], eng='pool')
            kb.cp(Vb[p][:, 2:3, :], Vld[p][:, 2:3, :], r=['Vld%d' % p], w=['Vb%d' % p], eng='pool')
            kb.cp(Vb[p][:, 3:4, :], Vld[p][:, 3:4, :], r=['Vld%d' % p], w=['Vb%d' % p], eng='pool')
            kb.dma(vSv[:, jb * 4:(jb + 1) * 4, :], Vb[p][:], r=['Vb%d' % p], w=['vS'])
    return emit


def phase_prep(nc, kb, c, A, uS, vS):
    with ExitStack() as es:
        ps = es.enter_context(nc.psum_tensor('ps_p', [128, 8, 512], F32))
        emit = make_prep(nc, kb, c, A, uS, vS, es, ps, list(range(8)))
        for jb in range(32):
            emit(jb, 0)
            emit(jb, 1)
        kb.S.flush()


def phase_peer(nc, kb, c, NTOK, x1, A, uS, vS, out, TG=2):
    S = kb.S
    idf, idb, iot, pidx = c['idf'], c['idb'], c['iot'], c['pidx']
    NG = NTOK // (128 * TG)
    NTK = 128 * TG
    vSv = vS.rearrange("(i j) d -> i j d", j=128)
    with ExitStack() as es:
        sb = lambda name, shape, dt: es.enter_context(nc.sbuf_tensor(name, shape, dt))
        ps = es.enter_context(nc.psum_tensor('ps_c', [128, 8, 512], F32))
        wq = sb('wq', [128, 8, 2048], BF16)
        skT = sb('skT', [128, 16, 128], BF16)
        lnf_b = sb('lnf_b', [128, 1024], F32)
        g2c = sb('g2c', [128, 8], F32)
        iob = sb('iob', [128, 128], BF16)
        GG = sb('GG', [128, NTK, 128], BF16)
        h2T = [sb('h2T%d' % i, [128, 8, NTK], BF16) for i in range(2)]
        xt = [[sb('cxt%d_%d' % (i, t), [128, 1024], F32) for t in range(TG)] for i in range(2)]
        junk = sb('cjunk', [128, 1024], BF16)
        junk2 = sb('cjunk2', [128, 1024], BF16)
        xn = sb('cxn', [128, 1024], BF16)
        ss = sb('css', [128, 8], F32)
        ss2 = sb('css2', [128, 8], F32)
        NBUF = 3
        ut = [sb('ut%d' % i, [128, 2, 8, 128], BF16) for i in range(NBUF)]
        vt = [sb('vt%d' % i, [128, 2, 1024], BF16) for i in range(NBUF)]
        qTb = sb('qTb', [128, 16, 128], BF16)
        sc = sb('sc', [128, 16, 128], F32)
        scr = sb('scr', [128, 256], F32)
        tv = sb('tv', [128, 16, 16], F32)
        ti = sb('ti', [128, 16, 16], U32)
        tif = [sb('tif%d' % i, [128, 16, 16], F32) for i in range(TG)]
        cand = sb('cand', [128, 8, 256], F32)
        sel = cand[:].rearrange("p h (a b) -> p h a b", a=16)
        cv = [sb('cv%d' % i, [128, 8, 16], F32) for i in range(TG)]
        cpi = [sb('cpi%d' % i, [128, 8, 16], U32) for i in range(TG)]
        cu1 = sb('cu1', [128, 8, 16], U32)
        akf = sb('akf', [128, 8, 16], F32)
        bkf = sb('bkf', [128, 8, 16], F32)
        ge = sb('ge', [128, 8, 16], F32)
        gz = sb('gz', [128, 8], F32)
        idxi = sb('idxi', [128, 128], F32)
        idxj = sb('idxj', [128, 128], F32)
        gate = sb('gate', [128, 128], F32)
        slotT = sb('slotT', [128, TG, 3, 128], BF16)
        slotF = sb('slotF', [128, TG, 2, 128], F32)
        TB = 8
        Aoh = [sb('Aoh%d' % i, [128, TB, 128], BF16) for i in range(2)]
        Boh = [sb('Boh%d' % i, [128, TB, 128], BF16) for i in range(2)]
        ga = [sb('ga%d' % i, [128, NTK], BF16) for i in range(2)]
        coef = [sb('coef%d' % i, [128, NTK], BF16) for i in range(2)]
        fin = sb('fin', [128, 1024], F32)
        kb.dma(g2c[:, :], A['ln2_g'][0].rearrange("(c p) -> p c", p=128), w=['cst'], slow=True)
        kb.dma(lnf_b[:], A['lnf_g'].rearrange("(o d) -> o d", o=1).partition_broadcast(128), w=['cst'])
        kb.cp(iob[:], iot[:], r=['iot'], w=['cst'])
        stg = cand[:].rearrange("p a b -> p (a b)")
        for cc in range(8):
            kb.dma(stg, A['peer_wq'][0, cc * 128:(cc + 1) * 128, :], w=['cand'])
            kb.cp(wq[:, cc, :], stg, r=['cand'], w=['wq'], eng=('act' if cc % 2 == 0 else 'dve'))
        for grp in range(16):
            h, cx = grp // 2, grp % 2
            kb.dma(sc[:, grp, :], A['peer_subkeys'][0, h, cx], w=['sc'])
        for grp in range(16):
            kb.tr(ps[:, 4 + (grp // 4) % 2, (grp % 4) * 128:(grp % 4 + 1) * 128], sc[:, grp, :], idf[:], r=['sc', 'idf'],
                  w=['ps%d' % (4 + (grp // 4) % 2)])
            if grp % 4 == 3:
                g0 = grp - 3
                bk = 4 + (grp // 4) % 2
                kb.cp(skT[:, g0:g0 + 4, :], ps[:, bk, :].rearrange("p (a c) -> p a c", a=4), r=['ps%d' % bk], w=['skT'])
        CST = ['cst', 'wq', 'skT']
        psT = ps[:, 7, :].bitcast(BF16)

        def frontA(g):
            gp = g % 2
            hk = 'h2T%d' % gp
            for tl in range(TG):
                t0 = (g * TG + tl) * 128
                xk = 'xt%d_%d' % (gp, tl)
                xtt = xt[gp][tl]
                kb.dma(xtt[:], x1[t0:t0 + 128, :], r=['x1'], w=[xk])
                kb.af(junk[:], xtt[:], AF.Square, accum=ss[:, 0:1], r=[xk], w=['junk', 'ss'])
                kb.af(ss[:, 1:2], ss[:, 0:1], AF.Ln, scale=1.0 / D, bias=RMS_EPS, r=['ss'], w=['ss1'])
                kb.af(ss[:, 2:3], ss[:, 1:2], AF.Exp, scale=-0.5, r=['ss1'], w=['ss2'])
                (lambda xtt_, xk_: S.act(lambda e: e.mul(out=xn[:], in_=xtt_[:], mul=ss[:, 2:3]), r=[xk_, 'ss2'], w=['xn']))(xtt, xk)
                for cc in range(8):
                    kb.tr(psT[:, cc * 128:(cc + 1) * 128], xn[:, cc * 128:(cc + 1) * 128], idb[:], r=['xn', 'idb'], w=['ps7'])
                for cc in range(8):
                    (lambda cc_, tl_, gp_: S.act(lambda e: e.mul(out=h2T[gp_][:, cc_, tl_ * 128:(tl_ + 1) * 128],
                                                                in_=psT[:, cc_ * 128:(cc_ + 1) * 128], mul=g2c[:, cc_:cc_ + 1]),
                                                 r=['ps7'] + CST, w=[hk]))(cc, tl, gp)
                for qb in range(4):
                    for gi in range(4):
                        grp = qb * 4 + gi
                        for cc in range(8):
                            kb.mm(ps[:, 6, gi * 128:(gi + 1) * 128], wq[:, cc, grp * 128:(grp + 1) * 128],
                                  h2T[gp][:, cc, tl * 128:(tl + 1) * 128], start=(cc == 0), stop=(cc == 7), r=['wq', hk], w=['ps6'])
                    kb.cp(qTb[:, qb * 4:qb * 4 + 4, :], ps[:, 6, :].rearrange("p (a c) -> p a c", a=4), r=['ps6'], w=['qTb'],
                          eng='act')
                for qb in range(4):
                    bk = 6 + qb % 2
                    for gi in range(4):
                        grp = qb * 4 + gi
                        kb.mm(ps[:, bk, gi * 128:(gi + 1) * 128], qTb[:, grp, :], skT[:, grp, :], r=['qTb', 'skT'], w=['ps%d' % bk])
                    kb.cp(sc[:, qb * 4:qb * 4 + 4, :], ps[:, bk, :].rearrange("p (a c) -> p a c", a=4), r=['ps%d' % bk], w=['sc'],
                          eng='act')
                for grp in range(16):
                    S.dve(lambda e, grp=grp: e.max(out=tv[:, grp, 0:8], in_=sc[:, grp, :]), r=['sc'], w=['tv'])
                    S.dve(lambda e, grp=grp: e.max_index(out=ti[:, grp, 0:8], in_max=tv[:, grp, 0:8], in_values=sc[:, grp, :]),
                          r=['sc', 'tv'], w=['ti'])
                    S.dve(lambda e, grp=grp: e.match_replace(out=scr[:, 0:128], in_to_replace=tv[:, grp, 0:8], in_values=sc[:, grp, :],
                                                             imm_value=NEG), r=['sc', 'tv'], w=['scr'])
                    S.dve(lambda e, grp=grp: e.max(out=tv[:, grp, 8:16], in_=scr[:, 0:128]), r=['scr'], w=['tv'])
                    S.dve(lambda e, grp=grp: e.max_index(out=ti[:, grp, 8:16], in_max=tv[:, grp, 8:16], in_values=scr[:, 0:128]),
                          r=['scr', 'tv'], w=['ti'])
                kb.cp(tif[tl][:], ti[:], r=['ti'], w=['tif%d' % tl])
                tvv = tv[:].rearrange("p (h c) k -> p h c k", c=2)
                kb.tt(cand[:].rearrange("p h (a b) -> p h a b", a=16), bc(tvv[:, :, 0, :], 3, [128, 8, 16, 16]),
                      bc(tvv[:, :, 1, :], 2, [128, 8, 16, 16]), ALU.add, r=['tv'], w=['cand'])
                cvt, cpt = cv[tl], cpi[tl]
                ck, pk = 'cv%d' % tl, 'cpi%d' % tl
                for h in range(8):
                    S.dve(lambda e, h=h, cvt=cvt: e.max(out=cvt[:, h, 0:8], in_=cand[:, h, :]), r=['cand'], w=[ck])
                    S.dve(lambda e, h=h, cvt=cvt, cpt=cpt: e.max_index(out=cpt[:, h, 0:8], in_max=cvt[:, h, 0:8], in_values=cand[:, h, :]),
                          r=['cand', ck], w=[pk])
                    S.dve(lambda e, h=h, cvt=cvt: e.match_replace(out=scr[:, 0:256], in_to_replace=cvt[:, h, 0:8], in_values=cand[:, h, :],
                                                                  imm_value=NEG), r=['cand', ck], w=['scr'])
                    S.dve(lambda e, h=h, cvt=cvt: e.max(out=cvt[:, h, 8:16], in_=scr[:, 0:256]), r=['scr'], w=[ck])
                    S.dve(lambda e, h=h, cvt=cvt, cpt=cpt: e.max_index(out=cpt[:, h, 8:16], in_max=cvt[:, h, 8:16], in_values=scr[:, 0:256]),
                          r=['scr', ck], w=[pk])

        def frontB(g):
            for tl in range(TG):
                cvt, cpt = cv[tl], cpi[tl]
                ck, pk = 'cv%d' % tl, 'cpi%d' % tl
                tfv = tif[tl][:].rearrange("p (h c) k -> p h c k", c=2)
                kb.tt(ge[:], cvt[:], cvt[:, :, 0:1].broadcast_to([128, 8, 16]), ALU.subtract, r=[ck], w=['ge'])
                kb.af(ge[:], ge[:], AF.Exp, r=['ge'], w=['ge'])
                kb.red(gz[:, 0:8], ge[:], ALU.add, r=['ge'], w=['gz'])
                kb.recip(gz[:, 0:8], gz[:, 0:8], r=['gz'], w=['gz'])
                kb.tt(gate[:].rearrange("p (h k) -> p h k", h=8), ge[:], bc(gz[:, 0:8], 2, [128, 8, 16]), ALU.mult,
                      r=['ge', 'gz'], w=['gate'])
                S.dve(lambda e, cpt=cpt: e.tensor_single_scalar(out=cu1[:], in_=cpt[:], scalar=4, op=ALU.logical_shift_right), r=[pk], w=['cu1'])
                kb.cp(akf[:], cu1[:], r=['cu1'], w=['akf'])
                S.dve(lambda e, cpt=cpt: e.tensor_single_scalar(out=cu1[:], in_=cpt[:], scalar=15, op=ALU.bitwise_and), r=[pk, 'akf'], w=['cu1'])
                kb.cp(bkf[:], cu1[:], r=['cu1'], w=['bkf'])
                io16 = iot[:, 0:16].unsqueeze(1).unsqueeze(1).broadcast_to([128, 8, 16, 16])
                for (rk, cx, dst, dk) in ((akf, 0, idxi, 'idxi'), (bkf, 1, idxj, 'idxj')):
                    kb.tt(sel, io16, bc(rk[:], 3, [128, 8, 16, 16]), ALU.is_equal, r=['akf', 'bkf', 'iot', ck, pk], w=['cand'])
                    kb.tt(sel, sel, bc(tfv[:, :, cx, :], 2, [128, 8, 16, 16]), ALU.mult, r=['cand', 'tif%d' % tl], w=['cand'], eng='pool')
                    kb.red(dst[:].rearrange("p (h k) -> p h k", h=8), sel, ALU.add, r=['cand'], w=[dk])
                for q, (src, sk_) in enumerate(((idxi, 'idxi'), (idxj, 'idxj'), (gate, 'gate'))):
                    kb.tr(ps[:, 6, q * 128:(q + 1) * 128], src[:], idf[:], r=[sk_, 'idf'], w=['ps6'])
                kb.cp(slotT[:, tl, :, :], ps[:, 6, 0:384].rearrange("p (a c) -> p a c", a=3), r=['ps6'], w=['slotT'], eng='act')
                kb.cp(slotF[:, tl, :, :], ps[:, 6, 128:384].rearrange("p (a c) -> p a c", a=2), r=['ps6'], w=['slotF'], eng='act')

        def onehot(g):
            for tl in range(TG):
                for tb in range(128 // TB):
                    ts_ = slice(tb * TB, (tb + 1) * TB)
                    ob = tb % 2
                    Ao, Bo = Aoh[ob], Boh[ob]
                    ak_, bk_ = 'Aoh%d' % ob, 'Boh%d' % ob
                    io128 = iob[:].unsqueeze(1).broadcast_to([128, TB, 128])
                    kb.tt(Ao[:], io128, bc(slotT[:, tl, 0, ts_], 2, [128, TB, 128]), ALU.is_equal, r=['slotT'] + CST, w=[ak_])
                    bkeys = ['%s_%d' % (bk_, q_) for q_ in range(TB)]
                    for tt_ in range(TB):
                        tok = tb * TB + tt_
                        kb.stt(Bo[:, tt_, :], iob[:], slotF[:, tl, 0, tok:tok + 1], slotF[:, tl, 1, tok:tok + 1].broadcast_to([128, 128]),
                               ALU.is_equal, ALU.mult, r=['slotF'] + CST, w=[bkeys[tt_]])
                    for q4 in range(TB // 4):
                        bk = 4 + q4 % 2
                        for u in range(4):
                            tt_ = q4 * 4 + u
                            kb.mm(ps[:, bk, u * 128:(u + 1) * 128], Ao[:, tt_, :], Bo[:, tt_, :], r=[ak_, bkeys[tt_]], w=['ps%d' % bk])
                        tg0 = tl * 128 + tb * TB + q4 * 4
                        kb.cp(GG[:, tg0:tg0 + 4, :], ps[:, bk, :].rearrange("p (a c) -> p a c", a=4), r=['ps%d' % bk], w=['GG'],
                              eng='act')

        def experts(g, extra, extraB):
            gp = g % 2
            hk = 'h2T%d' % gp
            per = (len(extra) + PEER_A_STEPS - 1) // PEER_A_STEPS if extra else 0
            pos = [0]
            perB = (len(extraB) + 23) // 24 if extraB else 0
            posB = [0]

            def load(jh):
                p = jh % NBUF
                jb, half = jh // 2, jh % 2
                kb.dma(ut[p][:].rearrange("p a b c -> p (a b c)"), uS[jb][:, half * 2048:(half + 1) * 2048], r=['uS'], w=['ut%d' % p])
                kb.dma(vt[p][:], vSv[:, jh * 2:(jh + 1) * 2, :], r=['vS'], w=['vt%d' % p])

            def act(j):
                jh, jj = j // 2, j % 2
                p = jh % NBUF
                bkA = 4 + j % 2
                for dc in range(8):
                    kb.mm(ps[:, bkA, 0:NTK], ut[p][:, jj, dc, :], h2T[gp][:, dc, :], start=(dc == 0), stop=(dc == 7),
                          r=['ut%d' % p, hk], w=['ps%d' % bkA])

            load(0)
            load(1)
            act(0)
            for j in range(128):
                jh, jj = j // 2, j % 2
                p = jh % NBUF
                pa = j % 2
                bkA = 4 + pa
                if jj == 0 and jh + 2 < 64:
                    load(jh + 2)
                kb.af(ga[pa][:], ps[:, bkA, 0:NTK], AF.Gelu, r=['ps%d' % bkA], w=['ga%d' % pa])
                if j + 1 < 128:
                    act(j + 1)
                kb.tt(coef[pa][:], ga[pa][:], GG[:, :, j], ALU.mult, r=['ga%d' % pa, 'GG'], w=['coef%d' % pa], eng='pool')
                for tl in range(TG):
                    for n2 in range(2):
                        bkY = tl * 2 + n2
                        kb.mm(ps[:, bkY, :], coef[pa][:, tl * 128:(tl + 1) * 128], vt[p][:, jj, n2 * 512:(n2 + 1) * 512],
                              start=(j == 0), stop=(j == 127), r=['coef%d' % pa, 'vt%d' % p], w=['ps%d' % bkY])
                if extra and pos[0] < len(extra):
                    S.ops.extend(extra[pos[0]:pos[0] + per])
                    pos[0] += per
                if j >= PEER_B_START and extraB and posB[0] < len(extraB):
                    S.ops.extend(extraB[posB[0]:posB[0] + perB])
                    posB[0] += perB
            if extra and pos[0] < len(extra):
                S.ops.extend(extra[pos[0]:])
            if extraB and posB[0] < len(extraB):
                S.ops.extend(extraB[posB[0]:])

        def finish(g):
            gp = g % 2
            for tl in range(TG):
                t0 = (g * TG + tl) * 128
                xk = 'xt%d_%d' % (gp, tl)
                xtt = xt[gp][tl]
                kb.tt(fin[:, 0:512], xtt[:, 0:512], ps[:, tl * 2, :], ALU.add, r=[xk, 'ps%d' % (tl * 2)], w=['fin'])
                kb.tt(fin[:, 512:1024], xtt[:, 512:1024], ps[:, tl * 2 + 1, :], ALU.add, r=[xk, 'ps%d' % (tl * 2 + 1)], w=['fin'])
                kb.af(junk2[:], fin[:], AF.Square, accum=ss2[:, 3:4], r=['fin'], w=['junk2', 'ss3'])
                kb.af(ss2[:, 4:5], ss2[:, 3:4], AF.Sqrt, scale=1.0 / D, bias=RMS_EPS, r=['ss3'], w=['ss4'])
                kb.recip(ss2[:, 5:6], ss2[:, 4:5], r=['ss4'], w=['ss5'])
                kb.stt(fin[:], fin[:], ss2[:, 5:6], lnf_b[:], ALU.mult, ALU.mult, r=['fin', 'ss5'] + CST, w=['fin'])
                kb.dma(out[t0:t0 + 128, :], fin[:], r=['fin'], w=['out'])

        def cap(fn, *args):
            saved = S.ops
            S.ops = []
            fn(*args)
            got = S.ops
            S.ops = saved
            return got

        frontA(0)
        frontB(0)
        for g in range(NG):
            oh = cap(onehot, g)
            fn_ = cap(finish, g - 1) if g > 0 else []
            ratio = max(1, len(oh) // max(1, len(fn_))) if fn_ else 0
            pi = 0
            for k_, op_ in enumerate(oh):
                S.ops.append(op_)
                if fn_ and (k_ + 1) % ratio == 0 and pi < len(fn_):
                    S.ops.append(fn_[pi])
                    pi += 1
            S.ops.extend(fn_[pi:])
            extra, extraB = [], []
            if g + 1 < NG:
                saved = S.ops
                S.ops = []
                frontA(g + 1)
                extra = S.ops
                S.ops = []
                frontB(g + 1)
                extraB = S.ops
                S.ops = saved
            experts(g, extra, extraB)
        finish(NG - 1)
        S.flush()


NSEQ_CORE = 4
SEQ_LEN = 2048
N_CORES = 8


def kernel(**inputs):
    x = np.asarray(inputs["x"], dtype=np.float32)
    B, T, Dm = x.shape
    assert B == NSEQ_CORE * N_CORES and T == SEQ_LEN and Dm == D
    nc = build_program(NSEQ_CORE, SEQ_LEN)
    params = {k: np.ascontiguousarray(np.asarray(inputs[k], dtype=np.float32)) for k in PARAM_SHAPES}
    in_maps = []
    for c in range(N_CORES):
        m = dict(params)
        m["x"] = np.ascontiguousarray(x[c * NSEQ_CORE:(c + 1) * NSEQ_CORE].reshape(NSEQ_CORE * SEQ_LEN, D))
        in_maps.append(m)
    res = run_bass_kernel_spmd(nc, in_maps, core_ids=list(range(N_CORES)))
    outs = [np.asarray(r["out"], dtype=np.float32).reshape(NSEQ_CORE, SEQ_LEN, D) for r in res.results]
    return np.concatenate(outs, axis=0)
```
